# Optimizing a Trainium2 kernel written in Bass

```python
import jax
import jax.numpy as jnp
from jax import lax
import numpy as np

D_MODEL = 1024
BATCH = 4
SEQ = 8192
DEPTH = 1

HEAD_DIM = 64
ROPE_THETA = 10000.0
RMS_EPS = 1e-6
Q_BLOCK = 128

A_HEADS = 8
A_KV_HEADS = 2
IDX_HEADS = 4
IDX_DIM = 64
TOPK_MAX = 256

B_PATTERNS = ((128, 1), (512, 4), (2048, 16))
B_N_GROUPS = 3
B_HEADS = 8

MEM_LEN = 256
M_HEADS = 4
M_HEAD_DIM = 128

BRANCH_WIDTH = 512
N_BRANCHES = 3

MOE_GROUPS = 4
EXPERTS_PER_GROUP = 4
N_EXPERTS = 16
MOE_TOPK = 2
EXPERT_HIDDEN = 512

IN_SPLITS = (512, 128, 128, 256, 64, 4, 1536, 1536, 1536, 512)
IN_WIDTH = 6212

kernel_name = 'hybrid_dsa_dilated_memory_hmoe'


def rmsnorm(x, g):
    xf = x.astype(jnp.float32)
    y = xf * lax.rsqrt(jnp.mean(xf * xf, axis=-1, keepdims=True) + RMS_EPS)
    return (y * g.astype(jnp.float32)).astype(x.dtype)


def rope(x):
    s, dh = x.shape[1], x.shape[-1]
    half = dh // 2
    inv = ROPE_THETA ** (-jnp.arange(half, dtype=jnp.float32) / half)
    ang = jnp.arange(s, dtype=jnp.float32)[:, None] * inv[None, :]
    cos = jnp.cos(ang)[None, :, None, :]
    sin = jnp.sin(ang)[None, :, None, :]
    xf = x.astype(jnp.float32)
    x1, x2 = xf[..., :half], xf[..., half:]
    return jnp.concatenate([x1 * cos - x2 * sin, x2 * cos + x1 * sin], axis=-1).astype(x.dtype)


def split_cols(a, sizes):
    out, off = [], 0
    for n in sizes:
        out.append(a[..., off:off + n])
        off += n
    return out


def sweep_query_blocks(fn, seq_len):
    out = lax.map(fn, jnp.arange(seq_len // Q_BLOCK))
    nb, b = out.shape[0], out.shape[1]
    return jnp.moveaxis(out, 0, 1).reshape(b, nb * Q_BLOCK, *out.shape[3:])


def dsa_attention(q, k, v, q_idx, k_idx, w_idx):
    b, s, h, dh = q.shape
    kvh = k.shape[2]
    g = h // kvh
    topk = min(TOPK_MAX, s // 4)
    key_pos = jnp.arange(s)
    local = jnp.arange(Q_BLOCK)
    k_idx_f = k_idx.astype(jnp.float32)

    def block(i):
        q0 = i * Q_BLOCK
        qpos = q0 + local
        qi = lax.dynamic_slice_in_dim(q_idx, q0, Q_BLOCK, axis=1).astype(jnp.float32)
        wi = lax.dynamic_slice_in_dim(w_idx, q0, Q_BLOCK, axis=1).astype(jnp.float32)
        dots = jnp.einsum('bqhd,bsd->bqhs', qi, k_idx_f) * (IDX_DIM ** -0.5)
        score = jnp.einsum('bqhs,bqh->bqs', jax.nn.relu(dots), wi) * (IDX_HEADS ** -0.5)
        causal = key_pos[None, :] <= qpos[:, None]
        score = jnp.where(causal[None], score, -jnp.inf)
        _, sel = lax.top_k(score, topk)
        valid = sel <= qpos[None, :, None]
        ks = jax.vmap(lambda kk, ii: kk[ii])(k, sel)
        vs = jax.vmap(lambda vv, ii: vv[ii])(v, sel)
        qb = lax.dynamic_slice_in_dim(q, q0, Q_BLOCK, axis=1).reshape(b, Q_BLOCK, kvh, g, dh)
        logits = jnp.einsum('bqkgd,bqjkd->bqkgj', qb, ks).astype(jnp.float32) * (dh ** -0.5)
        logits = jnp.where(valid[:, :, None, None, :], logits, -jnp.inf)
        p = jax.nn.softmax(logits, axis=-1)
        o = jnp.einsum('bqkgj,bqjkd->bqkgd', p.astype(vs.dtype), vs)
        return o.reshape(b, Q_BLOCK, h * dh)

    return sweep_query_blocks(block, s)


def dilated_attention(q, k, v):
    b, s, ng, h, dh = q.shape
    local = jnp.arange(Q_BLOCK)
    qs = [q[:, :, gi] for gi in range(ng)]
    kp = [jnp.pad(k[:, :, gi], ((0, 0), (w, 0), (0, 0), (0, 0))) for gi, (w, _) in enumerate(B_PATTERNS)]
    vp = [jnp.pad(v[:, :, gi], ((0, 0), (w, 0), (0, 0), (0, 0))) for gi, (w, _) in enumerate(B_PATTERNS)]

    def block(i):
        q0 = i * Q_BLOCK
        outs, lses = [], []
        for gi, (window, dil) in enumerate(B_PATTERNS):
            steps = jnp.arange(window // dil + 1)
            back = local[:, None] - steps[None, :] * dil
            valid = (q0 + back) >= 0
            rel = window + back
            ks = lax.dynamic_slice_in_dim(kp[gi], q0, window + Q_BLOCK, axis=1)[:, rel]
            vs = lax.dynamic_slice_in_dim(vp[gi], q0, window + Q_BLOCK, axis=1)[:, rel]
            qg = lax.dynamic_slice_in_dim(qs[gi], q0, Q_BLOCK, axis=1)
            logits = jnp.einsum('bqhd,bqjhd->bqhj', qg, ks).astype(jnp.float32) * (dh ** -0.5)
            logits = jnp.where(valid[None, :, None, :], logits, -jnp.inf)
            lse = jax.nn.logsumexp(logits, axis=-1)
            p = jnp.exp(logits - lse[..., None])
            outs.append(jnp.einsum('bqhj,bqjhd->bqhd', p.astype(vs.dtype), vs))
            lses.append(lse)
        alpha = jax.nn.softmax(jnp.stack(lses, axis=2), axis=2)
        o = jnp.einsum('bqgh,bqghd->bqhd', alpha.astype(outs[0].dtype), jnp.stack(outs, axis=2))
        return o.reshape(b, Q_BLOCK, h * dh)

    return sweep_query_blocks(block, s)


def memory_attention(qm, mem_n, w_mem_kv):
    b, m, _ = mem_n.shape
    km, vm = split_cols(mem_n @ w_mem_kv, (M_HEADS * M_HEAD_DIM, M_HEADS * M_HEAD_DIM))
    km = km.reshape(b, m, M_HEADS, M_HEAD_DIM)
    vm = vm.reshape(b, m, M_HEADS, M_HEAD_DIM)
    logits = jnp.einsum('bshd,bmhd->bhsm', qm, km).astype(jnp.float32) * (M_HEAD_DIM ** -0.5)
    p = jax.nn.softmax(logits, axis=-1)
    o = jnp.einsum('bhsm,bmhd->bshd', p.astype(vm.dtype), vm)
    return o.reshape(b, qm.shape[1], M_HEADS * M_HEAD_DIM)


def hier_moe(xn, w_group, b_group, w_sub, b_sub, w1, w3, w2):
    b, s, d = xn.shape
    t = xn.reshape(b * s, d)
    gl = (t @ w_group).astype(jnp.float32) + b_group.astype(jnp.float32)
    gp = jax.nn.softmax(gl, axis=-1)
    g_w, g_sel = lax.top_k(gp, 1)
    sl = jnp.einsum('td,gde->tge', t, w_sub).astype(jnp.float32) + b_sub.astype(jnp.float32)
    sl = jnp.take_along_axis(sl, g_sel[:, :, None], axis=1)[:, 0]
    sp = jax.nn.softmax(sl, axis=-1)
    top_p, top_e = lax.top_k(sp, MOE_TOPK)
    top_p = top_p / jnp.sum(top_p, axis=-1, keepdims=True)
    expert_id = g_sel * EXPERTS_PER_GROUP + top_e
    combine = jnp.einsum('tk,tke->te', g_w * top_p, jax.nn.one_hot(expert_id, N_EXPERTS, dtype=jnp.float32))
    out = jnp.zeros((b * s, d), jnp.float32)
    for e in range(N_EXPERTS):
        hid = jax.nn.silu(t @ w1[e]) * (t @ w3[e])
        out = out + combine[:, e:e + 1] * (hid @ w2[e]).astype(jnp.float32)
    return out.astype(xn.dtype).reshape(b, s, d)


def setup_inputs(seed: int = 0) -> dict:
    key = jax.random.key(seed)
    ks = jax.random.split(key, 19)
    f32 = jnp.float32
    L, D = DEPTH, D_MODEL

    def dense(k, shape, fan_in):
        return jax.random.normal(k, shape, f32) * (fan_in ** -0.5)

    def gain(k, shape):
        return 1.0 + 0.02 * jax.random.normal(k, shape, f32)

    def small(k, shape, scale):
        return scale * jax.random.normal(k, shape, f32)

    return {
        'x': jax.random.normal(ks[0], (BATCH, SEQ, D), f32),
        'mem': jax.random.normal(ks[1], (BATCH, MEM_LEN, D), f32),
        'g_mix': gain(ks[2], (L, D)),
        'g_mem': gain(ks[3], (L, D)),
        'w_in': dense(ks[4], (L, D, IN_WIDTH), D),
        'w_mem_kv': dense(ks[5], (L, D, 2 * M_HEADS * M_HEAD_DIM), D),
        'w_gate': dense(ks[6], (L, D, N_BRANCHES * D), D),
        'b_gate': small(ks[7], (L, N_BRANCHES * D), 0.1),
        'w_branch': dense(ks[8], (L, N_BRANCHES, BRANCH_WIDTH, D), BRANCH_WIDTH),
        'w_out': dense(ks[9], (L, D, D), D),
        'g_ffn': gain(ks[10], (L, D)),
        'w_group': dense(ks[11], (L, D, MOE_GROUPS), D),
        'b_group': small(ks[12], (L, MOE_GROUPS), 0.01),
        'w_sub': dense(ks[13], (L, MOE_GROUPS, D, EXPERTS_PER_GROUP), D),
        'b_sub': small(ks[14], (L, MOE_GROUPS, EXPERTS_PER_GROUP), 0.01),
        'w1': dense(ks[15], (L, N_EXPERTS, D, EXPERT_HIDDEN), D),
        'w3': dense(ks[16], (L, N_EXPERTS, D, EXPERT_HIDDEN), D),
        'w2': dense(ks[17], (L, N_EXPERTS, EXPERT_HIDDEN, D), EXPERT_HIDDEN),
        'g_final': gain(ks[18], (D,)),
    }


def reference(x, mem, g_mix, g_mem, w_in, w_mem_kv, w_gate, b_gate, w_branch, w_out, g_ffn,
              w_group, b_group, w_sub, b_sub, w1, w3, w2, g_final):
    b, s, d = x.shape
    h = x
    for l in range(DEPTH):
        n = rmsnorm(h, g_mix[l])
        aq, ak, av, iq, ik, iw, bq, bk, bv, mq = split_cols(n @ w_in[l], IN_SPLITS)
        aq = rope(aq.reshape(b, s, A_HEADS, HEAD_DIM))
        ak = rope(ak.reshape(b, s, A_KV_HEADS, HEAD_DIM))
        av = av.reshape(b, s, A_KV_HEADS, HEAD_DIM)
        iq = rope(iq.reshape(b, s, IDX_HEADS, IDX_DIM))
        ik = rope(ik.reshape(b, s, 1, IDX_DIM))[:, :, 0]
        y_a = dsa_attention(aq, ak, av, iq, ik, iw)
        bq = rope(bq.reshape(b, s, B_N_GROUPS * B_HEADS, HEAD_DIM)).reshape(b, s, B_N_GROUPS, B_HEADS, HEAD_DIM)
        bk = rope(bk.reshape(b, s, B_N_GROUPS * B_HEADS, HEAD_DIM)).reshape(b, s, B_N_GROUPS, B_HEADS, HEAD_DIM)
        bv = bv.reshape(b, s, B_N_GROUPS, B_HEADS, HEAD_DIM)
        y_b = dilated_attention(bq, bk, bv)
        y_m = memory_attention(mq.reshape(b, s, M_HEADS, M_HEAD_DIM), rmsnorm(mem, g_mem[l]), w_mem_kv[l])
        gates = jax.nn.sigmoid((n @ w_gate[l] + b_gate[l]).astype(jnp.float32)).astype(n.dtype)
        gates = gates.reshape(b, s, N_BRANCHES, d)
        merged = (gates[:, :, 0] * (y_a @ w_branch[l, 0])
                  + gates[:, :, 1] * (y_b @ w_branch[l, 1])
                  + gates[:, :, 2] * (y_m @ w_branch[l, 2]))
        h = h + merged @ w_out[l]
        h = h + hier_moe(rmsnorm(h, g_ffn[l]), w_group[l], b_group[l], w_sub[l], b_sub[l], w1[l], w3[l], w2[l])
    return rmsnorm(h, g_final)
```

```python
import contextlib
import os
RMS_CUT = int(os.environ.get('RMS_CUT', '9'))
BM_CUT = int(os.environ.get('BM_CUT', '9'))
import numpy as np
import concourse.bass as bass
import concourse.mybir as mybir
from concourse.bass_utils import run_bass_kernel_spmd

F32 = mybir.dt.float32
BF16 = mybir.dt.bfloat16
AF = mybir.ActivationFunctionType
ALU = mybir.AluOpType
AX = mybir.AxisListType.X

D = 1024
NEG = -30000.0
NINF = -1.0e30
NBIS = 17
B_PAT = ((128, 1), (512, 4), (2048, 16))
B_NB = (1, 4, 16)
B_MOFF = (0, 2, 7)
ENGS = ("pe", "act", "dve", "pool", "sp")
EPOCH = 12000


class Res:
    __slots__ = ("w", "r", "excl")

    def __init__(self, excl=False):
        self.w = None
        self.r = []
        self.excl = excl


class Prog:
    def __init__(self, nc, n_dma_sems=24):
        self.nc = nc
        self.q = {e: [] for e in ENGS}
        self.sem = {}
        self.cnt = {}
        self.key_eng = {}
        self.cur = {}
        for e in ENGS:
            self._new_epoch(e, 0)
        self.ndma = n_dma_sems
        for k in range(n_dma_sems):
            key = "dma%d" % k
            self.sem[key] = nc.alloc_semaphore(name="d%d" % k)
            self.cnt[key] = 0
            self.key_eng[key] = "dma"
        self.dma_rr = 0
        self.known = {e: {} for e in ENGS}
        self.nwaits = 0
        self.nops = 0

    def _new_epoch(self, e, k):
        key = "%s#%d" % (e, k)
        self.sem[key] = self.nc.alloc_semaphore(name="s_%s_%d" % (e, k))
        self.cnt[key] = 0
        self.key_eng[key] = e
        self.cur[e] = (key, k)

    def _need(self, eng, tok, waits):
        if tok is None:
            return
        key, val = tok
        if self.known[eng].get(key, 0) >= val:
            return
        if waits.get(key, 0) < val:
            waits[key] = val

    def _deps(self, eng, reads, writes, same_sync):
        waits = {}
        for r in reads:
            if r.excl:
                for t in r.r:
                    if self.key_eng[t[0]] != eng:
                        self._need(eng, t, waits)
            if r.w is not None:
                if self.key_eng[r.w[0]] == eng and not same_sync:
                    continue
                self._need(eng, r.w, waits)
        for w in writes:
            if w.w is not None:
                if not (self.key_eng[w.w[0]] == eng and not same_sync):
                    self._need(eng, w.w, waits)
            for t in w.r:
                if self.key_eng[t[0]] == eng:
                    continue
                self._need(eng, t, waits)
        for k, v in waits.items():
            self.known[eng][k] = v
        return waits

    def _commit(self, tok, reads, writes):
        for r in reads:
            r.r.append(tok)
            if len(r.r) > 48:
                best = {}
                for k, v in r.r:
                    if best.get(k, 0) < v:
                        best[k] = v
                r.r = list(best.items())
        for w in writes:
            w.w = tok
            w.r = []

    def op(self, eng, fn, reads=(), writes=(), same_sync=True):
        if eng == "pe":
            same_sync = False
        waits = self._deps(eng, reads, writes, same_sync)
        key, k = self.cur[eng]
        if self.cnt[key] >= EPOCH:
            self._new_epoch(eng, k + 1)
            key, k = self.cur[eng]
        self.cnt[key] += 1
        tok = (key, self.cnt[key])
        self._commit(tok, reads, writes)
        self.q[eng].append((waits, fn, (key, 1)))
        self.nwaits += len(waits)
        self.nops += 1
        return tok

    def dma(self, qeng, fn, reads=(), writes=()):
        k = self.dma_rr
        self.dma_rr = (self.dma_rr + 1) % self.ndma
        key = "dma%d" % k
        waits = self._deps(qeng, reads, writes, True)
        prev = self.cnt[key]
        if prev > 0 and self.known[qeng].get(key, 0) < prev:
            waits[key] = max(waits.get(key, 0), prev)
            self.known[qeng][key] = prev
        self.cnt[key] += 16
        tok = (key, self.cnt[key])
        self._commit(tok, reads, writes)
        self.q[qeng].append((waits, fn, (key, 16)))
        self.nwaits += len(waits)
        self.nops += 1
        return tok

    def wait_tokens(self, eng, toks):
        waits = {}
        for t in toks:
            self._need(eng, t, waits)
        for k, v in waits.items():
            self.known[eng][k] = v
        self.q[eng].append((waits, None, None))

    def emit(self):
        nc = self.nc
        with nc.Block() as block:
            def mk(ename):
                def body(e):
                    for waits, fn, inc in self.q[ename]:
                        for k, v in waits.items():
                            e.wait_ge(self.sem[k], v)
                        if fn is not None:
                            ins = fn(e)
                            ins.then_inc(self.sem[inc[0]], inc[1])
                return body
            block.tensor(mk("pe"))
            block.scalar(mk("act"))
            block.vector(mk("dve"))
            block.gpsimd(mk("pool"))
            block.sync(mk("sp"))


WK_COLS = 3392
WQ_COLS = 2820
K_SLABS = [("a", 0, 320)] + [("bk%d" % g, 320 + 512 * g, 512) for g in range(3)] + \
          [("bv%d" % g, 1856 + 512 * g, 512) for g in range(3)]
Q_SLABS = [("aq", 0, 512), ("iq", 512, 260)] + [("bq%d" % g, 772 + 512 * g, 512) for g in range(3)] + \
          [("mq", 2308, 512)]


def build(S, QB, dbg=False, stop=None):
    NBLK = S // 128
    NQ = NBLK // 2
    NIT = NQ // QB
    N = QB * 128
    assert NBLK % 4 == 0 and NQ % QB == 0

    nc = bass.Bass("TRN2", target_bir_lowering=False)

    def din(name, shape, dt=F32):
        return nc.dram_tensor(name, shape, dt, kind="ExternalInput").ap()

    def dscr(name, shape, dt=BF16):
        return nc.dram_tensor(name, shape, dt).ap()

    xs = din("xs", [S, D])
    cs = din("cs", [S, 64])
    memx = din("memx", [256, D])
    wk = din("wk", [D, WK_COLS])
    wq = din("wq", [D, WQ_COLS])
    wg = din("wg", [D, 3072])
    wbr = din("wbr", [1536, D])
    wo = din("wo", [D, D])
    wmem = din("wmem", [D, 1024])
    w1 = din("w1", [16 * D, 512])
    w3 = din("w3", [16 * D, 512])
    w2 = din("w2", [16 * 512, D])
    wr = din("wr", [D, 20])
    gcols_d = din("gcols", [128, 24])
    bgate_d = din("bgate", [128, 24])
    gfin_d = din("gfin", [128, D])
    rbias_d = din("rbias", [128, 20])
    ident_d = din("ident", [128, 128])
    cm_d = din("dsa_cm", [128, 256])
    m0_d = din("dsa_m0", [128, 128])
    mb_d = din("mbias", [128, 24 * 128])
    mb0_d = din("mb0", [128, 24 * 128])
    out_d = nc.dram_tensor("out", [NQ * 128, D], F32, kind="ExternalOutput").ap()
    dbg_d = {}
    if dbg:
        for nm, shp in (("d_ya", [64, 8 * N]), ("d_yb", [64, 8 * N]), ("d_ym", [128, 4 * N]), ("d_h", [128, QB * D])):
            dbg_d[nm] = nc.dram_tensor(nm, shp, F32, kind="ExternalOutput").ap()

    wk_b = dscr("wk_b", [D, WK_COLS])
    wq_b = dscr("wq_b", [D, WQ_COLS])
    wg_b = dscr("wg_b", [D, 3072])
    wbr_b = dscr("wbr_b", [1536, D])
    wo_b = dscr("wo_b", [D, D])
    wmem_b = dscr("wmem_b", [D, 1024])
    w1_b = dscr("w1_b", [16 * D, 512])
    w3_b = dscr("w3_b", [16 * D, 512])
    w2_b = dscr("w2_b", [16 * 512, D])
    KaT_d = dscr("KaT_d", [128, S])
    kiT_d = dscr("kiT_d", [64, S])
    Va_d = dscr("Va_d", [S, 256])
    bkT_d = dscr("bkT_d", [128, 3, 4, S])
    Vb_d = dscr("Vb_d", [S, 3, 1024])

    P = Prog(nc)
    es = contextlib.ExitStack()
    with es:
        def sb(name, shape, dt=F32):
            return es.enter_context(nc.sbuf_tensor("s_" + name, shape, dt))

        pbank = [es.enter_context(nc.psum_tensor("pb%d" % i, [128, 512], F32)) for i in range(8)]
        Rb = [Res(excl=True) for _ in range(8)]
        rot_state = [0]

        def rot():
            k = 4 + rot_state[0]
            rot_state[0] = (rot_state[0] + 1) % 4
            return pbank[k], Rb[k]

        identf = sb("identf", [128, 128]); identb = sb("identb", [128, 128], BF16)
        i4b = sb("i4b", [128, 512], BF16)
        ones_b = sb("ones_b", [128, 128], BF16)
        gcols = sb("gcols", [128, 24]); bgate = sb("bgate", [128, 24])
        gfin = sb("gfin", [128, D]); rbias = sb("rbias", [128, 20])
        cm = sb("cm", [128, 256]); m0 = sb("m0", [128, 128])
        mbt = sb("mbt", [128, 24 * 128], BF16); mb0t = sb("mb0t", [128, 24 * 128], BF16)
        pow2 = sb("pow2", [128, NBIS + 1])
        wr32 = sb("wr32", [128, 8, 20])
        kmT = sb("kmT", [128, 4, 256], BF16); vmb = sb("vmb", [128, 2, 512], BF16)
        Rc = Res()

        def ld_const(dst, src):
            P.dma("sp", lambda e: e.dma_start(out=dst, in_=src), writes=[Rc])

        ld_const(identf[:], ident_d)
        ld_const(gcols[:], gcols_d); ld_const(bgate[:], bgate_d); ld_const(gfin[:], gfin_d)
        ld_const(rbias[:], rbias_d); ld_const(cm[:], cm_d); ld_const(m0[:], m0_d)
        ld_const(wr32[:], wr.rearrange("(c p) n -> p c n", p=128))
        P.op("dve", lambda e: e.tensor_copy(out=identb[:], in_=identf[:]), reads=[Rc], writes=[Rc])
        for j in range(4):
            P.op("dve", lambda e, j=j: e.tensor_copy(out=i4b[:, j * 128:(j + 1) * 128], in_=identf[:]), reads=[Rc], writes=[Rc])
        P.op("dve", lambda e: e.memset(ones_b[:], 1.0), writes=[Rc])
        for i in range(NBIS + 1):
            P.op("dve", lambda e, i=i: e.memset(pow2[:, i:i + 1], 2.0 ** (-i)), writes=[Rc])

        def finish0():
            toks = [("dma%d" % k, P.cnt["dma%d" % k]) for k in range(P.ndma) if P.cnt["dma%d" % k] > 0]
            toks += [(P.cur[e_][0], P.cnt[P.cur[e_][0]]) for e_ in ("pe", "act", "dve", "pool") if P.cnt[P.cur[e_][0]] > 0]
            P.wait_tokens("sp", toks)
            P.emit()
        if stop == "c":
            finish0(); return nc, P
        Rw_b = Res()
        conv_hist = []

        def convert(src, dst):
            fs = src.rearrange("r c -> (r c)").rearrange("(n k) -> n k", k=1024)
            fd = dst.rearrange("r c -> (r c)").rearrange("(n k) -> n k", k=1024)
            n = fs.shape[0]
            for r0 in range(0, n, 1024):
                r1_ = min(n, r0 + 1024)
                if len(conv_hist) >= 4:
                    P.wait_tokens("pool", [conv_hist[-4]])
                conv_hist.append(P.dma("pool", lambda e, r0=r0, r1_=r1_: e.dma_start(out=fd[r0:r1_, :], in_=fs[r0:r1_, :])))

        for s_, d_ in ((wmem, wmem_b), (wk, wk_b), (wq, wq_b), (wg, wg_b), (wbr, wbr_b), (wo, wo_b),
                       (w1, w1_b), (w3, w3_b), (w2, w2_b)):
            convert(s_, d_)
        conv_tokens = []
        for k in range(P.ndma):
            key = "dma%d" % k
            if P.cnt[key] > 0:
                conv_tokens.append((key, P.cnt[key]))

        if stop == "conv":
            finish0(); return nc, P
        ss_t = sb("ss_t", [128, 4]); rs_t = sb("rs_t", [128, 4])
        sqj = sb("sqj", [128, D], BF16)
        xb_t = [sb("xb0", [128, D], BF16)] * 2
        Rstat = [Res() for _ in range(4)]
        Rsqj = Res()
        Rxb = [Res()] * 2
        rms_ctr = [0]

        def rms_T(x_ap, Rx, gofs, dst_fn, Rdst):
            i = rms_ctr[0] % 4
            j = rms_ctr[0] % 2
            rms_ctr[0] += 1
            ss = ss_t[:, i:i + 1]; rs = rs_t[:, i:i + 1]
            P.op("act", lambda e: e.activation(out=sqj[:], in_=x_ap, func=AF.Square, accum_out=ss),
                 reads=[Rx], writes=[Rsqj, Rstat[i]])
            if RMS_CUT <= 1:
                return i
            P.op("dve", lambda e: e.tensor_scalar(out=rs, in0=ss, scalar1=1.0 / D, scalar2=1e-6, op0=ALU.mult, op1=ALU.add),
                 reads=[Rstat[i]], writes=[Rstat[i]])
            P.op("act", lambda e: e.activation(out=rs, in_=rs, func=AF.Sqrt),
                 reads=[Rstat[i]], writes=[Rstat[i]])
            P.op("dve", lambda e: e.reciprocal(out=rs, in_=rs), reads=[Rstat[i]], writes=[Rstat[i]])
            if RMS_CUT <= 2:
                return i
            xb = xb_t[j]
            P.op("dve", lambda e: e.tensor_scalar(out=xb[:], in0=x_ap, scalar1=rs, scalar2=None, op0=ALU.mult),
                 reads=[Rx, Rstat[i]], writes=[Rxb[j]])
            if RMS_CUT <= 3:
                return i
            for half in range(2):
                pb, Rp = rot()
                pbb = pb[:].bitcast(BF16)
                for c4 in range(4):
                    c = half * 4 + c4
                    P.op("pe", lambda e, c=c, c4=c4, pbb=pbb: e.transpose(out=pbb[:, c4 * 128:(c4 + 1) * 128], in_=xb[:, c * 128:(c + 1) * 128], identity=identb[:]),
                         reads=[Rxb[j], Rc], writes=[Rp])
                if RMS_CUT <= 4:
                    continue
                for c4 in range(4):
                    c = half * 4 + c4
                    eng = "dve"
                    if eng == "dve":
                        P.op("dve", lambda e, c=c, c4=c4, pbb=pbb: e.tensor_scalar(out=dst_fn(c), in0=pbb[:, c4 * 128:(c4 + 1) * 128], scalar1=gcols[:, gofs + c:gofs + c + 1], scalar2=None, op0=ALU.mult),
                             reads=[Rp, Rc], writes=[Rdst])
                    else:
                        P.op("act", lambda e, c=c, c4=c4, pbb=pbb: e.activation(out=dst_fn(c), in_=pbb[:, c4 * 128:(c4 + 1) * 128], func=AF.Copy, scale=gcols[:, gofs + c:gofs + c + 1]),
                             reads=[Rp, Rc], writes=[Rdst])
            return i

        rt = [sb("rt%d" % i, [128, 256]) for i in range(4)]
        Rrt = [Res() for _ in range(4)]

        def rope(ps_ap, Rps, H, cs_ap, Rcs, out_ap, Rout, perm=False):
            n = H * 32
            if os.environ.get("NOROPE"):
                P.op("act", lambda e: e.copy(out=out_ap, in_=ps_ap), reads=[Rps], writes=[Rout])
                return
            if perm:
                x = ps_ap.rearrange("p (g j t d) -> p g j t d", g=2, t=2, d=32)
                o = out_ap.rearrange("p (j g t d) -> p g j t d", g=2, t=2, d=32)
                x1, x2 = x[:, :, :, 0, :], x[:, :, :, 1, :]
                o1, o2 = o[:, :, :, 0, :], o[:, :, :, 1, :]
                cos = cs_ap[:, 0:32].unsqueeze(1).unsqueeze(1).to_broadcast([128, 2, 4, 32])
                sin = cs_ap[:, 32:64].unsqueeze(1).unsqueeze(1).to_broadcast([128, 2, 4, 32])
                tv = [rt[i][:, 0:n].rearrange("p (g j d) -> p g j d", g=2, d=32) for i in range(4)]
            else:
                x = ps_ap.rearrange("p (h t d) -> p h t d", t=2, d=32)
                o = out_ap.rearrange("p (h t d) -> p h t d", t=2, d=32)
                x1, x2 = x[:, :, 0, :], x[:, :, 1, :]
                o1, o2 = o[:, :, 0, :], o[:, :, 1, :]
                cos = cs_ap[:, 0:32].unsqueeze(1).to_broadcast([128, H, 32])
                sin = cs_ap[:, 32:64].unsqueeze(1).to_broadcast([128, H, 32])
                tv = [rt[i][:, 0:n].rearrange("p (h d) -> p h d", d=32) for i in range(4)]
            P.op("dve", lambda e: e.tensor_tensor(out=tv[0], in0=x1, in1=cos, op=ALU.mult), reads=[Rps, Rcs], writes=[Rrt[0]])
            P.op("dve", lambda e: e.tensor_tensor(out=tv[1], in0=x2, in1=sin, op=ALU.mult), reads=[Rps, Rcs], writes=[Rrt[1]])
            P.op("dve", lambda e: e.tensor_tensor(out=tv[2], in0=x2, in1=cos, op=ALU.mult), reads=[Rps, Rcs], writes=[Rrt[2]])
            P.op("dve", lambda e: e.tensor_tensor(out=tv[3], in0=x1, in1=sin, op=ALU.mult), reads=[Rps, Rcs], writes=[Rrt[3]])
            P.op("dve", lambda e: e.tensor_tensor(out=o1, in0=tv[0], in1=tv[1], op=ALU.subtract), reads=[Rrt[0], Rrt[1]], writes=[Rout])
            P.op("dve", lambda e: e.tensor_tensor(out=o2, in0=tv[2], in1=tv[3], op=ALU.add), reads=[Rrt[2], Rrt[3]], writes=[Rout])

        wslab = [sb("wslab%d" % i, [128, 8, 512], BF16) for i in range(2)]
        Rwslab = [Res(), Res()]
        slab_ctr = [0]

        def load_slab(wsrc_b, c0, ncol):
            i = slab_ctr[0] % 2
            slab_ctr[0] += 1
            src = wsrc_b.rearrange("(c p) n -> p c n", p=128)[:, :, c0:c0 + ncol]
            P.dma("sp", lambda e: e.dma_start(out=wslab[i][:, :, 0:ncol], in_=src), reads=[Rw_b], writes=[Rwslab[i]])
            return wslab[i], Rwslab[i]

        def proj(nT, RnT, tok0, ws, Rws, ncol):
            pb, Rp = rot()
            for c in range(8):
                P.op("pe", lambda e, c=c: e.matmul(pb[:, 0:ncol], lhsT=nT[:, c, tok0:tok0 + 128], rhs=ws[:, c, 0:ncol], start=(c == 0), stop=(c == 7)),
                     reads=[RnT, Rws], writes=[Rp])
            return pb, Rp

        tst = [sb("tst%d" % i, [128, 512], BF16) for i in range(2)]
        Rtst = [Res(), Res()]
        tst_ctr = [0]

        def next_tst():
            i = tst_ctr[0] % 2
            tst_ctr[0] += 1
            return tst[i], Rtst[i]

        def transposes(src, Rsrc, ncols_each, nchunks, dst_fn, Rdst, eng="act"):
            pb, Rp = rot()
            pbb = pb[:].bitcast(BF16)
            for c in range(nchunks):
                P.op("pe", lambda e, c=c: e.transpose(out=pbb[0:ncols_each, c * 128:(c + 1) * 128], in_=src[:, c * ncols_each:(c + 1) * ncols_each], identity=identb[:]),
                     reads=[Rsrc, Rc], writes=[Rp])
            for c in range(nchunks):
                if eng == "act":
                    P.op("act", lambda e, c=c: e.copy(out=dst_fn(c), in_=pbb[0:ncols_each, c * 128:(c + 1) * 128]), reads=[Rp], writes=[Rdst])
                else:
                    P.op("dve", lambda e, c=c: e.tensor_copy(out=dst_fn(c), in_=pbb[0:ncols_each, c * 128:(c + 1) * 128]), reads=[Rp], writes=[Rdst])

        for e_ in ("sp", "act", "dve", "pe", "pool"):
            P.wait_tokens(e_, conv_tokens)

        xt = [sb("xt%d" % i, [128, D]) for i in range(2)]
        Rxt = [Res(), Res()]
        cst = [sb("cst%d" % i, [128, 64]) for i in range(4)]
        Rcst = [Res() for _ in range(4)]
        nTk = sb("nTk", [128, 8, 512], BF16)
        RnTk = Res()
        pcs = 0
        for (src, dst) in ((mb_d, mbt), (mb0_d, mb0t)):
            for pc in range(3):
                jx = pcs % 2; pcs += 1
                P.dma("sp", lambda e, src=src, pc=pc, jx=jx: e.dma_start(out=xt[jx][:], in_=src[:, pc * 1024:(pc + 1) * 1024]), writes=[Rxt[jx]])
                P.op("dve", lambda e, dst=dst, pc=pc, jx=jx: e.tensor_copy(out=dst[:, pc * 1024:(pc + 1) * 1024], in_=xt[jx][:]), reads=[Rxt[jx]], writes=[Rc])
        if stop == "m1":
            finish0(); return nc, P
        for mbk in range(2):
            P.dma("sp", lambda e, mbk=mbk: e.dma_start(out=xt[mbk][:], in_=memx[mbk * 128:(mbk + 1) * 128, :]), writes=[Rxt[mbk]])
            rms_T(xt[mbk][:], Rxt[mbk], 8, lambda c, mbk=mbk: nTk[:, c, mbk * 128:(mbk + 1) * 128], RnTk)
        if stop == "m2":
            finish0(); return nc, P
        Rkm = Res()
        for half in range(2):
            ws, Rws = load_slab(wmem_b, half * 512, 512)
            for mbk in range(2):
                pb, Rp = proj(nTk, RnTk, mbk * 128, ws, Rws, 512)
                if half == 0:
                    t_, Rt_ = next_tst()
                    P.op("act", lambda e, t_=t_, pb=pb: e.copy(out=t_[:], in_=pb[:]), reads=[Rp], writes=[Rt_])
                    transposes(t_, Rt_, 128, 4, lambda c, mbk=mbk: kmT[:, c, mbk * 128:(mbk + 1) * 128], Rkm)
                else:
                    P.op("act", lambda e, pb=pb, mbk=mbk: e.copy(out=vmb[:, mbk, :], in_=pb[:]), reads=[Rp], writes=[Rkm])

        def finish():
            toks = [("dma%d" % k, P.cnt["dma%d" % k]) for k in range(P.ndma) if P.cnt["dma%d" % k] > 0]
            toks += [(P.cur[e_][0], P.cnt[P.cur[e_][0]]) for e_ in ("pe", "act", "dve", "pool") if P.cnt[P.cur[e_][0]] > 0]
            P.wait_tokens("sp", toks)
            P.emit()

        if stop == "p0":
            finish(); return nc, P
        r1 = sb("r1", [128, 24576], BF16)
        KaTst = r1[:, 0:512]; kiTst = r1[0:64, 512:1024]
        Vast = r1[:, 1024:2048].rearrange("p (b n) -> p b n", b=4)
        bkTst = [r1[:, 2048 + i * 2048:2048 + (i + 1) * 2048].rearrange("p (c n) -> p c n", c=4) for i in range(2)]
        Vbst = [r1[:, 6144 + i * 4096:6144 + (i + 1) * 4096].rearrange("p (b n) -> p b n", b=4) for i in range(2)]
        RKaTst, RkiTst, RVast = Res(), Res(), Res()
        RbkTst = [Res(), Res()]; RVbst = [Res(), Res()]
        RKd = Res()
        P.op("pool", lambda e: e.memset(Vast, 1.0), writes=[RVast])
        for i in range(2):
            P.op("pool", lambda e, i=i: e.memset(Vbst[i], 1.0), writes=[RVbst[i]])

        for T in range(NBLK // 4):
            for bl in range(4):
                sbk = T * 4 + bl
                j = sbk % 2
                P.dma("sp", lambda e, sbk=sbk, j=j: e.dma_start(out=xt[j][:], in_=xs[sbk * 128:(sbk + 1) * 128, :]), writes=[Rxt[j]])
                P.dma("sp", lambda e, sbk=sbk, bl=bl: e.dma_start(out=cst[bl][:], in_=cs[sbk * 128:(sbk + 1) * 128, :]), writes=[Rcst[bl]])
                rms_T(xt[j][:], Rxt[j], 0, lambda c, bl=bl: nTk[:, c, bl * 128:(bl + 1) * 128], RnTk)
            for si, (nm, c0, ncol) in enumerate(K_SLABS):
                ws, Rws = load_slab(wk_b, c0, ncol)
                for bl in range(4):
                    pb, Rp = proj(nTk, RnTk, bl * 128, ws, Rws, ncol)
                    if nm == "a":
                        t_, Rt_ = next_tst()
                        rope(pb[:, 0:192], Rp, 3, cst[bl][:], Rcst[bl], t_[:, 0:192], Rt_)
                        Vv = Vast[:, bl, :].rearrange("p (g t d) -> p g t d", g=2, t=2)[:, :, 0, :]
                        P.op("act", lambda e, pb=pb, Vv=Vv: e.copy(out=Vv, in_=pb[:, 192:320].rearrange("p (g d) -> p g d", g=2)), reads=[Rp], writes=[RVast])
                        transposes(t_, Rt_, 128, 1, lambda c, bl=bl: KaTst[:, bl * 128:(bl + 1) * 128], RKaTst)
                        pb2, Rp2 = rot()
                        pbb2 = pb2[:].bitcast(BF16)
                        P.op("pe", lambda e, t_=t_, pbb2=pbb2: e.transpose(out=pbb2[0:64, 0:128], in_=t_[:, 128:192], identity=identb[:]), reads=[Rt_, Rc], writes=[Rp2])
                        P.op("act", lambda e, pbb2=pbb2, bl=bl: e.copy(out=kiTst[:, bl * 128:(bl + 1) * 128], in_=pbb2[0:64, 0:128]), reads=[Rp2], writes=[RkiTst])
                    elif nm.startswith("bk"):
                        g = int(nm[2]); jb = g % 2
                        t_, Rt_ = next_tst()
                        rope(pb[:, 0:512], Rp, 8, cst[bl][:], Rcst[bl], t_[:, 0:512], Rt_)
                        transposes(t_, Rt_, 128, 4, lambda c, bl=bl, jb=jb: bkTst[jb][:, c, bl * 128:(bl + 1) * 128], RbkTst[jb])
                    else:
                        g = int(nm[2]); jb = g % 2
                        Vv = Vbst[jb][:, bl, :].rearrange("p (h t d) -> p h t d", h=8, t=2)[:, :, 0, :]
                        P.op("act", lambda e, pb=pb, Vv=Vv: e.copy(out=Vv, in_=pb[:, 0:512].rearrange("p (h d) -> p h d", h=8)), reads=[Rp], writes=[RVbst[jb]])
                if nm == "a":
                    P.dma("pool", lambda e, T=T: e.dma_start(out=KaT_d[:, T * 512:(T + 1) * 512], in_=KaTst), reads=[RKaTst])
                    P.dma("pool", lambda e, T=T: e.dma_start(out=kiT_d[:, T * 512:(T + 1) * 512], in_=kiTst), reads=[RkiTst])
                    P.dma("pool", lambda e, T=T: e.dma_start(out=Va_d[T * 512:(T + 1) * 512, :].rearrange("(b p) n -> p b n", p=128), in_=Vast), reads=[RVast])
                elif nm.startswith("bk"):
                    g = int(nm[2]); jb = g % 2
                    P.dma("pool", lambda e, T=T, g=g, jb=jb: e.dma_start(out=bkT_d[:, g, :, T * 512:(T + 1) * 512], in_=bkTst[jb]), reads=[RbkTst[jb]])
                else:
                    g = int(nm[2]); jb = g % 2
                    P.dma("pool", lambda e, T=T, g=g, jb=jb: e.dma_start(out=Vb_d[T * 512:(T + 1) * 512, g, :].rearrange("(b p) n -> p b n", p=128), in_=Vbst[jb]), reads=[RVbst[jb]])
        kd_tokens = [("dma%d" % k, P.cnt["dma%d" % k]) for k in range(P.ndma) if P.cnt["dma%d" % k] > 0]
        P.wait_tokens("sp", kd_tokens)

        if stop == "p1":
            finish(); return nc, P
        xh = sb("xh", [128, QB, D]); Rxh = [Res() for _ in range(QB)]
        nTq = nTk[:, :, 0:N]; RnTq = RnTk
        csq = [sb("csq%d" % i, [128, 64]) for i in range(QB)]; Rcsq = [Res() for _ in range(QB)]
        QaT = [sb("QaT%d" % i, [128, 4, 128], BF16) for i in range(QB)]; RQaT = [Res() for _ in range(QB)]
        iqT = [sb("iqT%d" % i, [64, 4, 128], BF16) for i in range(QB)]; RiqT = [Res() for _ in range(QB)]
        bqT = [sb("bqT%d" % i, [128, 3, 4, 128], BF16) for i in range(QB)]; RbqT = [Res() for _ in range(QB)]
        mqT = [sb("mqT%d" % i, [128, 4, 128], BF16) for i in range(QB)]; RmqT = [Res() for _ in range(QB)]
        wv = [sb("wv%d" % i, [128, 12]) for i in range(QB)]; Rwv = [Res() for _ in range(QB)]
        dg = [sb("dg%d" % i, [128, 4, 128], BF16) for i in range(QB)]; Rdg = [Res() for _ in range(QB)]
        yaT = sb("yaT", [64, 8, N], BF16); ybT = sb("ybT", [64, 8, N], BF16); ymT = sb("ymT", [128, 4, N], BF16)
        RyaT, RybT, RymT = Res(), Res(), Res()
        score = r1[:, 0:16384].bitcast(F32)
        junk = r1[:, 16384:24576]
        Rsc, Rjk = Res(), Res()
        Rmw = [Res(), Res()]
        kib = [sb("kib%d" % i, [64, 512], BF16) for i in range(2)]; Rkib = [Res(), Res()]
        Rh = [sb("Rh%d" % i, [128, 512], BF16) for i in range(4)]; RRh = [Res() for _ in range(4)]
        bis = sb("bis", [128, 8 + NBIS + 1]); Rbis = Res()
        nmb = [sb("nmb%d" % i, [128, 512], BF16) for i in range(2)]; Rnmb = [Res(), Res()]
        kab = [sb("kab%d" % i, [128, 512], BF16) for i in range(2)]; Rkab = [Res(), Res()]
        vab = [sb("vab%d" % i, [128, 4, 256], BF16) for i in range(2)]; Rvab = [Res(), Res()]
        pt = [sb("pt%d" % i, [128, 512], BF16) for i in range(4)]; Rpt = [Res() for _ in range(4)]
        pt_ctr = [0]
        rden = sb("rden", [128, 512]); Rrden = Res()
        NBB = 3
        bkb = [sb("bkb%d" % i, [128, 4, 128], BF16) for i in range(NBB)]; Rbkb = [Res() for _ in range(NBB)]
        vbb = [sb("vbb%d" % i, [128, 1024], BF16) for i in range(NBB)]; Rvbb = [Res() for _ in range(NBB)]
        bb_ctr = [0]
        gt = [sb("gt%d" % i, [128, N]) for i in range(3)]; Rgt = [Res() for _ in range(3)]
        wgs = [sb("wgs0", [128, 8, 384], BF16)] * 2; Rwgs = [Res()] * 2
        wba = [sb("wba0", [64, 16, 128], BF16)] * 2; Rwba = [Res()] * 2
        wbm = [sb("wbm0", [128, 4, 128], BF16)] * 2; Rwbm = [Res()] * 2
        mtmp = [sb("mtmp%d" % i, [128, N]) for i in range(3)]; Rmtmp = [Res() for _ in range(3)]
        mT = sb("mT", [128, 8, N], BF16); RmT = Res()
        xnT = mT; RxnT = RmT
        xf32 = xt[0]; Rxf32 = Rxt[0]
        xnT32 = xt[1][:].rearrange("p (c n) -> p c n", c=8); RxnT32 = Rxt[1]
        rl = sb("rl", [128, 64]); Rrl = Res()
        comb = [sb("comb%d" % i, [128, 16]) for i in range(QB)]; Rcomb = [Res() for _ in range(QB)]
        sA = [sb("sA%d" % i, [128, N], BF16) for i in range(2)]; RsA = [Res(), Res()]
        hT = [sb("hT%d" % i, [128, 4, N], BF16) for i in range(2)]; RhT = [Res(), Res()]
        ot = xf32; Rot = Rxf32
        out_tokens = []

        def next_pt():
            i = pt_ctr[0] % 4
            pt_ctr[0] += 1
            return pt[i], Rpt[i]

        def dsa(t, sq):
            nkb = sq + 1
            nk = nkb * 128
            nch = (nk + 511) // 512
            aw = wv[t][:, 4:8]
            for ch in range(nch):
                ncol = min(512, nk - ch * 512)
                kb_ = kib[ch % 2]
                P.dma("sp", lambda e, ch=ch, ncol=ncol, kb_=kb_: e.dma_start(out=kb_[:, 0:ncol], in_=kiT_d[:, ch * 512:ch * 512 + ncol]), reads=[RKd], writes=[Rkib[ch % 2]])
                for h in range(4):
                    pb, Rp = rot()
                    P.op("pe", lambda e, pb=pb, h=h, ncol=ncol, kb_=kb_: e.matmul(pb[:, 0:ncol], lhsT=iqT[t][:, h, :], rhs=kb_[:, 0:ncol], start=True, stop=True),
                         reads=[RiqT[t], Rkib[ch % 2]], writes=[Rp])
                    P.op("act", lambda e, pb=pb, h=h, ncol=ncol: e.activation(out=Rh[h][:, 0:ncol], in_=pb[:, 0:ncol], func=AF.Relu, scale=aw[:, h:h + 1]),
                         reads=[Rp, Rwv[t]], writes=[RRh[h]])
                pb, Rp = rot()
                for h in range(4):
                    P.op("pe", lambda e, pb=pb, h=h, ncol=ncol: e.matmul(pb[:, 0:ncol], lhsT=dg[t][:, h, :], rhs=Rh[h][:, 0:ncol], start=(h == 0), stop=(h == 3)),
                         reads=[Rdg[t], RRh[h]], writes=[Rp])
                P.op("dve", lambda e, pb=pb, ch=ch, ncol=ncol: e.tensor_copy(out=score[:, ch * 512:ch * 512 + ncol], in_=pb[:, 0:ncol]),
                     reads=[Rp], writes=[Rsc, Rmw[0], Rmw[1]])
            am = bis[:, 0:1]; w0 = bis[:, 1:2]; mid = bis[:, 2:3]; cnt = bis[:, 3:4]; sg = bis[:, 4:5]; thr = bis[:, 5:6]
            wt2 = bis[:, 8:8 + NBIS + 1]
            P.op("dve", lambda e: e.tensor_reduce(out=am, in_=score[:, 0:nk], axis=AX, op=ALU.max, apply_absolute_value=True), reads=[Rsc], writes=[Rbis])
            P.op("dve", lambda e: e.tensor_tensor(out=score[:, nk - 256:nk], in0=score[:, nk - 256:nk], in1=cm[:], op=ALU.add), reads=[Rsc, Rc, Rbis], writes=[Rsc])
            P.op("dve", lambda e: e.tensor_tensor(out=score[:, 0:128], in0=score[:, 0:128], in1=m0[:], op=ALU.add), reads=[Rsc, Rc], writes=[Rsc])
            P.op("dve", lambda e: e.tensor_scalar(out=w0, in0=am, scalar1=1.001, scalar2=1e-6, op0=ALU.mult, op1=ALU.add), reads=[Rbis], writes=[Rbis])
            P.op("dve", lambda e: e.tensor_scalar(out=wt2, in0=pow2[:], scalar1=w0, scalar2=None, op0=ALU.mult), reads=[Rbis, Rc], writes=[Rbis])
            P.op("dve", lambda e: e.memset(mid, 0.0), writes=[Rbis])
            for it in range(NBIS):
                P.op("dve", lambda e: e.tensor_scalar(out=junk[:, 0:nk], in0=score[:, 0:nk], scalar1=mid, scalar2=None, op0=ALU.is_ge, op1=ALU.add, accum_out=cnt),
                     reads=[Rsc, Rbis], writes=[Rjk, Rbis, Rmw[1]])
                P.op("dve", lambda e: e.tensor_scalar(out=sg, in0=cnt, scalar1=255.5, scalar2=0.5, op0=ALU.is_ge, op1=ALU.subtract), reads=[Rbis], writes=[Rbis])
                P.op("dve", lambda e, it=it: e.scalar_tensor_tensor(out=mid, in0=sg, scalar=wt2[:, it:it + 1], in1=mid, op0=ALU.mult, op1=ALU.add), reads=[Rbis], writes=[Rbis])
            P.op("dve", lambda e: e.tensor_tensor(out=thr, in0=mid, in1=wt2[:, NBIS:NBIS + 1], op=ALU.subtract), reads=[Rbis], writes=[Rbis])
            for k4 in range(nch):
                ncol = min(512, nk - k4 * 512)
                nb_here = ncol // 128
                j = k4 % 2
                P.op("dve", lambda e, k4=k4, ncol=ncol, j=j: e.tensor_scalar(out=nmb[j][:, 0:ncol], in0=score[:, k4 * 512:k4 * 512 + ncol], scalar1=thr, scalar2=NEG, op0=ALU.is_lt, op1=ALU.mult),
                     reads=[Rsc, Rbis], writes=[Rnmb[j]])
                P.dma("sp", lambda e, k4=k4, ncol=ncol, j=j: e.dma_start(out=kab[j][:, 0:ncol], in_=KaT_d[:, k4 * 512:k4 * 512 + ncol]), reads=[RKd], writes=[Rkab[j]])
                P.dma("sp", lambda e, k4=k4, nb_here=nb_here, j=j: e.dma_start(out=vab[j][:, 0:nb_here, :], in_=Va_d[k4 * 512:k4 * 512 + nb_here * 128, :].rearrange("(b p) n -> p b n", p=128)), reads=[RKd], writes=[Rvab[j]])
                for b in range(nb_here):
                    kb = k4 * 4 + b
                    for g in range(2):
                        pb, Rp = rot()
                        P.op("pe", lambda e, pb=pb, g=g, b=b, j=j: e.matmul(pb[:], lhsT=kab[j][g * 64:(g + 1) * 64, b * 128:(b + 1) * 128], rhs=QaT[t][g * 64:(g + 1) * 64, :, :].rearrange("p j q -> p (j q)"), start=True, stop=False),
                             reads=[Rkab[j], RQaT[t]], writes=[Rp])
                        P.op("pe", lambda e, pb=pb, b=b, j=j: e.matmul(pb[:], lhsT=nmb[j][:, b * 128:(b + 1) * 128], rhs=i4b[:], start=False, stop=True),
                             reads=[Rnmb[j], Rc], writes=[Rp])
                        p_, Rp_ = next_pt()
                        P.op("act", lambda e, pb=pb, p_=p_: e.activation(out=p_[:], in_=pb[:], func=AF.Exp, scale=0.125), reads=[Rp], writes=[Rp_])
                        P.op("pe", lambda e, g=g, b=b, j=j, p_=p_, kb=kb: e.matmul(pbank[g][:], lhsT=vab[j][:, b, g * 128:(g + 1) * 128], rhs=p_[:], start=(kb == 0), stop=(kb == nkb - 1)),
                             reads=[Rvab[j], Rp_], writes=[Rb[g]])
            for g in range(2):
                P.op("dve", lambda e, g=g: e.reciprocal(out=rden[0:64, :], in_=pbank[g][64:128, :]), reads=[Rb[g]], writes=[Rrden])
                P.op("dve", lambda e, g=g: e.tensor_tensor(out=yaT[:, g * 4:(g + 1) * 4, t * 128:(t + 1) * 128], in0=pbank[g][0:64, :].rearrange("p (j q) -> p j q", j=4), in1=rden[0:64, :].rearrange("p (j q) -> p j q", j=4), op=ALU.mult),
                     reads=[Rb[g], Rrden], writes=[RyaT])

        def bmix(t, sq):
            first = [True, True]
            work = []
            for g in range(3):
                for jj in range(B_NB[g], -1, -1):
                    kb = sq - jj
                    if kb >= 0:
                        work.append((g, jj, kb))
            last_idx = len(work) - 1
            for wi, (g, jj, kb) in enumerate(work):
                i = bb_ctr[0] % NBB
                bb_ctr[0] += 1
                P.dma("sp", lambda e, g=g, kb=kb, i=i: e.dma_start(out=bkb[i][:], in_=bkT_d[:, g, :, kb * 128:(kb + 1) * 128]), reads=[RKd], writes=[Rbkb[i]])
                P.dma("sp", lambda e, g=g, kb=kb, i=i: e.dma_start(out=vbb[i][:], in_=Vb_d[kb * 128:(kb + 1) * 128, g, :]), reads=[RKd], writes=[Rvbb[i]])
                mtab = mb0t if kb == 0 else mbt
                mi = B_MOFF[g] + jj
                if BM_CUT <= 1:
                    continue
                for hh in range(2):
                    pb, Rp = rot()
                    P.op("pe", lambda e, pb=pb, mtab=mtab, mi=mi: e.matmul(pb[:], lhsT=mtab[:, mi * 128:(mi + 1) * 128], rhs=i4b[:], start=True, stop=False),
                         reads=[Rc], writes=[Rp])
                    for h4 in range(4):
                        p2 = h4; hf = hh
                        P.op("pe", lambda e, pb=pb, h4=h4, p2=p2, hf=hf, i=i, g=g: e.matmul(pb[:, h4 * 128:(h4 + 1) * 128], lhsT=bkb[i][hf * 64:(hf + 1) * 64, p2, :], rhs=bqT[t][hf * 64:(hf + 1) * 64, g, p2, :], start=False, stop=(h4 == 3), skip_group_check=True),
                             reads=[Rbkb[i], RbqT[t]], writes=[Rp])
                    p_, Rp_ = next_pt()
                    P.op("act", lambda e, pb=pb, p_=p_: e.activation(out=p_[:], in_=pb[:], func=AF.Exp, scale=0.125), reads=[Rp], writes=[Rp_])
                    if BM_CUT <= 2:
                        continue
                    for h4 in range(4):
                        h = 2 * h4 + hh
                        st = first[hh]
                        first[hh] = False
                        P.op("pe", lambda e, hh=hh, h4=h4, h=h, i=i, p_=p_, st=st, wi=wi: e.matmul(pbank[2 + hh][:, h4 * 128:(h4 + 1) * 128], lhsT=vbb[i][:, h * 128:(h + 1) * 128], rhs=p_[:, h4 * 128:(h4 + 1) * 128], start=st, stop=(wi == last_idx and h4 == 3), skip_group_check=True),
                             reads=[Rvbb[i], Rp_], writes=[Rb[2 + hh]])
            if BM_CUT <= 3:
                return
            for hh in range(2):
                P.op("dve", lambda e, hh=hh: e.reciprocal(out=rden[0:64, :], in_=pbank[2 + hh][64:128, :]), reads=[Rb[2 + hh]], writes=[Rrden])
                P.op("dve", lambda e, hh=hh: e.tensor_tensor(out=ybT[:, :, t * 128:(t + 1) * 128].rearrange("p (p2 hf) q -> p hf p2 q", hf=2)[:, hh], in0=pbank[2 + hh][0:64, :].rearrange("p (j q) -> p j q", j=4), in1=rden[0:64, :].rearrange("p (j q) -> p j q", j=4), op=ALU.mult),
                     reads=[Rb[2 + hh], Rrden], writes=[RybT])

        def memattn(t):
            sc = 128.0 ** -0.5
            for mbk in range(2):
                pb, Rp = rot()
                for h in range(4):
                    P.op("pe", lambda e, pb=pb, h=h, mbk=mbk: e.matmul(pb[:, h * 128:(h + 1) * 128], lhsT=kmT[:, h, mbk * 128:(mbk + 1) * 128], rhs=mqT[t][:, h, :], start=(h == 0), stop=(h == 3), skip_group_check=True),
                         reads=[Rkm, RmqT[t]], writes=[Rp])
                p_, Rp_ = next_pt()
                P.op("act", lambda e, pb=pb, p_=p_: e.activation(out=p_[:], in_=pb[:], func=AF.Exp, scale=sc), reads=[Rp], writes=[Rp_])
                for h in range(4):
                    P.op("pe", lambda e, h=h, mbk=mbk, p_=p_: e.matmul(pbank[0][:, h * 128:(h + 1) * 128], lhsT=vmb[:, mbk, h * 128:(h + 1) * 128], rhs=p_[:, h * 128:(h + 1) * 128], start=(mbk == 0 and h == 0), stop=(mbk == 1 and h == 3), skip_group_check=True),
                         reads=[Rkm, Rp_], writes=[Rb[0]])
                P.op("pe", lambda e, mbk=mbk, p_=p_: e.matmul(pbank[1][:], lhsT=ones_b[:], rhs=p_[:], start=(mbk == 0), stop=(mbk == 1)),
                     reads=[Rc, Rp_], writes=[Rb[1]])
            P.op("dve", lambda e: e.reciprocal(out=rden[:], in_=pbank[1][:]), reads=[Rb[1]], writes=[Rrden])
            P.op("dve", lambda e: e.tensor_tensor(out=ymT[:, :, t * 128:(t + 1) * 128], in0=pbank[0][:].rearrange("p (j q) -> p j q", j=4), in1=rden[:].rearrange("p (j q) -> p j q", j=4), op=ALU.mult),
                 reads=[Rb[0], Rrden], writes=[RymT])

        for I in range(NIT):
            for t in range(QB):
                sq = 2 * (I * QB + t) + 1
                P.dma("sp", lambda e, t=t, sq=sq: e.dma_start(out=xh[:, t, :], in_=xs[sq * 128:(sq + 1) * 128, :]), writes=[Rxh[t]])
                P.dma("sp", lambda e, t=t, sq=sq: e.dma_start(out=csq[t][:], in_=cs[sq * 128:(sq + 1) * 128, :]), writes=[Rcsq[t]])
                rms_T(xh[:, t, :], Rxh[t], 0, lambda c, t=t: nTq[:, c, t * 128:(t + 1) * 128], RnTq)
            for (nm, c0, ncol) in Q_SLABS:
                ws, Rws = load_slab(wq_b, c0, ncol)
                for t in range(QB):
                    pb, Rp = proj(nTq, RnTq, t * 128, ws, Rws, ncol)
                    t_, Rt_ = next_tst()
                    if nm == "aq":
                        rope(pb[:, 0:512], Rp, 8, csq[t][:], Rcsq[t], t_[:, 0:512], Rt_, perm=True)
                        transposes(t_, Rt_, 128, 4, lambda c, t=t: QaT[t][:, c, :], RQaT[t])
                    elif nm == "iq":
                        rope(pb[:, 0:256], Rp, 4, csq[t][:], Rcsq[t], t_[:, 0:256], Rt_)
                        transposes(t_, Rt_, 64, 4, lambda c, t=t: iqT[t][:, c, :], RiqT[t])
                        w_ = wv[t]
                        P.op("dve", lambda e, pb=pb, w_=w_: e.tensor_copy(out=w_[:, 0:4], in_=pb[:, 256:260]), reads=[Rp], writes=[Rwv[t]])
                        P.op("dve", lambda e, w_=w_: e.tensor_scalar(out=w_[:, 8:12], in0=w_[:, 0:4], scalar1=0.0, scalar2=2.0, op0=ALU.is_ge, op1=ALU.mult), reads=[Rwv[t]], writes=[Rwv[t]])
                        P.op("dve", lambda e, w_=w_: e.tensor_scalar(out=w_[:, 8:12], in0=w_[:, 8:12], scalar1=-1.0, scalar2=None, op0=ALU.add), reads=[Rwv[t]], writes=[Rwv[t]])
                        P.op("dve", lambda e, w_=w_: e.scalar_tensor_tensor(out=w_[:, 4:8], in0=w_[:, 0:4], scalar=0.0625, in1=w_[:, 8:12], op0=ALU.mult, op1=ALU.mult), reads=[Rwv[t]], writes=[Rwv[t]])
                        for h in range(4):
                            P.op("dve", lambda e, w_=w_, h=h, t=t: e.tensor_scalar(out=dg[t][:, h, :], in0=identf[:], scalar1=w_[:, 8 + h:9 + h], scalar2=None, op0=ALU.mult), reads=[Rwv[t], Rc], writes=[Rdg[t]])
                    elif nm.startswith("bq"):
                        g = int(nm[2])
                        rope(pb[:, 0:512], Rp, 8, csq[t][:], Rcsq[t], t_[:, 0:512], Rt_)
                        transposes(t_, Rt_, 128, 4, lambda c, t=t, g=g: bqT[t][:, g, c, :], RbqT[t])
                    else:
                        P.op("act", lambda e, pb=pb, t_=t_: e.copy(out=t_[:], in_=pb[:]), reads=[Rp], writes=[Rt_])
                        transposes(t_, Rt_, 128, 4, lambda c, t=t: mqT[t][:, c, :], RmqT[t])
            if stop == "q":
                finish(); return nc, P
            for t in range(QB):
                sq = 2 * (I * QB + t) + 1
                dsa(t, sq)
                if stop == "dsa":
                    finish(); return nc, P
                bmix(t, sq)
                if stop == "bmix":
                    finish(); return nc, P
                memattn(t)
                if stop == "mem":
                    finish(); return nc, P
            if dbg and I == NIT - 1:
                for (nm, src, R_) in (("d_ya", yaT, RyaT), ("d_yb", ybT, RybT), ("d_ym", ymT, RymT)):
                    np_ = src.shape[0]
                    P.op("dve", lambda e, src=src, np_=np_: e.tensor_copy(out=score[0:np_, 0:src.shape[1] * N], in_=src[:].rearrange("p a n -> p (a n)")), reads=[R_], writes=[Rsc, Rmw[0], Rmw[1]])
                    out_tokens.append(P.dma("sp", lambda e, nm=nm, np_=np_, src=src: e.dma_start(out=dbg_d[nm], in_=score[0:np_, 0:src.shape[1] * N]), reads=[Rsc]))
            for f in range(8):
                j = f % 2
                P.dma("sp", lambda e, f=f, j=j: e.dma_start(out=wgs[j][:], in_=wg_b.rearrange("(c p) n -> p c n", p=128)[:, :, f * 384:(f + 1) * 384]), reads=[Rw_b], writes=[Rwgs[j]])
                P.dma("sp", lambda e, f=f, j=j: e.dma_start(out=wba[j][:], in_=wbr_b[0:1024, f * 128:(f + 1) * 128].rearrange("(h p) n -> p h n", p=64)), reads=[Rw_b], writes=[Rwba[j]])
                P.dma("sp", lambda e, f=f, j=j: e.dma_start(out=wbm[j][:], in_=wbr_b[1024:1536, f * 128:(f + 1) * 128].rearrange("(h p) n -> p h n", p=128)), reads=[Rw_b], writes=[Rwbm[j]])
                for r in range(3):
                    pb, Rp = rot()
                    for c in range(8):
                        P.op("pe", lambda e, pb=pb, c=c, r=r, j=j: e.matmul(pb[:, 0:N], lhsT=wgs[j][:, c, r * 128:(r + 1) * 128], rhs=nTq[:, c, :], start=(c == 0), stop=(c == 7)),
                             reads=[Rwgs[j], RnTq], writes=[Rp])
                    P.op("act", lambda e, pb=pb, r=r, f=f: e.activation(out=gt[r][:], in_=pb[:, 0:N], func=AF.Sigmoid, bias=bgate[:, f * 3 + r:f * 3 + r + 1], scale=1.0),
                         reads=[Rp, Rc], writes=[Rgt[r]])
                for r in range(3):
                    pb, Rp = rot()
                    if r < 2:
                        ysrc, Ry = (yaT, RyaT) if r == 0 else (ybT, RybT)
                        for h in range(8):
                            P.op("pe", lambda e, pb=pb, h=h, r=r, j=j, ysrc=ysrc: e.matmul(pb[:, 0:N], lhsT=wba[j][:, r * 8 + h, :], rhs=ysrc[:, h, :], start=(h == 0), stop=(h == 7)),
                                 reads=[Rwba[j], Ry], writes=[Rp])
                    else:
                        for h in range(4):
                            P.op("pe", lambda e, pb=pb, h=h, j=j: e.matmul(pb[:, 0:N], lhsT=wbm[j][:, h, :], rhs=ymT[:, h, :], start=(h == 0), stop=(h == 3)),
                                 reads=[Rwbm[j], RymT], writes=[Rp])
                    P.op("dve", lambda e, pb=pb, r=r: e.tensor_tensor(out=mtmp[r][:], in0=pb[:, 0:N], in1=gt[r][:], op=ALU.mult), reads=[Rp, Rgt[r]], writes=[Rmtmp[r]])
                P.op("pool", lambda e: e.tensor_tensor(out=mtmp[0][:], in0=mtmp[0][:], in1=mtmp[1][:], op=ALU.add), reads=[Rmtmp[0], Rmtmp[1]], writes=[Rmtmp[0]])
                P.op("pool", lambda e, f=f: e.tensor_tensor(out=mT[:, f, :], in0=mtmp[0][:], in1=mtmp[2][:], op=ALU.add), reads=[Rmtmp[0], Rmtmp[2]], writes=[RmT])
            for n2 in range(2):
                ws, Rws = load_slab(wo_b, n2 * 512, 512)
                for t in range(QB):
                    pb, Rp = proj(mT, RmT, t * 128, ws, Rws, 512)
                    P.op("dve", lambda e, pb=pb, t=t, n2=n2: e.tensor_tensor(out=xh[:, t, n2 * 512:(n2 + 1) * 512], in0=pb[:], in1=xh[:, t, n2 * 512:(n2 + 1) * 512], op=ALU.add),
                         reads=[Rp, Rxh[t]], writes=[Rxh[t]])
            if dbg and I == NIT - 1:
                out_tokens.append(P.dma("sp", lambda e: e.dma_start(out=dbg_d["d_h"], in_=xh[:].rearrange("p t n -> p (t n)")), reads=Rxh))
            if stop == "d":
                finish(); return nc, P
            for t in range(QB):
                si = rms_T(xh[:, t, :], Rxh[t], 16, lambda c, t=t: xnT[:, c, t * 128:(t + 1) * 128], RxnT)
                rs = rs_t[:, si:si + 1]
                P.op("dve", lambda e, t=t, rs=rs: e.tensor_scalar(out=xf32[:], in0=xh[:, t, :], scalar1=rs, scalar2=None, op0=ALU.mult), reads=[Rxh[t], Rstat[si]], writes=[Rxf32])
                for half in range(2):
                    pb, Rp = rot()
                    for c4 in range(4):
                        c = half * 4 + c4
                        P.op("pe", lambda e, pb=pb, c=c, c4=c4: e.transpose(out=pb[:, c4 * 128:(c4 + 1) * 128], in_=xf32[:, c * 128:(c + 1) * 128], identity=identf[:]), reads=[Rxf32, Rc], writes=[Rp])
                    for c4 in range(4):
                        c = half * 4 + c4
                        P.op("dve", lambda e, pb=pb, c=c, c4=c4: e.tensor_scalar(out=xnT32[:, c, :], in0=pb[:, c4 * 128:(c4 + 1) * 128], scalar1=gcols[:, 16 + c:17 + c], scalar2=None, op0=ALU.mult), reads=[Rp, Rc], writes=[RxnT32])
                pb, Rp = rot()
                for c in range(8):
                    P.op("pe", lambda e, pb=pb, c=c: e.matmul(pb[:, 0:20], lhsT=xnT32[:, c, :], rhs=wr32[:, c, :], start=(c == 0), stop=(c == 7)), reads=[RxnT32, Rc], writes=[Rp])
                lg = rl[:, 0:20]; gmx = rl[:, 20:21]; ngmx = rl[:, 21:22]; gex = rl[:, 22:26]; gsum = rl[:, 26:27]; gw = rl[:, 27:28]
                ohg = rl[:, 28:32]; sel = rl[:, 32:36]; m1 = rl[:, 36:37]; oh1 = rl[:, 37:41]; sel2 = rl[:, 41:45]; m2 = rl[:, 45:46]
                oh2 = rl[:, 46:50]; ee = rl[:, 50:51]; p1 = rl[:, 51:52]; p2 = rl[:, 52:53]; cw = rl[:, 53:57]; nm1 = rl[:, 57:58]; cw2 = rl[:, 58:62]
                RW = dict(reads=[Rrl], writes=[Rrl])
                P.op("dve", lambda e, pb=pb: e.tensor_tensor(out=lg, in0=pb[:, 0:20], in1=rbias[:], op=ALU.add), reads=[Rp, Rc], writes=[Rrl])
                P.op("dve", lambda e: e.tensor_reduce(out=gmx, in_=lg[:, 0:4], axis=AX, op=ALU.max), **RW)
                P.op("dve", lambda e: e.tensor_scalar(out=ngmx, in0=gmx, scalar1=-1.0, scalar2=None, op0=ALU.mult), **RW)
                P.op("act", lambda e: e.activation(out=gex, in_=lg[:, 0:4], func=AF.Exp, bias=ngmx, scale=1.0, accum_out=gsum), **RW)
                P.op("dve", lambda e: e.reciprocal(out=gw, in_=gsum), **RW)
                P.op("dve", lambda e: e.tensor_scalar(out=ohg, in0=lg[:, 0:4], scalar1=gmx, scalar2=None, op0=ALU.is_equal), **RW)
                P.op("dve", lambda e: e.tensor_scalar(out=sel, in0=lg[:, 4:8], scalar1=ohg[:, 0:1], scalar2=None, op0=ALU.mult), **RW)
                for g in range(1, 4):
                    P.op("dve", lambda e, g=g: e.scalar_tensor_tensor(out=sel, in0=lg[:, 4 + 4 * g:8 + 4 * g], scalar=ohg[:, g:g + 1], in1=sel, op0=ALU.mult, op1=ALU.add), **RW)
                P.op("dve", lambda e: e.tensor_reduce(out=m1, in_=sel, axis=AX, op=ALU.max), **RW)
                P.op("dve", lambda e: e.tensor_scalar(out=oh1, in0=sel, scalar1=m1, scalar2=None, op0=ALU.is_equal), **RW)
                P.op("dve", lambda e: e.scalar_tensor_tensor(out=sel2, in0=oh1, scalar=NINF, in1=sel, op0=ALU.mult, op1=ALU.add), **RW)
                P.op("dve", lambda e: e.tensor_reduce(out=m2, in_=sel2, axis=AX, op=ALU.max), **RW)
                P.op("dve", lambda e: e.tensor_scalar(out=oh2, in0=sel2, scalar1=m2, scalar2=None, op0=ALU.is_equal), **RW)
                P.op("dve", lambda e: e.tensor_scalar(out=nm1, in0=m1, scalar1=-1.0, scalar2=None, op0=ALU.mult), **RW)
                P.op("act", lambda e: e.activation(out=ee, in_=m2, func=AF.Exp, bias=nm1, scale=1.0), **RW)
                P.op("dve", lambda e: e.tensor_scalar(out=p1, in0=ee, scalar1=1.0, scalar2=None, op0=ALU.add), **RW)
                P.op("dve", lambda e: e.reciprocal(out=p1, in_=p1), **RW)
                P.op("dve", lambda e: e.tensor_tensor(out=p2, in0=ee, in1=p1, op=ALU.mult), **RW)
                P.op("dve", lambda e: e.tensor_scalar(out=cw, in0=oh1, scalar1=p1, scalar2=None, op0=ALU.mult), **RW)
                P.op("dve", lambda e: e.scalar_tensor_tensor(out=cw2, in0=oh2, scalar=p2, in1=cw, op0=ALU.mult, op1=ALU.add), **RW)
                P.op("dve", lambda e: e.tensor_scalar(out=cw, in0=cw2, scalar1=gw, scalar2=None, op0=ALU.mult), **RW)
                for g in range(4):
                    P.op("dve", lambda e, g=g, t=t: e.tensor_scalar(out=comb[t][:, g * 4:(g + 1) * 4], in0=cw, scalar1=ohg[:, g:g + 1], scalar2=None, op0=ALU.mult), reads=[Rrl], writes=[Rcomb[t]])
            for ex in range(16):
                j = ex % 2
                base = j * 12288
                w1e = r1[:, base:base + 4096].rearrange("p (c n) -> p c n", c=8)
                w3e = r1[:, base + 4096:base + 8192].rearrange("p (c n) -> p c n", c=8)
                w2e = r1[:, base + 8192:base + 12288].rearrange("p (c n) -> p c n", c=4)
                wr_ = [Rmw[j], Rsc, Rjk]
                P.dma("sp", lambda e, ex=ex, w1e=w1e: e.dma_start(out=w1e, in_=w1_b[ex * D:(ex + 1) * D, :].rearrange("(c p) n -> p c n", p=128)), reads=[Rw_b], writes=wr_)
                P.dma("sp", lambda e, ex=ex, w3e=w3e: e.dma_start(out=w3e, in_=w3_b[ex * D:(ex + 1) * D, :].rearrange("(c p) n -> p c n", p=128)), reads=[Rw_b], writes=wr_)
                P.dma("sp", lambda e, ex=ex, w2e=w2e: e.dma_start(out=w2e, in_=w2_b[ex * 512:(ex + 1) * 512, :].rearrange("(c p) n -> p c n", p=128)), reads=[Rw_b], writes=wr_)
                for c in range(4):
                    pa, Rpa = rot()
                    for k in range(8):
                        P.op("pe", lambda e, pa=pa, k=k, c=c, w1e=w1e: e.matmul(pa[:, 0:N], lhsT=w1e[:, k, c * 128:(c + 1) * 128], rhs=xnT[:, k, :], start=(k == 0), stop=(k == 7)), reads=[Rmw[j], RxnT], writes=[Rpa])
                    pb, Rp = rot()
                    for k in range(8):
                        P.op("pe", lambda e, pb=pb, k=k, c=c, w3e=w3e: e.matmul(pb[:, 0:N], lhsT=w3e[:, k, c * 128:(c + 1) * 128], rhs=xnT[:, k, :], start=(k == 0), stop=(k == 7)), reads=[Rmw[j], RxnT], writes=[Rp])
                    sj = c % 2
                    P.op("act", lambda e, pa=pa, sj=sj: e.activation(out=sA[sj][:], in_=pa[:, 0:N], func=AF.Silu), reads=[Rpa], writes=[RsA[sj]])
                    P.op("dve", lambda e, pb=pb, sj=sj, c=c, j=j: e.tensor_tensor(out=hT[j][:, c, :], in0=pb[:, 0:N], in1=sA[sj][:], op=ALU.mult), reads=[Rp, RsA[sj]], writes=[RhT[j]])
                for t in range(QB):
                    for n2 in range(2):
                        pb, Rp = rot()
                        for c in range(4):
                            P.op("pe", lambda e, pb=pb, c=c, t=t, n2=n2, j=j, w2e=w2e: e.matmul(pb[:], lhsT=hT[j][:, c, t * 128:(t + 1) * 128], rhs=w2e[:, c, n2 * 512:(n2 + 1) * 512], start=(c == 0), stop=(c == 3)), reads=[RhT[j], Rmw[j]], writes=[Rp])
                        P.op("dve", lambda e, pb=pb, t=t, n2=n2, ex=ex: e.scalar_tensor_tensor(out=xh[:, t, n2 * 512:(n2 + 1) * 512], in0=pb[:], scalar=comb[t][:, ex:ex + 1], in1=xh[:, t, n2 * 512:(n2 + 1) * 512], op0=ALU.mult, op1=ALU.add),
                             reads=[Rp, Rcomb[t], Rxh[t]], writes=[Rxh[t]])
            for t in range(QB):
                qi = I * QB + t
                ss = ss_t[:, 0:1]; rs = rs_t[:, 0:1]
                P.op("act", lambda e, t=t, ss=ss: e.activation(out=sqj[:], in_=xh[:, t, :], func=AF.Square, accum_out=ss), reads=[Rxh[t]], writes=[Rsqj, Rstat[0]])
                P.op("dve", lambda e, ss=ss, rs=rs: e.tensor_scalar(out=rs, in0=ss, scalar1=1.0 / D, scalar2=1e-6, op0=ALU.mult, op1=ALU.add), reads=[Rstat[0]], writes=[Rstat[0]])
                P.op("act", lambda e, rs=rs: e.activation(out=rs, in_=rs, func=AF.Sqrt), reads=[Rstat[0]], writes=[Rstat[0]])
                P.op("dve", lambda e, rs=rs: e.reciprocal(out=rs, in_=rs), reads=[Rstat[0]], writes=[Rstat[0]])
                P.op("dve", lambda e, t=t, rs=rs: e.scalar_tensor_tensor(out=ot[:], in0=xh[:, t, :], scalar=rs, in1=gfin[:], op0=ALU.mult, op1=ALU.mult), reads=[Rxh[t], Rstat[0], Rc], writes=[Rot])
                out_tokens.append(P.dma("sp", lambda e, qi=qi: e.dma_start(out=out_d[qi * 128:(qi + 1) * 128, :], in_=ot[:]), reads=[Rot]))
        P.wait_tokens("sp", out_tokens)
        P.emit()
    return nc, P


def _host_consts(S, parity):
    pos = np.arange(S, dtype=np.float32) - (0 if parity else 128)
    half = 32
    inv = (10000.0 ** (-np.arange(half, dtype=np.float32) / half)).astype(np.float32)
    ang = pos[:, None] * inv[None, :]
    cs = np.concatenate([np.cos(ang), np.sin(ang)], axis=1).astype(np.float32)
    q = np.arange(128)[:, None]; k = np.arange(128)[None, :]
    cm = np.zeros((128, 256), np.float32)
    cm[:, 128:] = np.where(k <= q, 0.0, NINF)
    m0 = np.full((128, 128), 0.0 if parity else NINF, np.float32)
    mb = np.zeros((128, 24, 128), np.float32)
    for g, (W, d) in enumerate(B_PAT):
        for jj in range(B_NB[g] + 1):
            diff = 128 * jj + q - k
            ok = (diff >= 0) & (diff <= W) & (diff % d == 0)
            mb[:, B_MOFF[g] + jj, :] = np.where(ok, 0.0, NEG)
    mb0 = mb.copy() if parity else np.full_like(mb, NEG)
    return cs, cm, m0, mb.reshape(128, -1), mb0.reshape(128, -1)


def _prep_inputs(inputs, S, n_cores=8):
    f = lambda a: np.ascontiguousarray(np.asarray(a, dtype=np.float32))
    x = f(inputs["x"]); mem = f(inputs["mem"])
    w_in = f(inputs["w_in"])[0]
    o = np.cumsum([0, 512, 128, 128, 256, 64, 4, 1536, 1536, 1536, 512])
    aq, ak, av, iq, ik, iw, bq, bk, bv, mq = [w_in[:, o[i]:o[i + 1]] for i in range(10)]
    wk = np.ascontiguousarray(np.concatenate([ak, ik, av, bk, bv], axis=1))
    wq = np.ascontiguousarray(np.concatenate([aq, iq, iw, bq, mq], axis=1))
    wg0 = f(inputs["w_gate"])[0]
    wg = np.ascontiguousarray(wg0.reshape(D, 3, 8, 128).transpose(0, 2, 1, 3).reshape(D, 3072))
    bg0 = f(inputs["b_gate"])[0].reshape(3, 8, 128)
    bgate = np.ascontiguousarray(bg0.transpose(2, 1, 0).reshape(128, 24))
    wbr = np.ascontiguousarray(f(inputs["w_branch"])[0].reshape(1536, D))
    wo = f(inputs["w_out"])[0]
    wmem = f(inputs["w_mem_kv"])[0]
    w1 = np.ascontiguousarray(f(inputs["w1"])[0].reshape(16 * D, 512))
    w3 = np.ascontiguousarray(f(inputs["w3"])[0].reshape(16 * D, 512))
    w2 = np.ascontiguousarray(f(inputs["w2"])[0].reshape(16 * 512, D))
    wsub = f(inputs["w_sub"])[0]
    wr = np.ascontiguousarray(np.concatenate([f(inputs["w_group"])[0], wsub.transpose(1, 0, 2).reshape(D, 16)], axis=1))
    gc = lambda g: np.asarray(g, np.float32).reshape(8, 128).T
    gcols = np.ascontiguousarray(np.concatenate([gc(f(inputs["g_mix"])[0]), gc(f(inputs["g_mem"])[0]), gc(f(inputs["g_ffn"])[0])], axis=1))
    gfin = np.ascontiguousarray(np.broadcast_to(f(inputs["g_final"])[None, :], (128, D)))
    rb = np.concatenate([f(inputs["b_group"])[0], f(inputs["b_sub"])[0].reshape(16)])
    rbias = np.ascontiguousarray(np.broadcast_to(rb[None, :], (128, 20)))
    ident = np.eye(128, dtype=np.float32)
    shared = dict(wk=wk, wq=wq, wg=wg, wbr=wbr, wo=wo, wmem=wmem, w1=w1, w3=w3, w2=w2, wr=wr, gcols=gcols,
                  bgate=bgate, gfin=gfin, rbias=rbias, ident=ident)
    in_maps = []
    for c in range(n_cores):
        b, p = c // 2, c % 2
        if p == 0:
            xs = np.concatenate([np.zeros((128, D), np.float32), x[b, :S - 128]], axis=0)
        else:
            xs = x[b, :S]
        cs, cm, m0, mb, mb0 = _host_consts(S, p)
        m = dict(shared)
        m.update(xs=np.ascontiguousarray(xs), cs=cs, memx=np.ascontiguousarray(mem[b]), dsa_cm=cm, dsa_m0=m0, mbias=mb, mb0=mb0)
        in_maps.append(m)
    return in_maps


_CACHE = {}


def run(inputs, S, QB=2, dbg=False):
    key = (S, QB, dbg)
    if key not in _CACHE:
        _CACHE[key] = build(S, QB, dbg)
    nc, P = _CACHE[key]
    in_maps = _prep_inputs(inputs, S)
    res = run_bass_kernel_spmd(nc, in_maps, core_ids=list(range(8)))
    B = 4
    out = np.zeros((B, S, D), np.float32)
    for c in range(8):
        b, p = c // 2, c % 2
        o = np.asarray(res.results[c]["out"]).reshape(S // 256, 128, D)
        out[b].reshape(S // 256, 2, 128, D)[:, p] = o
    return out, res


def kernel(**inputs):
    out, _ = run(inputs, 8192, QB=2)
    return out
```

```python
import contextlib
import os
RMS_CUT = int(os.environ.get('RMS_CUT', '9'))
BM_CUT = int(os.environ.get('BM_CUT', '9'))
import numpy as np
import concourse.bass as bass
import concourse.mybir as mybir
from concourse.bass_utils import run_bass_kernel_spmd

F32 = mybir.dt.float32
BF16 = mybir.dt.bfloat16
AF = mybir.ActivationFunctionType
ALU = mybir.AluOpType
AX = mybir.AxisListType.X

D = 1024
NEG = -30000.0
NINF = -1.0e30
NBIS = 17
B_PAT = ((128, 1), (512, 4), (2048, 16))
B_NB = (1, 4, 16)
B_MOFF = (0, 2, 7)
ENGS = ("pe", "act", "dve", "pool", "sp")
EPOCH = 12000


class Res:
    __slots__ = ("w", "r", "excl")

    def __init__(self, excl=False):
        self.w = None
        self.r = []
        self.excl = excl


class Prog:
    def __init__(self, nc, n_dma_sems=24):
        self.nc = nc
        self.q = {e: [] for e in ENGS}
        self.sem = {}
        self.cnt = {}
        self.key_eng = {}
        self.cur = {}
        for e in ENGS:
            self._new_epoch(e, 0)
        self.ndma = n_dma_sems
        for k in range(n_dma_sems):
            key = "dma%d" % k
            self.sem[key] = nc.alloc_semaphore(name="d%d" % k)
            self.cnt[key] = 0
            self.key_eng[key] = "dma"
        self.dma_rr = 0
        self.known = {e: {} for e in ENGS}
        self.nwaits = 0
        self.nops = 0

    def _new_epoch(self, e, k):
        key = "%s#%d" % (e, k)
        self.sem[key] = self.nc.alloc_semaphore(name="s_%s_%d" % (e, k))
        self.cnt[key] = 0
        self.key_eng[key] = e
        self.cur[e] = (key, k)

    def _need(self, eng, tok, waits):
        if tok is None:
            return
        key, val = tok
        if self.known[eng].get(key, 0) >= val:
            return
        if waits.get(key, 0) < val:
            waits[key] = val

    def _deps(self, eng, reads, writes, same_sync):
        waits = {}
        for r in reads:
            if r.excl:
                for t in r.r:
                    if self.key_eng[t[0]] != eng:
                        self._need(eng, t, waits)
            if r.w is not None:
                if self.key_eng[r.w[0]] == eng and not same_sync:
                    continue
                self._need(eng, r.w, waits)
        for w in writes:
            if w.w is not None:
                if not (self.key_eng[w.w[0]] == eng and not same_sync):
                    self._need(eng, w.w, waits)
            for t in w.r:
                if self.key_eng[t[0]] == eng:
                    continue
                self._need(eng, t, waits)
        for k, v in waits.items():
            self.known[eng][k] = v
        return waits

    def _commit(self, tok, reads, writes):
        for r in reads:
            r.r.append(tok)
            if len(r.r) > 48:
                best = {}
                for k, v in r.r:
                    if best.get(k, 0) < v:
                        best[k] = v
                r.r = list(best.items())
        for w in writes:
            w.w = tok
            w.r = []

    def op(self, eng, fn, reads=(), writes=(), same_sync=True):
        if eng == "pe":
            same_sync = False
        waits = self._deps(eng, reads, writes, same_sync)
        key, k = self.cur[eng]
        if self.cnt[key] >= EPOCH:
            self._new_epoch(eng, k + 1)
            key, k = self.cur[eng]
        self.cnt[key] += 1
        tok = (key, self.cnt[key])
        self._commit(tok, reads, writes)
        self.q[eng].append((waits, fn, (key, 1)))
        self.nwaits += len(waits)
        self.nops += 1
        return tok

    def dma(self, qeng, fn, reads=(), writes=()):
        k = self.dma_rr
        self.dma_rr = (self.dma_rr + 1) % self.ndma
        key = "dma%d" % k
        waits = self._deps(qeng, reads, writes, True)
        prev = self.cnt[key]
        if prev > 0 and self.known[qeng].get(key, 0) < prev:
            waits[key] = max(waits.get(key, 0), prev)
            self.known[qeng][key] = prev
        self.cnt[key] += 16
        tok = (key, self.cnt[key])
        self._commit(tok, reads, writes)
        self.q[qeng].append((waits, fn, (key, 16)))
        self.nwaits += len(waits)
        self.nops += 1
        return tok

    def wait_tokens(self, eng, toks):
        waits = {}
        for t in toks:
            self._need(eng, t, waits)
        for k, v in waits.items():
            self.known[eng][k] = v
        self.q[eng].append((waits, None, None))

    def emit(self):
        nc = self.nc
        with nc.Block() as block:
            def mk(ename):
                def body(e):
                    for waits, fn, inc in self.q[ename]:
                        for k, v in waits.items():
                            e.wait_ge(self.sem[k], v)
                        if fn is not None:
                            ins = fn(e)
                            ins.then_inc(self.sem[inc[0]], inc[1])
                return body
            block.tensor(mk("pe"))
            block.scalar(mk("act"))
            block.vector(mk("dve"))
            block.gpsimd(mk("pool"))
            block.sync(mk("sp"))


WK_COLS = 3392
WQ_COLS = 2820
K_SLABS = [("a", 0, 320)] + [("bk%d" % g, 320 + 512 * g, 512) for g in range(3)] + \
          [("bv%d" % g, 1856 + 512 * g, 512) for g in range(3)]
Q_SLABS = [("aq", 0, 512), ("iq", 512, 260)] + [("bq%d" % g, 772 + 512 * g, 512) for g in range(3)] + \
          [("mq", 2308, 512)]


def build(S, QB, dbg=False, stop=None):
    NBLK = S // 128
    NQ = NBLK // 2
    NIT = NQ // QB
    N = QB * 128
    assert NBLK % 4 == 0 and NQ % QB == 0

    nc = bass.Bass("TRN2", target_bir_lowering=False)

    def din(name, shape, dt=F32):
        return nc.dram_tensor(name, shape, dt, kind="ExternalInput").ap()

    def dscr(name, shape, dt=BF16):
        return nc.dram_tensor(name, shape, dt).ap()

    xs = din("xs", [S, D])
    cs = din("cs", [S, 64])
    memx = din("memx", [256, D])
    wk = din("wk", [D, WK_COLS])
    wq = din("wq", [D, WQ_COLS])
    wg = din("wg", [D, 3072])
    wbr = din("wbr", [1536, D])
    wo = din("wo", [D, D])
    wmem = din("wmem", [D, 1024])
    w1 = din("w1", [16 * D, 512])
    w3 = din("w3", [16 * D, 512])
    w2 = din("w2", [16 * 512, D])
    wr = din("wr", [D, 20])
    gcols_d = din("gcols", [128, 24])
    bgate_d = din("bgate", [128, 24])
    gfin_d = din("gfin", [128, D])
    rbias_d = din("rbias", [128, 20])
    ident_d = din("ident", [128, 128])
    cm_d = din("dsa_cm", [128, 256])
    m0_d = din("dsa_m0", [128, 128])
    mb_d = din("mbias", [128, 24 * 128])
    mb0_d = din("mb0", [128, 24 * 128])
    out_d = nc.dram_tensor("out", [NQ * 128, D], F32, kind="ExternalOutput").ap()
    dbg_d = {}
    if dbg:
        for nm, shp in (("d_ya", [64, 8 * N]), ("d_yb", [64, 8 * N]), ("d_ym", [128, 4 * N]), ("d_h", [128, QB * D])):
            dbg_d[nm] = nc.dram_tensor(nm, shp, F32, kind="ExternalOutput").ap()

    wk_b = dscr("wk_b", [D, WK_COLS])
    wq_b = dscr("wq_b", [D, WQ_COLS])
    wg_b = dscr("wg_b", [D, 3072])
    wbr_b = dscr("wbr_b", [1536, D])
    wo_b = dscr("wo_b", [D, D])
    wmem_b = dscr("wmem_b", [D, 1024])
    w1_b = dscr("w1_b", [16 * D, 512])
    w3_b = dscr("w3_b", [16 * D, 512])
    w2_b = dscr("w2_b", [16 * 512, D])
    KaT_d = dscr("KaT_d", [128, S])
    kiT_d = dscr("kiT_d", [64, S])
    Va_d = dscr("Va_d", [S, 256])
    bkT_d = dscr("bkT_d", [128, 3, 4, S])
    Vb_d = dscr("Vb_d", [S, 3, 1024])

    P = Prog(nc)
    es = contextlib.ExitStack()
    with es:
        def sb(name, shape, dt=F32):
            return es.enter_context(nc.sbuf_tensor("s_" + name, shape, dt))

        pbank = [es.enter_context(nc.psum_tensor("pb%d" % i, [128, 512], F32)) for i in range(8)]
        Rb = [Res(excl=True) for _ in range(8)]
        rot_state = [0]

        def rot():
            k = 4 + rot_state[0]
            rot_state[0] = (rot_state[0] + 1) % 4
            return pbank[k], Rb[k]

        identf = sb("identf", [128, 128]); identb = sb("identb", [128, 128], BF16)
        i4b = sb("i4b", [128, 512], BF16)
        ones_b = sb("ones_b", [128, 128], BF16)
        gcols = sb("gcols", [128, 24]); bgate = sb("bgate", [128, 24])
        gfin = sb("gfin", [128, D]); rbias = sb("rbias", [128, 20])
        cm = sb("cm", [128, 256]); m0 = sb("m0", [128, 128])
        mbt = sb("mbt", [128, 24 * 128], BF16); mb0t = sb("mb0t", [128, 24 * 128], BF16)
        pow2 = sb("pow2", [128, NBIS + 1])
        wr32 = sb("wr32", [128, 8, 20])
        kmT = sb("kmT", [128, 4, 256], BF16); vmb = sb("vmb", [128, 2, 512], BF16)
        Rc = Res()

        def ld_const(dst, src):
            P.dma("sp", lambda e: e.dma_start(out=dst, in_=src), writes=[Rc])

        ld_const(identf[:], ident_d)
        ld_const(gcols[:], gcols_d); ld_const(bgate[:], bgate_d); ld_const(gfin[:], gfin_d)
        ld_const(rbias[:], rbias_d); ld_const(cm[:], cm_d); ld_const(m0[:], m0_d)
        ld_const(wr32[:], wr.rearrange("(c p) n -> p c n", p=128))
        P.op("dve", lambda e: e.tensor_copy(out=identb[:], in_=identf[:]), reads=[Rc], writes=[Rc])
        for j in range(4):
            P.op("dve", lambda e, j=j: e.tensor_copy(out=i4b[:, j * 128:(j + 1) * 128], in_=identf[:]), reads=[Rc], writes=[Rc])
        P.op("dve", lambda e: e.memset(ones_b[:], 1.0), writes=[Rc])
        for i in range(NBIS + 1):
            P.op("dve", lambda e, i=i: e.memset(pow2[:, i:i + 1], 2.0 ** (-i)), writes=[Rc])

        def finish0():
            toks = [("dma%d" % k, P.cnt["dma%d" % k]) for k in range(P.ndma) if P.cnt["dma%d" % k] > 0]
            toks += [(P.cur[e_][0], P.cnt[P.cur[e_][0]]) for e_ in ("pe", "act", "dve", "pool") if P.cnt[P.cur[e_][0]] > 0]
            P.wait_tokens("sp", toks)
            P.emit()
        if stop == "c":
            finish0(); return nc, P
        Rw_b = Res()
        conv_hist = []

        def convert(src, dst):
            fs = src.rearrange("r c -> (r c)").rearrange("(n k) -> n k", k=1024)
            fd = dst.rearrange("r c -> (r c)").rearrange("(n k) -> n k", k=1024)
            n = fs.shape[0]
            for r0 in range(0, n, 1024):
                r1_ = min(n, r0 + 1024)
                if len(conv_hist) >= 4:
                    P.wait_tokens("pool", [conv_hist[-4]])
                conv_hist.append(P.dma("pool", lambda e, r0=r0, r1_=r1_: e.dma_start(out=fd[r0:r1_, :], in_=fs[r0:r1_, :])))

        for s_, d_ in ((wmem, wmem_b), (wk, wk_b), (wq, wq_b), (wg, wg_b), (wbr, wbr_b), (wo, wo_b),
                       (w1, w1_b), (w3, w3_b), (w2, w2_b)):
            convert(s_, d_)
        conv_tokens = []
        for k in range(P.ndma):
            key = "dma%d" % k
            if P.cnt[key] > 0:
                conv_tokens.append((key, P.cnt[key]))

        if stop == "conv":
            finish0(); return nc, P
        ss_t = sb("ss_t", [128, 4]); rs_t = sb("rs_t", [128, 4])
        sqj = sb("sqj", [128, D], BF16)
        xb_t = [sb("xb0", [128, D], BF16)] * 2
        Rstat = [Res() for _ in range(4)]
        Rsqj = Res()
        Rxb = [Res()] * 2
        rms_ctr = [0]

        def rms_T(x_ap, Rx, gofs, dst_fn, Rdst):
            i = rms_ctr[0] % 4
            j = rms_ctr[0] % 2
            rms_ctr[0] += 1
            ss = ss_t[:, i:i + 1]; rs = rs_t[:, i:i + 1]
            P.op("act", lambda e: e.activation(out=sqj[:], in_=x_ap, func=AF.Square, accum_out=ss),
                 reads=[Rx], writes=[Rsqj, Rstat[i]])
            if RMS_CUT <= 1:
                return i
            P.op("dve", lambda e: e.tensor_scalar(out=rs, in0=ss, scalar1=1.0 / D, scalar2=1e-6, op0=ALU.mult, op1=ALU.add),
                 reads=[Rstat[i]], writes=[Rstat[i]])
            P.op("act", lambda e: e.activation(out=rs, in_=rs, func=AF.Sqrt),
                 reads=[Rstat[i]], writes=[Rstat[i]])
            P.op("dve", lambda e: e.reciprocal(out=rs, in_=rs), reads=[Rstat[i]], writes=[Rstat[i]])
            if RMS_CUT <= 2:
                return i
            xb = xb_t[j]
            P.op("dve", lambda e: e.tensor_scalar(out=xb[:], in0=x_ap, scalar1=rs, scalar2=None, op0=ALU.mult),
                 reads=[Rx, Rstat[i]], writes=[Rxb[j]])
            if RMS_CUT <= 3:
                return i
            for half in range(2):
                pb, Rp = rot()
                pbb = pb[:].bitcast(BF16)
                for c4 in range(4):
                    c = half * 4 + c4
                    P.op("pe", lambda e, c=c, c4=c4, pbb=pbb: e.transpose(out=pbb[:, c4 * 128:(c4 + 1) * 128], in_=xb[:, c * 128:(c + 1) * 128], identity=identb[:]),
                         reads=[Rxb[j], Rc], writes=[Rp])
                if RMS_CUT <= 4:
                    continue
                for c4 in range(4):
                    c = half * 4 + c4
                    eng = "dve"
                    if eng == "dve":
                        P.op("dve", lambda e, c=c, c4=c4, pbb=pbb: e.tensor_scalar(out=dst_fn(c), in0=pbb[:, c4 * 128:(c4 + 1) * 128], scalar1=gcols[:, gofs + c:gofs + c + 1], scalar2=None, op0=ALU.mult),
                             reads=[Rp, Rc], writes=[Rdst])
                    else:
                        P.op("act", lambda e, c=c, c4=c4, pbb=pbb: e.activation(out=dst_fn(c), in_=pbb[:, c4 * 128:(c4 + 1) * 128], func=AF.Copy, scale=gcols[:, gofs + c:gofs + c + 1]),
                             reads=[Rp, Rc], writes=[Rdst])
            return i

        rt = [sb("rt%d" % i, [128, 256]) for i in range(4)]
        Rrt = [Res() for _ in range(4)]

        def rope(ps_ap, Rps, H, cs_ap, Rcs, out_ap, Rout, perm=False):
            n = H * 32
            if os.environ.get("NOROPE"):
                P.op("act", lambda e: e.copy(out=out_ap, in_=ps_ap), reads=[Rps], writes=[Rout])
                return
            if perm:
                x = ps_ap.rearrange("p (g j t d) -> p g j t d", g=2, t=2, d=32)
                o = out_ap.rearrange("p (j g t d) -> p g j t d", g=2, t=2, d=32)
                x1, x2 = x[:, :, :, 0, :], x[:, :, :, 1, :]
                o1, o2 = o[:, :, :, 0, :], o[:, :, :, 1, :]
                cos = cs_ap[:, 0:32].unsqueeze(1).unsqueeze(1).to_broadcast([128, 2, 4, 32])
                sin = cs_ap[:, 32:64].unsqueeze(1).unsqueeze(1).to_broadcast([128, 2, 4, 32])
                tv = [rt[i][:, 0:n].rearrange("p (g j d) -> p g j d", g=2, d=32) for i in range(4)]
            else:
                x = ps_ap.rearrange("p (h t d) -> p h t d", t=2, d=32)
                o = out_ap.rearrange("p (h t d) -> p h t d", t=2, d=32)
                x1, x2 = x[:, :, 0, :], x[:, :, 1, :]
                o1, o2 = o[:, :, 0, :], o[:, :, 1, :]
                cos = cs_ap[:, 0:32].unsqueeze(1).to_broadcast([128, H, 32])
                sin = cs_ap[:, 32:64].unsqueeze(1).to_broadcast([128, H, 32])
                tv = [rt[i][:, 0:n].rearrange("p (h d) -> p h d", d=32) for i in range(4)]
            P.op("dve", lambda e: e.tensor_tensor(out=tv[0], in0=x1, in1=cos, op=ALU.mult), reads=[Rps, Rcs], writes=[Rrt[0]])
            P.op("dve", lambda e: e.tensor_tensor(out=tv[1], in0=x2, in1=sin, op=ALU.mult), reads=[Rps, Rcs], writes=[Rrt[1]])
            P.op("dve", lambda e: e.tensor_tensor(out=tv[2], in0=x2, in1=cos, op=ALU.mult), reads=[Rps, Rcs], writes=[Rrt[2]])
            P.op("dve", lambda e: e.tensor_tensor(out=tv[3], in0=x1, in1=sin, op=ALU.mult), reads=[Rps, Rcs], writes=[Rrt[3]])
            P.op("dve", lambda e: e.tensor_tensor(out=o1, in0=tv[0], in1=tv[1], op=ALU.subtract), reads=[Rrt[0], Rrt[1]], writes=[Rout])
            P.op("dve", lambda e: e.tensor_tensor(out=o2, in0=tv[2], in1=tv[3], op=ALU.add), reads=[Rrt[2], Rrt[3]], writes=[Rout])

        wslab = [sb("wslab%d" % i, [128, 8, 512], BF16) for i in range(2)]
        Rwslab = [Res(), Res()]
        slab_ctr = [0]

        def load_slab(wsrc_b, c0, ncol):
            i = slab_ctr[0] % 2
            slab_ctr[0] += 1
            src = wsrc_b.rearrange("(c p) n -> p c n", p=128)[:, :, c0:c0 + ncol]
            P.dma("sp", lambda e: e.dma_start(out=wslab[i][:, :, 0:ncol], in_=src), reads=[Rw_b], writes=[Rwslab[i]])
            return wslab[i], Rwslab[i]

        def proj(nT, RnT, tok0, ws, Rws, ncol):
            pb, Rp = rot()
            for c in range(8):
                P.op("pe", lambda e, c=c: e.matmul(pb[:, 0:ncol], lhsT=nT[:, c, tok0:tok0 + 128], rhs=ws[:, c, 0:ncol], start=(c == 0), stop=(c == 7)),
                     reads=[RnT, Rws], writes=[Rp])
            return pb, Rp

        tst = [sb("tst%d" % i, [128, 512], BF16) for i in range(2)]
        Rtst = [Res(), Res()]
        tst_ctr = [0]

        def next_tst():
            i = tst_ctr[0] % 2
            tst_ctr[0] += 1
            return tst[i], Rtst[i]

        def transposes(src, Rsrc, ncols_each, nchunks, dst_fn, Rdst, eng="act"):
            pb, Rp = rot()
            pbb = pb[:].bitcast(BF16)
            for c in range(nchunks):
                P.op("pe", lambda e, c=c: e.transpose(out=pbb[0:ncols_each, c * 128:(c + 1) * 128], in_=src[:, c * ncols_each:(c + 1) * ncols_each], identity=identb[:]),
                     reads=[Rsrc, Rc], writes=[Rp])
            for c in range(nchunks):
                if eng == "act":
                    P.op("act", lambda e, c=c: e.copy(out=dst_fn(c), in_=pbb[0:ncols_each, c * 128:(c + 1) * 128]), reads=[Rp], writes=[Rdst])
                else:
                    P.op("dve", lambda e, c=c: e.tensor_copy(out=dst_fn(c), in_=pbb[0:ncols_each, c * 128:(c + 1) * 128]), reads=[Rp], writes=[Rdst])

        for e_ in ("sp", "act", "dve", "pe", "pool"):
            P.wait_tokens(e_, conv_tokens)

        xt = [sb("xt%d" % i, [128, D]) for i in range(2)]
        Rxt = [Res(), Res()]
        cst = [sb("cst%d" % i, [128, 64]) for i in range(4)]
        Rcst = [Res() for _ in range(4)]
        nTk = sb("nTk", [128, 8, 512], BF16)
        RnTk = Res()
        pcs = 0
        for (src, dst) in ((mb_d, mbt), (mb0_d, mb0t)):
            for pc in range(3):
                jx = pcs % 2; pcs += 1
                P.dma("sp", lambda e, src=src, pc=pc, jx=jx: e.dma_start(out=xt[jx][:], in_=src[:, pc * 1024:(pc + 1) * 1024]), writes=[Rxt[jx]])
                P.op("dve", lambda e, dst=dst, pc=pc, jx=jx: e.tensor_copy(out=dst[:, pc * 1024:(pc + 1) * 1024], in_=xt[jx][:]), reads=[Rxt[jx]], writes=[Rc])
        if stop == "m1":
            finish0(); return nc, P
        for mbk in range(2):
            P.dma("sp", lambda e, mbk=mbk: e.dma_start(out=xt[mbk][:], in_=memx[mbk * 128:(mbk + 1) * 128, :]), writes=[Rxt[mbk]])
            rms_T(xt[mbk][:], Rxt[mbk], 8, lambda c, mbk=mbk: nTk[:, c, mbk * 128:(mbk + 1) * 128], RnTk)
        if stop == "m2":
            finish0(); return nc, P
        Rkm = Res()
        for half in range(2):
            ws, Rws = load_slab(wmem_b, half * 512, 512)
            for mbk in range(2):
                pb, Rp = proj(nTk, RnTk, mbk * 128, ws, Rws, 512)
                if half == 0:
                    t_, Rt_ = next_tst()
                    P.op("act", lambda e, t_=t_, pb=pb: e.copy(out=t_[:], in_=pb[:]), reads=[Rp], writes=[Rt_])
                    transposes(t_, Rt_, 128, 4, lambda c, mbk=mbk: kmT[:, c, mbk * 128:(mbk + 1) * 128], Rkm)
                else:
                    P.op("act", lambda e, pb=pb, mbk=mbk: e.copy(out=vmb[:, mbk, :], in_=pb[:]), reads=[Rp], writes=[Rkm])

        def finish():
            toks = [("dma%d" % k, P.cnt["dma%d" % k]) for k in range(P.ndma) if P.cnt["dma%d" % k] > 0]
            toks += [(P.cur[e_][0], P.cnt[P.cur[e_][0]]) for e_ in ("pe", "act", "dve", "pool") if P.cnt[P.cur[e_][0]] > 0]
            P.wait_tokens("sp", toks)
            P.emit()

        if stop == "p0":
            finish(); return nc, P
        r1 = sb("r1", [128, 24576], BF16)
        KaTst = r1[:, 0:512]; kiTst = r1[0:64, 512:1024]
        Vast = r1[:, 1024:2048].rearrange("p (b n) -> p b n", b=4)
        bkTst = [r1[:, 2048 + i * 2048:2048 + (i + 1) * 2048].rearrange("p (c n) -> p c n", c=4) for i in range(2)]
        Vbst = [r1[:, 6144 + i * 4096:6144 + (i + 1) * 4096].rearrange("p (b n) -> p b n", b=4) for i in range(2)]
        RKaTst, RkiTst, RVast = Res(), Res(), Res()
        RbkTst = [Res(), Res()]; RVbst = [Res(), Res()]
        RKd = Res()
        P.op("pool", lambda e: e.memset(Vast, 1.0), writes=[RVast])
        for i in range(2):
            P.op("pool", lambda e, i=i: e.memset(Vbst[i], 1.0), writes=[RVbst[i]])

        for T in range(NBLK // 4):
            for bl in range(4):
                sbk = T * 4 + bl
                j = sbk % 2
                P.dma("sp", lambda e, sbk=sbk, j=j: e.dma_start(out=xt[j][:], in_=xs[sbk * 128:(sbk + 1) * 128, :]), writes=[Rxt[j]])
                P.dma("sp", lambda e, sbk=sbk, bl=bl: e.dma_start(out=cst[bl][:], in_=cs[sbk * 128:(sbk + 1) * 128, :]), writes=[Rcst[bl]])
                rms_T(xt[j][:], Rxt[j], 0, lambda c, bl=bl: nTk[:, c, bl * 128:(bl + 1) * 128], RnTk)
            for si, (nm, c0, ncol) in enumerate(K_SLABS):
                ws, Rws = load_slab(wk_b, c0, ncol)
                for bl in range(4):
                    pb, Rp = proj(nTk, RnTk, bl * 128, ws, Rws, ncol)
                    if nm == "a":
                        t_, Rt_ = next_tst()
                        rope(pb[:, 0:192], Rp, 3, cst[bl][:], Rcst[bl], t_[:, 0:192], Rt_)
                        Vv = Vast[:, bl, :].rearrange("p (g t d) -> p g t d", g=2, t=2)[:, :, 0, :]
                        P.op("act", lambda e, pb=pb, Vv=Vv: e.copy(out=Vv, in_=pb[:, 192:320].rearrange("p (g d) -> p g d", g=2)), reads=[Rp], writes=[RVast])
                        transposes(t_, Rt_, 128, 1, lambda c, bl=bl: KaTst[:, bl * 128:(bl + 1) * 128], RKaTst)
                        pb2, Rp2 = rot()
                        pbb2 = pb2[:].bitcast(BF16)
                        P.op("pe", lambda e, t_=t_, pbb2=pbb2: e.transpose(out=pbb2[0:64, 0:128], in_=t_[:, 128:192], identity=identb[:]), reads=[Rt_, Rc], writes=[Rp2])
                        P.op("act", lambda e, pbb2=pbb2, bl=bl: e.copy(out=kiTst[:, bl * 128:(bl + 1) * 128], in_=pbb2[0:64, 0:128]), reads=[Rp2], writes=[RkiTst])
                    elif nm.startswith("bk"):
                        g = int(nm[2]); jb = g % 2
                        t_, Rt_ = next_tst()
                        rope(pb[:, 0:512], Rp, 8, cst[bl][:], Rcst[bl], t_[:, 0:512], Rt_)
                        transposes(t_, Rt_, 128, 4, lambda c, bl=bl, jb=jb: bkTst[jb][:, c, bl * 128:(bl + 1) * 128], RbkTst[jb])
                    else:
                        g = int(nm[2]); jb = g % 2
                        Vv = Vbst[jb][:, bl, :].rearrange("p (h t d) -> p h t d", h=8, t=2)[:, :, 0, :]
                        P.op("act", lambda e, pb=pb, Vv=Vv: e.copy(out=Vv, in_=pb[:, 0:512].rearrange("p (h d) -> p h d", h=8)), reads=[Rp], writes=[RVbst[jb]])
                if nm == "a":
                    P.dma("pool", lambda e, T=T: e.dma_start(out=KaT_d[:, T * 512:(T + 1) * 512], in_=KaTst), reads=[RKaTst])
                    P.dma("pool", lambda e, T=T: e.dma_start(out=kiT_d[:, T * 512:(T + 1) * 512], in_=kiTst), reads=[RkiTst])
                    P.dma("pool", lambda e, T=T: e.dma_start(out=Va_d[T * 512:(T + 1) * 512, :].rearrange("(b p) n -> p b n", p=128), in_=Vast), reads=[RVast])
                elif nm.startswith("bk"):
                    g = int(nm[2]); jb = g % 2
                    P.dma("pool", lambda e, T=T, g=g, jb=jb: e.dma_start(out=bkT_d[:, g, :, T * 512:(T + 1) * 512], in_=bkTst[jb]), reads=[RbkTst[jb]])
                else:
                    g = int(nm[2]); jb = g % 2
                    P.dma("pool", lambda e, T=T, g=g, jb=jb: e.dma_start(out=Vb_d[T * 512:(T + 1) * 512, g, :].rearrange("(b p) n -> p b n", p=128), in_=Vbst[jb]), reads=[RVbst[jb]])
        kd_tokens = [("dma%d" % k, P.cnt["dma%d" % k]) for k in range(P.ndma) if P.cnt["dma%d" % k] > 0]
        P.wait_tokens("sp", kd_tokens)

        if stop == "p1":
            finish(); return nc, P
        xh = sb("xh", [128, QB, D]); Rxh = [Res() for _ in range(QB)]
        nTq = nTk[:, :, 0:N]; RnTq = RnTk
        csq = [sb("csq%d" % i, [128, 64]) for i in range(QB)]; Rcsq = [Res() for _ in range(QB)]
        QaT = [sb("QaT%d" % i, [128, 4, 128], BF16) for i in range(QB)]; RQaT = [Res() for _ in range(QB)]
        iqT = [sb("iqT%d" % i, [64, 4, 128], BF16) for i in range(QB)]; RiqT = [Res() for _ in range(QB)]
        bqT = [sb("bqT%d" % i, [128, 3, 4, 128], BF16) for i in range(QB)]; RbqT = [Res() for _ in range(QB)]
        mqT = [sb("mqT%d" % i, [128, 4, 128], BF16) for i in range(QB)]; RmqT = [Res() for _ in range(QB)]
        wv = [sb("wv%d" % i, [128, 12]) for i in range(QB)]; Rwv = [Res() for _ in range(QB)]
        dg = [sb("dg%d" % i, [128, 4, 128], BF16) for i in range(QB)]; Rdg = [Res() for _ in range(QB)]
        yaT = sb("yaT", [64, 8, N], BF16); ybT = sb("ybT", [64, 8, N], BF16); ymT = sb("ymT", [128, 4, N], BF16)
        RyaT, RybT, RymT = Res(), Res(), Res()
        score = r1[:, 0:16384].bitcast(F32)
        junk = r1[:, 16384:24576]
        Rsc, Rjk = Res(), Res()
        Rmw = [Res(), Res()]
        kib = [sb("kib%d" % i, [64, 512], BF16) for i in range(2)]; Rkib = [Res(), Res()]
        Rh = [sb("Rh%d" % i, [128, 512], BF16) for i in range(4)]; RRh = [Res() for _ in range(4)]
        bis = sb("bis", [128, 8 + NBIS + 1]); Rbis = Res()
        nmb = [sb("nmb%d" % i, [128, 512], BF16) for i in range(2)]; Rnmb = [Res(), Res()]
        kab = [sb("kab%d" % i, [128, 512], BF16) for i in range(2)]; Rkab = [Res(), Res()]
        vab = [sb("vab%d" % i, [128, 4, 256], BF16) for i in range(2)]; Rvab = [Res(), Res()]
        pt = [sb("pt%d" % i, [128, 512], BF16) for i in range(4)]; Rpt = [Res() for _ in range(4)]
        pt_ctr = [0]
        rden = sb("rden", [128, 512]); Rrden = Res()
        NBB = 3
        bkb = [sb("bkb%d" % i, [128, 4, 128], BF16) for i in range(NBB)]; Rbkb = [Res() for _ in range(NBB)]
        vbb = [sb("vbb%d" % i, [128, 1024], BF16) for i in range(NBB)]; Rvbb = [Res() for _ in range(NBB)]
        bb_ctr = [0]
        gt = [sb("gt%d" % i, [128, N]) for i in range(3)]; Rgt = [Res() for _ in range(3)]
        wgs = [sb("wgs0", [128, 8, 384], BF16)] * 2; Rwgs = [Res()] * 2
        wba = [sb("wba0", [64, 16, 128], BF16)] * 2; Rwba = [Res()] * 2
        wbm = [sb("wbm0", [128, 4, 128], BF16)] * 2; Rwbm = [Res()] * 2
        mtmp = [sb("mtmp%d" % i, [128, N]) for i in range(3)]; Rmtmp = [Res() for _ in range(3)]
        mT = sb("mT", [128, 8, N], BF16); RmT = Res()
        xnT = mT; RxnT = RmT
        xf32 = xt[0]; Rxf32 = Rxt[0]
        xnT32 = xt[1][:].rearrange("p (c n) -> p c n", c=8); RxnT32 = Rxt[1]
        rl = sb("rl", [128, 64]); Rrl = Res()
        comb = [sb("comb%d" % i, [128, 16]) for i in range(QB)]; Rcomb = [Res() for _ in range(QB)]
        sA = [sb("sA%d" % i, [128, N], BF16) for i in range(2)]; RsA = [Res(), Res()]
        hT = [sb("hT%d" % i, [128, 4, N], BF16) for i in range(2)]; RhT = [Res(), Res()]
        ot = xf32; Rot = Rxf32
        out_tokens = []

        def next_pt():
            i = pt_ctr[0] % 4
            pt_ctr[0] += 1
            return pt[i], Rpt[i]

        def dsa(t, sq):
            nkb = sq + 1
            nk = nkb * 128
            nch = (nk + 511) // 512
            aw = wv[t][:, 4:8]
            for ch in range(nch):
                ncol = min(512, nk - ch * 512)
                kb_ = kib[ch % 2]
                P.dma("sp", lambda e, ch=ch, ncol=ncol, kb_=kb_: e.dma_start(out=kb_[:, 0:ncol], in_=kiT_d[:, ch * 512:ch * 512 + ncol]), reads=[RKd], writes=[Rkib[ch % 2]])
                for h in range(4):
                    pb, Rp = rot()
                    P.op("pe", lambda e, pb=pb, h=h, ncol=ncol, kb_=kb_: e.matmul(pb[:, 0:ncol], lhsT=iqT[t][:, h, :], rhs=kb_[:, 0:ncol], start=True, stop=True),
                         reads=[RiqT[t], Rkib[ch % 2]], writes=[Rp])
                    P.op("act", lambda e, pb=pb, h=h, ncol=ncol: e.activation(out=Rh[h][:, 0:ncol], in_=pb[:, 0:ncol], func=AF.Relu, scale=aw[:, h:h + 1]),
                         reads=[Rp, Rwv[t]], writes=[RRh[h]])
                pb, Rp = rot()
                for h in range(4):
                    P.op("pe", lambda e, pb=pb, h=h, ncol=ncol: e.matmul(pb[:, 0:ncol], lhsT=dg[t][:, h, :], rhs=Rh[h][:, 0:ncol], start=(h == 0), stop=(h == 3)),
                         reads=[Rdg[t], RRh[h]], writes=[Rp])
                P.op("dve", lambda e, pb=pb, ch=ch, ncol=ncol: e.tensor_copy(out=score[:, ch * 512:ch * 512 + ncol], in_=pb[:, 0:ncol]),
                     reads=[Rp], writes=[Rsc, Rmw[0], Rmw[1]])
            am = bis[:, 0:1]; w0 = bis[:, 1:2]; mid = bis[:, 2:3]; cnt = bis[:, 3:4]; sg = bis[:, 4:5]; thr = bis[:, 5:6]
            wt2 = bis[:, 8:8 + NBIS + 1]
            P.op("dve", lambda e: e.tensor_reduce(out=am, in_=score[:, 0:nk], axis=AX, op=ALU.max, apply_absolute_value=True), reads=[Rsc], writes=[Rbis])
            P.op("dve", lambda e: e.tensor_tensor(out=score[:, nk - 256:nk], in0=score[:, nk - 256:nk], in1=cm[:], op=ALU.add), reads=[Rsc, Rc, Rbis], writes=[Rsc])
            P.op("dve", lambda e: e.tensor_tensor(out=score[:, 0:128], in0=score[:, 0:128], in1=m0[:], op=ALU.add), reads=[Rsc, Rc], writes=[Rsc])
            P.op("dve", lambda e: e.tensor_scalar(out=w0, in0=am, scalar1=1.001, scalar2=1e-6, op0=ALU.mult, op1=ALU.add), reads=[Rbis], writes=[Rbis])
            P.op("dve", lambda e: e.tensor_scalar(out=wt2, in0=pow2[:], scalar1=w0, scalar2=None, op0=ALU.mult), reads=[Rbis, Rc], writes=[Rbis])
            P.op("dve", lambda e: e.memset(mid, 0.0), writes=[Rbis])
            for it in range(NBIS):
                P.op("dve", lambda e: e.tensor_scalar(out=junk[:, 0:nk], in0=score[:, 0:nk], scalar1=mid, scalar2=None, op0=ALU.is_ge, op1=ALU.add, accum_out=cnt),
                     reads=[Rsc, Rbis], writes=[Rjk, Rbis, Rmw[1]])
                P.op("dve", lambda e: e.tensor_scalar(out=sg, in0=cnt, scalar1=255.5, scalar2=0.5, op0=ALU.is_ge, op1=ALU.subtract), reads=[Rbis], writes=[Rbis])
                P.op("dve", lambda e, it=it: e.scalar_tensor_tensor(out=mid, in0=sg, scalar=wt2[:, it:it + 1], in1=mid, op0=ALU.mult, op1=ALU.add), reads=[Rbis], writes=[Rbis])
            P.op("dve", lambda e: e.tensor_tensor(out=thr, in0=mid, in1=wt2[:, NBIS:NBIS + 1], op=ALU.subtract), reads=[Rbis], writes=[Rbis])
            def chunk_loads(k4):
                ncol = min(512, nk - k4 * 512)
                nb_here = ncol // 128
                j = k4 % 2
                P.op("dve", lambda e: e.tensor_scalar(out=nmb[j][:, 0:ncol], in0=score[:, k4 * 512:k4 * 512 + ncol], scalar1=thr, scalar2=NEG, op0=ALU.is_lt, op1=ALU.mult),
                     reads=[Rsc, Rbis], writes=[Rnmb[j]])
                P.dma("sp", lambda e: e.dma_start(out=kab[j][:, 0:ncol], in_=KaT_d[:, k4 * 512:k4 * 512 + ncol]), reads=[RKd], writes=[Rkab[j]])
                P.dma("sp", lambda e: e.dma_start(out=vab[j][:, 0:nb_here, :], in_=Va_d[k4 * 512:k4 * 512 + nb_here * 128, :].rearrange("(b p) n -> p b n", p=128)), reads=[RKd], writes=[Rvab[j]])

            items = []
            for k4 in range(nch):
                ncol = min(512, nk - k4 * 512)
                for b in range(ncol // 128):
                    for g in range(2):
                        items.append((k4, b, g))
            last_of_chunk = {}
            for idx, (k4, b, g) in enumerate(items):
                last_of_chunk[k4] = idx
            stage = {}

            def s_stage(idx):
                k4, b, g = items[idx]
                j = k4 % 2
                pb, Rp = rot()
                P.op("pe", lambda e: e.matmul(pb[:], lhsT=kab[j][g * 64:(g + 1) * 64, b * 128:(b + 1) * 128], rhs=QaT[t][g * 64:(g + 1) * 64, :, :].rearrange("p j q -> p (j q)"), start=True, stop=False),
                     reads=[Rkab[j], RQaT[t]], writes=[Rp])
                P.op("pe", lambda e: e.matmul(pb[:], lhsT=nmb[j][:, b * 128:(b + 1) * 128], rhs=i4b[:], start=False, stop=True),
                     reads=[Rnmb[j], Rc], writes=[Rp])
                stage[idx] = (pb, Rp)

            def e_stage(idx):
                pb, Rp = stage[idx]
                p_, Rp_ = next_pt()
                P.op("act", lambda e: e.activation(out=p_[:], in_=pb[:], func=AF.Exp, scale=0.125), reads=[Rp], writes=[Rp_])
                stage[idx] = (p_, Rp_)

            def v_stage(idx):
                k4, b, g = items[idx]
                j = k4 % 2
                kb = k4 * 4 + b
                p_, Rp_ = stage.pop(idx)
                P.op("pe", lambda e: e.matmul(pbank[g][:], lhsT=vab[j][:, b, g * 128:(g + 1) * 128], rhs=p_[:], start=(kb == 0), stop=(kb == nkb - 1)),
                     reads=[Rvab[j], Rp_], writes=[Rb[g]])

            chunk_loads(0)
            if nch > 1:
                chunk_loads(1)
            nit = len(items)
            s_stage(0)
            if nit > 1:
                s_stage(1)
            for idx in range(nit):
                e_stage(idx)
                if idx + 2 < nit:
                    s_stage(idx + 2)
                v_stage(idx)
                k4 = items[idx][0]
                if last_of_chunk[k4] == idx and k4 + 2 < nch:
                    chunk_loads(k4 + 2)
            for g in range(2):
                P.op("dve", lambda e, g=g: e.reciprocal(out=rden[0:64, :], in_=pbank[g][64:128, :]), reads=[Rb[g]], writes=[Rrden])
                P.op("dve", lambda e, g=g: e.tensor_tensor(out=yaT[:, g * 4:(g + 1) * 4, t * 128:(t + 1) * 128], in0=pbank[g][0:64, :].rearrange("p (j q) -> p j q", j=4), in1=rden[0:64, :].rearrange("p (j q) -> p j q", j=4), op=ALU.mult),
                     reads=[Rb[g], Rrden], writes=[RyaT])

        def bmix(t, sq):
            first = [True, True]
            work = []
            for g in range(3):
                for jj in range(B_NB[g], -1, -1):
                    kb = sq - jj
                    if kb >= 0:
                        work.append((g, jj, kb))
            nwork = len(work)
            bufof = {}

            def b_load(w):
                g, jj, kb = work[w]
                i = bb_ctr[0] % NBB
                bb_ctr[0] += 1
                bufof[w] = i
                P.dma("sp", lambda e: e.dma_start(out=bkb[i][:], in_=bkT_d[:, g, :, kb * 128:(kb + 1) * 128]), reads=[RKd], writes=[Rbkb[i]])
                P.dma("sp", lambda e: e.dma_start(out=vbb[i][:], in_=Vb_d[kb * 128:(kb + 1) * 128, g, :]), reads=[RKd], writes=[Rvbb[i]])

            items = [(w, hh) for w in range(nwork) for hh in range(2)]
            nit = len(items)
            stage = {}
            loaded = [0]

            def ensure_loaded(upto):
                while loaded[0] <= min(upto, nwork - 1):
                    b_load(loaded[0])
                    loaded[0] += 1

            def s_stage(idx):
                w, hh = items[idx]
                g, jj, kb = work[w]
                ensure_loaded(w + 1 if hh == 0 else w)
                i = bufof[w]
                mtab = mb0t if kb == 0 else mbt
                mi = B_MOFF[g] + jj
                pb, Rp = rot()
                P.op("pe", lambda e: e.matmul(pb[:], lhsT=mtab[:, mi * 128:(mi + 1) * 128], rhs=i4b[:], start=True, stop=False),
                     reads=[Rc], writes=[Rp])
                for p2 in range(4):
                    P.op("pe", lambda e, p2=p2: e.matmul(pb[:, p2 * 128:(p2 + 1) * 128], lhsT=bkb[i][hh * 64:(hh + 1) * 64, p2, :], rhs=bqT[t][hh * 64:(hh + 1) * 64, g, p2, :], start=False, stop=(p2 == 3), skip_group_check=True),
                         reads=[Rbkb[i], RbqT[t]], writes=[Rp])
                stage[idx] = (pb, Rp)

            def e_stage(idx):
                pb, Rp = stage[idx]
                p_, Rp_ = next_pt()
                P.op("act", lambda e: e.activation(out=p_[:], in_=pb[:], func=AF.Exp, scale=0.125), reads=[Rp], writes=[Rp_])
                stage[idx] = (p_, Rp_)

            def v_stage(idx):
                w, hh = items[idx]
                i = bufof[w]
                p_, Rp_ = stage.pop(idx)
                for h4 in range(4):
                    h = 2 * h4 + hh
                    st = first[hh]
                    first[hh] = False
                    P.op("pe", lambda e, h4=h4, h=h, st=st: e.matmul(pbank[2 + hh][:, h4 * 128:(h4 + 1) * 128], lhsT=vbb[i][:, h * 128:(h + 1) * 128], rhs=p_[:, h4 * 128:(h4 + 1) * 128], start=st, stop=(w == nwork - 1 and h4 == 3), skip_group_check=True),
                         reads=[Rvbb[i], Rp_], writes=[Rb[2 + hh]])

            s_stage(0)
            if nit > 1:
                s_stage(1)
            for idx in range(nit):
                e_stage(idx)
                if idx + 2 < nit:
                    s_stage(idx + 2)
                v_stage(idx)
            if BM_CUT <= 3:
                return
            for hh in range(2):
                P.op("dve", lambda e, hh=hh: e.reciprocal(out=rden[0:64, :], in_=pbank[2 + hh][64:128, :]), reads=[Rb[2 + hh]], writes=[Rrden])
                P.op("dve", lambda e, hh=hh: e.tensor_tensor(out=ybT[:, :, t * 128:(t + 1) * 128].rearrange("p (p2 hf) q -> p hf p2 q", hf=2)[:, hh], in0=pbank[2 + hh][0:64, :].rearrange("p (j q) -> p j q", j=4), in1=rden[0:64, :].rearrange("p (j q) -> p j q", j=4), op=ALU.mult),
                     reads=[Rb[2 + hh], Rrden], writes=[RybT])

        def memattn(t):
            sc = 128.0 ** -0.5
            for mbk in range(2):
                pb, Rp = rot()
                for h in range(4):
                    P.op("pe", lambda e, pb=pb, h=h, mbk=mbk: e.matmul(pb[:, h * 128:(h + 1) * 128], lhsT=kmT[:, h, mbk * 128:(mbk + 1) * 128], rhs=mqT[t][:, h, :], start=(h == 0), stop=(h == 3), skip_group_check=True),
                         reads=[Rkm, RmqT[t]], writes=[Rp])
                p_, Rp_ = next_pt()
                P.op("act", lambda e, pb=pb, p_=p_: e.activation(out=p_[:], in_=pb[:], func=AF.Exp, scale=sc), reads=[Rp], writes=[Rp_])
                for h in range(4):
                    P.op("pe", lambda e, h=h, mbk=mbk, p_=p_: e.matmul(pbank[0][:, h * 128:(h + 1) * 128], lhsT=vmb[:, mbk, h * 128:(h + 1) * 128], rhs=p_[:, h * 128:(h + 1) * 128], start=(mbk == 0 and h == 0), stop=(mbk == 1 and h == 3), skip_group_check=True),
                         reads=[Rkm, Rp_], writes=[Rb[0]])
                P.op("pe", lambda e, mbk=mbk, p_=p_: e.matmul(pbank[1][:], lhsT=ones_b[:], rhs=p_[:], start=(mbk == 0), stop=(mbk == 1)),
                     reads=[Rc, Rp_], writes=[Rb[1]])
            P.op("dve", lambda e: e.reciprocal(out=rden[:], in_=pbank[1][:]), reads=[Rb[1]], writes=[Rrden])
            P.op("dve", lambda e: e.tensor_tensor(out=ymT[:, :, t * 128:(t + 1) * 128], in0=pbank[0][:].rearrange("p (j q) -> p j q", j=4), in1=rden[:].rearrange("p (j q) -> p j q", j=4), op=ALU.mult),
                 reads=[Rb[0], Rrden], writes=[RymT])

        for I in range(NIT):
            for t in range(QB):
                sq = 2 * (I * QB + t) + 1
                P.dma("sp", lambda e, t=t, sq=sq: e.dma_start(out=xh[:, t, :], in_=xs[sq * 128:(sq + 1) * 128, :]), writes=[Rxh[t]])
                P.dma("sp", lambda e, t=t, sq=sq: e.dma_start(out=csq[t][:], in_=cs[sq * 128:(sq + 1) * 128, :]), writes=[Rcsq[t]])
                rms_T(xh[:, t, :], Rxh[t], 0, lambda c, t=t: nTq[:, c, t * 128:(t + 1) * 128], RnTq)
            for (nm, c0, ncol) in Q_SLABS:
                ws, Rws = load_slab(wq_b, c0, ncol)
                for t in range(QB):
                    pb, Rp = proj(nTq, RnTq, t * 128, ws, Rws, ncol)
                    t_, Rt_ = next_tst()
                    if nm == "aq":
                        rope(pb[:, 0:512], Rp, 8, csq[t][:], Rcsq[t], t_[:, 0:512], Rt_, perm=True)
                        transposes(t_, Rt_, 128, 4, lambda c, t=t: QaT[t][:, c, :], RQaT[t])
                    elif nm == "iq":
                        rope(pb[:, 0:256], Rp, 4, csq[t][:], Rcsq[t], t_[:, 0:256], Rt_)
                        transposes(t_, Rt_, 64, 4, lambda c, t=t: iqT[t][:, c, :], RiqT[t])
                        w_ = wv[t]
                        P.op("dve", lambda e, pb=pb, w_=w_: e.tensor_copy(out=w_[:, 0:4], in_=pb[:, 256:260]), reads=[Rp], writes=[Rwv[t]])
                        P.op("dve", lambda e, w_=w_: e.tensor_scalar(out=w_[:, 8:12], in0=w_[:, 0:4], scalar1=0.0, scalar2=2.0, op0=ALU.is_ge, op1=ALU.mult), reads=[Rwv[t]], writes=[Rwv[t]])
                        P.op("dve", lambda e, w_=w_: e.tensor_scalar(out=w_[:, 8:12], in0=w_[:, 8:12], scalar1=-1.0, scalar2=None, op0=ALU.add), reads=[Rwv[t]], writes=[Rwv[t]])
                        P.op("dve", lambda e, w_=w_: e.scalar_tensor_tensor(out=w_[:, 4:8], in0=w_[:, 0:4], scalar=0.0625, in1=w_[:, 8:12], op0=ALU.mult, op1=ALU.mult), reads=[Rwv[t]], writes=[Rwv[t]])
                        for h in range(4):
                            P.op("dve", lambda e, w_=w_, h=h, t=t: e.tensor_scalar(out=dg[t][:, h, :], in0=identf[:], scalar1=w_[:, 8 + h:9 + h], scalar2=None, op0=ALU.mult), reads=[Rwv[t], Rc], writes=[Rdg[t]])
                    elif nm.startswith("bq"):
                        g = int(nm[2])
                        rope(pb[:, 0:512], Rp, 8, csq[t][:], Rcsq[t], t_[:, 0:512], Rt_)
                        transposes(t_, Rt_, 128, 4, lambda c, t=t, g=g: bqT[t][:, g, c, :], RbqT[t])
                    else:
                        P.op("act", lambda e, pb=pb, t_=t_: e.copy(out=t_[:], in_=pb[:]), reads=[Rp], writes=[Rt_])
                        transposes(t_, Rt_, 128, 4, lambda c, t=t: mqT[t][:, c, :], RmqT[t])
            if stop == "q":
                finish(); return nc, P
            for t in range(QB):
                sq = 2 * (I * QB + t) + 1
                dsa(t, sq)
                if stop == "dsa":
                    finish(); return nc, P
                bmix(t, sq)
                if stop == "bmix":
                    finish(); return nc, P
                memattn(t)
                if stop == "mem":
                    finish(); return nc, P
            if dbg and I == NIT - 1:
                for (nm, src, R_) in (("d_ya", yaT, RyaT), ("d_yb", ybT, RybT), ("d_ym", ymT, RymT)):
                    np_ = src.shape[0]
                    P.op("dve", lambda e, src=src, np_=np_: e.tensor_copy(out=score[0:np_, 0:src.shape[1] * N], in_=src[:].rearrange("p a n -> p (a n)")), reads=[R_], writes=[Rsc, Rmw[0], Rmw[1]])
                    out_tokens.append(P.dma("sp", lambda e, nm=nm, np_=np_, src=src: e.dma_start(out=dbg_d[nm], in_=score[0:np_, 0:src.shape[1] * N]), reads=[Rsc]))
            for f in range(8):
                j = f % 2
                P.dma("sp", lambda e, f=f, j=j: e.dma_start(out=wgs[j][:], in_=wg_b.rearrange("(c p) n -> p c n", p=128)[:, :, f * 384:(f + 1) * 384]), reads=[Rw_b], writes=[Rwgs[j]])
                P.dma("sp", lambda e, f=f, j=j: e.dma_start(out=wba[j][:], in_=wbr_b[0:1024, f * 128:(f + 1) * 128].rearrange("(h p) n -> p h n", p=64)), reads=[Rw_b], writes=[Rwba[j]])
                P.dma("sp", lambda e, f=f, j=j: e.dma_start(out=wbm[j][:], in_=wbr_b[1024:1536, f * 128:(f + 1) * 128].rearrange("(h p) n -> p h n", p=128)), reads=[Rw_b], writes=[Rwbm[j]])
                for r in range(3):
                    pb, Rp = rot()
                    for c in range(8):
                        P.op("pe", lambda e, pb=pb, c=c, r=r, j=j: e.matmul(pb[:, 0:N], lhsT=wgs[j][:, c, r * 128:(r + 1) * 128], rhs=nTq[:, c, :], start=(c == 0), stop=(c == 7)),
                             reads=[Rwgs[j], RnTq], writes=[Rp])
                    P.op("act", lambda e, pb=pb, r=r, f=f: e.activation(out=gt[r][:], in_=pb[:, 0:N], func=AF.Sigmoid, bias=bgate[:, f * 3 + r:f * 3 + r + 1], scale=1.0),
                         reads=[Rp, Rc], writes=[Rgt[r]])
                for r in range(3):
                    pb, Rp = rot()
                    if r < 2:
                        ysrc, Ry = (yaT, RyaT) if r == 0 else (ybT, RybT)
                        for h in range(8):
                            P.op("pe", lambda e, pb=pb, h=h, r=r, j=j, ysrc=ysrc: e.matmul(pb[:, 0:N], lhsT=wba[j][:, r * 8 + h, :], rhs=ysrc[:, h, :], start=(h == 0), stop=(h == 7)),
                                 reads=[Rwba[j], Ry], writes=[Rp])
                    else:
                        for h in range(4):
                            P.op("pe", lambda e, pb=pb, h=h, j=j: e.matmul(pb[:, 0:N], lhsT=wbm[j][:, h, :], rhs=ymT[:, h, :], start=(h == 0), stop=(h == 3)),
                                 reads=[Rwbm[j], RymT], writes=[Rp])
                    P.op("dve", lambda e, pb=pb, r=r: e.tensor_tensor(out=mtmp[r][:], in0=pb[:, 0:N], in1=gt[r][:], op=ALU.mult), reads=[Rp, Rgt[r]], writes=[Rmtmp[r]])
                P.op("pool", lambda e: e.tensor_tensor(out=mtmp[0][:], in0=mtmp[0][:], in1=mtmp[1][:], op=ALU.add), reads=[Rmtmp[0], Rmtmp[1]], writes=[Rmtmp[0]])
                P.op("pool", lambda e, f=f: e.tensor_tensor(out=mT[:, f, :], in0=mtmp[0][:], in1=mtmp[2][:], op=ALU.add), reads=[Rmtmp[0], Rmtmp[2]], writes=[RmT])
            for n2 in range(2):
                ws, Rws = load_slab(wo_b, n2 * 512, 512)
                for t in range(QB):
                    pb, Rp = proj(mT, RmT, t * 128, ws, Rws, 512)
                    P.op("dve", lambda e, pb=pb, t=t, n2=n2: e.tensor_tensor(out=xh[:, t, n2 * 512:(n2 + 1) * 512], in0=pb[:], in1=xh[:, t, n2 * 512:(n2 + 1) * 512], op=ALU.add),
                         reads=[Rp, Rxh[t]], writes=[Rxh[t]])
            if dbg and I == NIT - 1:
                out_tokens.append(P.dma("sp", lambda e: e.dma_start(out=dbg_d["d_h"], in_=xh[:].rearrange("p t n -> p (t n)")), reads=Rxh))
            if stop == "d":
                finish(); return nc, P
            for t in range(QB):
                si = rms_T(xh[:, t, :], Rxh[t], 16, lambda c, t=t: xnT[:, c, t * 128:(t + 1) * 128], RxnT)
                rs = rs_t[:, si:si + 1]
                P.op("dve", lambda e, t=t, rs=rs: e.tensor_scalar(out=xf32[:], in0=xh[:, t, :], scalar1=rs, scalar2=None, op0=ALU.mult), reads=[Rxh[t], Rstat[si]], writes=[Rxf32])
                for half in range(2):
                    pb, Rp = rot()
                    for c4 in range(4):
                        c = half * 4 + c4
                        P.op("pe", lambda e, pb=pb, c=c, c4=c4: e.transpose(out=pb[:, c4 * 128:(c4 + 1) * 128], in_=xf32[:, c * 128:(c + 1) * 128], identity=identf[:]), reads=[Rxf32, Rc], writes=[Rp])
                    for c4 in range(4):
                        c = half * 4 + c4
                        P.op("dve", lambda e, pb=pb, c=c, c4=c4: e.tensor_scalar(out=xnT32[:, c, :], in0=pb[:, c4 * 128:(c4 + 1) * 128], scalar1=gcols[:, 16 + c:17 + c], scalar2=None, op0=ALU.mult), reads=[Rp, Rc], writes=[RxnT32])
                pb, Rp = rot()
                for c in range(8):
                    P.op("pe", lambda e, pb=pb, c=c: e.matmul(pb[:, 0:20], lhsT=xnT32[:, c, :], rhs=wr32[:, c, :], start=(c == 0), stop=(c == 7)), reads=[RxnT32, Rc], writes=[Rp])
                lg = rl[:, 0:20]; gmx = rl[:, 20:21]; ngmx = rl[:, 21:22]; gex = rl[:, 22:26]; gsum = rl[:, 26:27]; gw = rl[:, 27:28]
                ohg = rl[:, 28:32]; sel = rl[:, 32:36]; m1 = rl[:, 36:37]; oh1 = rl[:, 37:41]; sel2 = rl[:, 41:45]; m2 = rl[:, 45:46]
                oh2 = rl[:, 46:50]; ee = rl[:, 50:51]; p1 = rl[:, 51:52]; p2 = rl[:, 52:53]; cw = rl[:, 53:57]; nm1 = rl[:, 57:58]; cw2 = rl[:, 58:62]
                RW = dict(reads=[Rrl], writes=[Rrl])
                P.op("dve", lambda e, pb=pb: e.tensor_tensor(out=lg, in0=pb[:, 0:20], in1=rbias[:], op=ALU.add), reads=[Rp, Rc], writes=[Rrl])
                P.op("dve", lambda e: e.tensor_reduce(out=gmx, in_=lg[:, 0:4], axis=AX, op=ALU.max), **RW)
                P.op("dve", lambda e: e.tensor_scalar(out=ngmx, in0=gmx, scalar1=-1.0, scalar2=None, op0=ALU.mult), **RW)
                P.op("act", lambda e: e.activation(out=gex, in_=lg[:, 0:4], func=AF.Exp, bias=ngmx, scale=1.0, accum_out=gsum), **RW)
                P.op("dve", lambda e: e.reciprocal(out=gw, in_=gsum), **RW)
                P.op("dve", lambda e: e.tensor_scalar(out=ohg, in0=lg[:, 0:4], scalar1=gmx, scalar2=None, op0=ALU.is_equal), **RW)
                P.op("dve", lambda e: e.tensor_scalar(out=sel, in0=lg[:, 4:8], scalar1=ohg[:, 0:1], scalar2=None, op0=ALU.mult), **RW)
                for g in range(1, 4):
                    P.op("dve", lambda e, g=g: e.scalar_tensor_tensor(out=sel, in0=lg[:, 4 + 4 * g:8 + 4 * g], scalar=ohg[:, g:g + 1], in1=sel, op0=ALU.mult, op1=ALU.add), **RW)
                P.op("dve", lambda e: e.tensor_reduce(out=m1, in_=sel, axis=AX, op=ALU.max), **RW)
                P.op("dve", lambda e: e.tensor_scalar(out=oh1, in0=sel, scalar1=m1, scalar2=None, op0=ALU.is_equal), **RW)
                P.op("dve", lambda e: e.scalar_tensor_tensor(out=sel2, in0=oh1, scalar=NINF, in1=sel, op0=ALU.mult, op1=ALU.add), **RW)
                P.op("dve", lambda e: e.tensor_reduce(out=m2, in_=sel2, axis=AX, op=ALU.max), **RW)
                P.op("dve", lambda e: e.tensor_scalar(out=oh2, in0=sel2, scalar1=m2, scalar2=None, op0=ALU.is_equal), **RW)
                P.op("dve", lambda e: e.tensor_scalar(out=nm1, in0=m1, scalar1=-1.0, scalar2=None, op0=ALU.mult), **RW)
                P.op("act", lambda e: e.activation(out=ee, in_=m2, func=AF.Exp, bias=nm1, scale=1.0), **RW)
                P.op("dve", lambda e: e.tensor_scalar(out=p1, in0=ee, scalar1=1.0, scalar2=None, op0=ALU.add), **RW)
                P.op("dve", lambda e: e.reciprocal(out=p1, in_=p1), **RW)
                P.op("dve", lambda e: e.tensor_tensor(out=p2, in0=ee, in1=p1, op=ALU.mult), **RW)
                P.op("dve", lambda e: e.tensor_scalar(out=cw, in0=oh1, scalar1=p1, scalar2=None, op0=ALU.mult), **RW)
                P.op("dve", lambda e: e.scalar_tensor_tensor(out=cw2, in0=oh2, scalar=p2, in1=cw, op0=ALU.mult, op1=ALU.add), **RW)
                P.op("dve", lambda e: e.tensor_scalar(out=cw, in0=cw2, scalar1=gw, scalar2=None, op0=ALU.mult), **RW)
                for g in range(4):
                    P.op("dve", lambda e, g=g, t=t: e.tensor_scalar(out=comb[t][:, g * 4:(g + 1) * 4], in0=cw, scalar1=ohg[:, g:g + 1], scalar2=None, op0=ALU.mult), reads=[Rrl], writes=[Rcomb[t]])
            for ex in range(16):
                j = ex % 2
                base = j * 12288
                w1e = r1[:, base:base + 4096].rearrange("p (c n) -> p c n", c=8)
                w3e = r1[:, base + 4096:base + 8192].rearrange("p (c n) -> p c n", c=8)
                w2e = r1[:, base + 8192:base + 12288].rearrange("p (c n) -> p c n", c=4)
                wr_ = [Rmw[j], Rsc, Rjk]
                P.dma("sp", lambda e, ex=ex, w1e=w1e: e.dma_start(out=w1e, in_=w1_b[ex * D:(ex + 1) * D, :].rearrange("(c p) n -> p c n", p=128)), reads=[Rw_b], writes=wr_)
                P.dma("sp", lambda e, ex=ex, w3e=w3e: e.dma_start(out=w3e, in_=w3_b[ex * D:(ex + 1) * D, :].rearrange("(c p) n -> p c n", p=128)), reads=[Rw_b], writes=wr_)
                P.dma("sp", lambda e, ex=ex, w2e=w2e: e.dma_start(out=w2e, in_=w2_b[ex * 512:(ex + 1) * 512, :].rearrange("(c p) n -> p c n", p=128)), reads=[Rw_b], writes=wr_)
                for c in range(4):
                    pa, Rpa = rot()
                    for k in range(8):
                        P.op("pe", lambda e, pa=pa, k=k, c=c, w1e=w1e: e.matmul(pa[:, 0:N], lhsT=w1e[:, k, c * 128:(c + 1) * 128], rhs=xnT[:, k, :], start=(k == 0), stop=(k == 7)), reads=[Rmw[j], RxnT], writes=[Rpa])
                    pb, Rp = rot()
                    for k in range(8):
                        P.op("pe", lambda e, pb=pb, k=k, c=c, w3e=w3e: e.matmul(pb[:, 0:N], lhsT=w3e[:, k, c * 128:(c + 1) * 128], rhs=xnT[:, k, :], start=(k == 0), stop=(k == 7)), reads=[Rmw[j], RxnT], writes=[Rp])
                    sj = c % 2
                    P.op("act", lambda e, pa=pa, sj=sj: e.activation(out=sA[sj][:], in_=pa[:, 0:N], func=AF.Silu), reads=[Rpa], writes=[RsA[sj]])
                    P.op("dve", lambda e, pb=pb, sj=sj, c=c, j=j: e.tensor_tensor(out=hT[j][:, c, :], in0=pb[:, 0:N], in1=sA[sj][:], op=ALU.mult), reads=[Rp, RsA[sj]], writes=[RhT[j]])
                for t in range(QB):
                    for n2 in range(2):
                        pb, Rp = rot()
                        for c in range(4):
                            P.op("pe", lambda e, pb=pb, c=c, t=t, n2=n2, j=j, w2e=w2e: e.matmul(pb[:], lhsT=hT[j][:, c, t * 128:(t + 1) * 128], rhs=w2e[:, c, n2 * 512:(n2 + 1) * 512], start=(c == 0), stop=(c == 3)), reads=[RhT[j], Rmw[j]], writes=[Rp])
                        P.op("dve", lambda e, pb=pb, t=t, n2=n2, ex=ex: e.scalar_tensor_tensor(out=xh[:, t, n2 * 512:(n2 + 1) * 512], in0=pb[:], scalar=comb[t][:, ex:ex + 1], in1=xh[:, t, n2 * 512:(n2 + 1) * 512], op0=ALU.mult, op1=ALU.add),
                             reads=[Rp, Rcomb[t], Rxh[t]], writes=[Rxh[t]])
            for t in range(QB):
                qi = I * QB + t
                ss = ss_t[:, 0:1]; rs = rs_t[:, 0:1]
                P.op("act", lambda e, t=t, ss=ss: e.activation(out=sqj[:], in_=xh[:, t, :], func=AF.Square, accum_out=ss), reads=[Rxh[t]], writes=[Rsqj, Rstat[0]])
                P.op("dve", lambda e, ss=ss, rs=rs: e.tensor_scalar(out=rs, in0=ss, scalar1=1.0 / D, scalar2=1e-6, op0=ALU.mult, op1=ALU.add), reads=[Rstat[0]], writes=[Rstat[0]])
                P.op("act", lambda e, rs=rs: e.activation(out=rs, in_=rs, func=AF.Sqrt), reads=[Rstat[0]], writes=[Rstat[0]])
                P.op("dve", lambda e, rs=rs: e.reciprocal(out=rs, in_=rs), reads=[Rstat[0]], writes=[Rstat[0]])
                P.op("dve", lambda e, t=t, rs=rs: e.scalar_tensor_tensor(out=ot[:], in0=xh[:, t, :], scalar=rs, in1=gfin[:], op0=ALU.mult, op1=ALU.mult), reads=[Rxh[t], Rstat[0], Rc], writes=[Rot])
                out_tokens.append(P.dma("sp", lambda e, qi=qi: e.dma_start(out=out_d[qi * 128:(qi + 1) * 128, :], in_=ot[:]), reads=[Rot]))
        P.wait_tokens("sp", out_tokens)
        P.emit()
    return nc, P


def _host_consts(S, parity):
    pos = np.arange(S, dtype=np.float32) - (0 if parity else 128)
    half = 32
    inv = (10000.0 ** (-np.arange(half, dtype=np.float32) / half)).astype(np.float32)
    ang = pos[:, None] * inv[None, :]
    cs = np.concatenate([np.cos(ang), np.sin(ang)], axis=1).astype(np.float32)
    q = np.arange(128)[:, None]; k = np.arange(128)[None, :]
    cm = np.zeros((128, 256), np.float32)
    cm[:, 128:] = np.where(k <= q, 0.0, NINF)
    m0 = np.full((128, 128), 0.0 if parity else NINF, np.float32)
    mb = np.zeros((128, 24, 128), np.float32)
    for g, (W, d) in enumerate(B_PAT):
        for jj in range(B_NB[g] + 1):
            diff = 128 * jj + q - k
            ok = (diff >= 0) & (diff <= W) & (diff % d == 0)
            mb[:, B_MOFF[g] + jj, :] = np.where(ok, 0.0, NEG)
    mb0 = mb.copy() if parity else np.full_like(mb, NEG)
    return cs, cm, m0, mb.reshape(128, -1), mb0.reshape(128, -1)


def _prep_inputs(inputs, S, n_cores=8):
    f = lambda a: np.ascontiguousarray(np.asarray(a, dtype=np.float32))
    x = f(inputs["x"]); mem = f(inputs["mem"])
    w_in = f(inputs["w_in"])[0]
    o = np.cumsum([0, 512, 128, 128, 256, 64, 4, 1536, 1536, 1536, 512])
    aq, ak, av, iq, ik, iw, bq, bk, bv, mq = [w_in[:, o[i]:o[i + 1]] for i in range(10)]
    wk = np.ascontiguousarray(np.concatenate([ak, ik, av, bk, bv], axis=1))
    wq = np.ascontiguousarray(np.concatenate([aq, iq, iw, bq, mq], axis=1))
    wg0 = f(inputs["w_gate"])[0]
    wg = np.ascontiguousarray(wg0.reshape(D, 3, 8, 128).transpose(0, 2, 1, 3).reshape(D, 3072))
    bg0 = f(inputs["b_gate"])[0].reshape(3, 8, 128)
    bgate = np.ascontiguousarray(bg0.transpose(2, 1, 0).reshape(128, 24))
    wbr = np.ascontiguousarray(f(inputs["w_branch"])[0].reshape(1536, D))
    wo = f(inputs["w_out"])[0]
    wmem = f(inputs["w_mem_kv"])[0]
    w1 = np.ascontiguousarray(f(inputs["w1"])[0].reshape(16 * D, 512))
    w3 = np.ascontiguousarray(f(inputs["w3"])[0].reshape(16 * D, 512))
    w2 = np.ascontiguousarray(f(inputs["w2"])[0].reshape(16 * 512, D))
    wsub = f(inputs["w_sub"])[0]
    wr = np.ascontiguousarray(np.concatenate([f(inputs["w_group"])[0], wsub.transpose(1, 0, 2).reshape(D, 16)], axis=1))
    gc = lambda g: np.asarray(g, np.float32).reshape(8, 128).T
    gcols = np.ascontiguousarray(np.concatenate([gc(f(inputs["g_mix"])[0]), gc(f(inputs["g_mem"])[0]), gc(f(inputs["g_ffn"])[0])], axis=1))
    gfin = np.ascontiguousarray(np.broadcast_to(f(inputs["g_final"])[None, :], (128, D)))
    rb = np.concatenate([f(inputs["b_group"])[0], f(inputs["b_sub"])[0].reshape(16)])
    rbias = np.ascontiguousarray(np.broadcast_to(rb[None, :], (128, 20)))
    ident = np.eye(128, dtype=np.float32)
    shared = dict(wk=wk, wq=wq, wg=wg, wbr=wbr, wo=wo, wmem=wmem, w1=w1, w3=w3, w2=w2, wr=wr, gcols=gcols,
                  bgate=bgate, gfin=gfin, rbias=rbias, ident=ident)
    in_maps = []
    for c in range(n_cores):
        b, p = c // 2, c % 2
        if p == 0:
            xs = np.concatenate([np.zeros((128, D), np.float32), x[b, :S - 128]], axis=0)
        else:
            xs = x[b, :S]
        cs, cm, m0, mb, mb0 = _host_consts(S, p)
        m = dict(shared)
        m.update(xs=np.ascontiguousarray(xs), cs=cs, memx=np.ascontiguousarray(mem[b]), dsa_cm=cm, dsa_m0=m0, mbias=mb, mb0=mb0)
        in_maps.append(m)
    return in_maps


_CACHE = {}


def run(inputs, S, QB=2, dbg=False):
    key = (S, QB, dbg)
    if key not in _CACHE:
        _CACHE[key] = build(S, QB, dbg)
    nc, P = _CACHE[key]
    in_maps = _prep_inputs(inputs, S)
    res = run_bass_kernel_spmd(nc, in_maps, core_ids=list(range(8)))
    B = 4
    out = np.zeros((B, S, D), np.float32)
    for c in range(8):
        b, p = c // 2, c % 2
        o = np.asarray(res.results[c]["out"]).reshape(S // 256, 128, D)
        out[b].reshape(S // 256, 2, 128, D)[:, p] = o
    return out, res


def kernel(**inputs):
    out, _ = run(inputs, 8192, QB=2)
    return out
```

```python
import contextlib
import os
RMS_CUT = int(os.environ.get('RMS_CUT', '9'))
BM_CUT = int(os.environ.get('BM_CUT', '9'))
import numpy as np
import concourse.bass as bass
import concourse.mybir as mybir
from concourse.bass_utils import run_bass_kernel_spmd

F32 = mybir.dt.float32
BF16 = mybir.dt.bfloat16
AF = mybir.ActivationFunctionType
ALU = mybir.AluOpType
AX = mybir.AxisListType.X

D = 1024
NEG = -30000.0
NINF = -1.0e30
NBIS = 17
B_PAT = ((128, 1), (512, 4), (2048, 16))
B_NB = (1, 4, 16)
B_MOFF = (0, 2, 7)
ENGS = ("pe", "act", "dve", "pool", "sp")
EPOCH = 12000


class Res:
    __slots__ = ("w", "r", "excl")

    def __init__(self, excl=False):
        self.w = None
        self.r = []
        self.excl = excl


class Prog:
    def __init__(self, nc, n_dma_sems=24):
        self.nc = nc
        self.q = {e: [] for e in ENGS}
        self.sem = {}
        self.cnt = {}
        self.key_eng = {}
        self.cur = {}
        for e in ENGS:
            self._new_epoch(e, 0)
        self.ndma = n_dma_sems
        for k in range(n_dma_sems):
            key = "dma%d" % k
            self.sem[key] = nc.alloc_semaphore(name="d%d" % k)
            self.cnt[key] = 0
            self.key_eng[key] = "dma"
        self.dma_rr = 0
        self.known = {e: {} for e in ENGS}
        self.nwaits = 0
        self.nops = 0

    def _new_epoch(self, e, k):
        key = "%s#%d" % (e, k)
        self.sem[key] = self.nc.alloc_semaphore(name="s_%s_%d" % (e, k))
        self.cnt[key] = 0
        self.key_eng[key] = e
        self.cur[e] = (key, k)

    def _need(self, eng, tok, waits):
        if tok is None:
            return
        key, val = tok
        if self.known[eng].get(key, 0) >= val:
            return
        if waits.get(key, 0) < val:
            waits[key] = val

    def _deps(self, eng, reads, writes, same_sync):
        waits = {}
        for r in reads:
            if r.excl:
                for t in r.r:
                    if self.key_eng[t[0]] != eng:
                        self._need(eng, t, waits)
            if r.w is not None:
                if self.key_eng[r.w[0]] == eng and not same_sync:
                    continue
                self._need(eng, r.w, waits)
        for w in writes:
            if w.w is not None:
                if not (self.key_eng[w.w[0]] == eng and not same_sync):
                    self._need(eng, w.w, waits)
            for t in w.r:
                if self.key_eng[t[0]] == eng:
                    continue
                self._need(eng, t, waits)
        for k, v in waits.items():
            self.known[eng][k] = v
        return waits

    def _commit(self, tok, reads, writes):
        for r in reads:
            r.r.append(tok)
            if len(r.r) > 48:
                best = {}
                for k, v in r.r:
                    if best.get(k, 0) < v:
                        best[k] = v
                r.r = list(best.items())
        for w in writes:
            w.w = tok
            w.r = []

    def op(self, eng, fn, reads=(), writes=(), same_sync=True):
        if eng == "pe":
            same_sync = False
        waits = self._deps(eng, reads, writes, same_sync)
        key, k = self.cur[eng]
        if self.cnt[key] >= EPOCH:
            self._new_epoch(eng, k + 1)
            key, k = self.cur[eng]
        self.cnt[key] += 1
        tok = (key, self.cnt[key])
        self._commit(tok, reads, writes)
        self.q[eng].append((waits, fn, (key, 1)))
        self.nwaits += len(waits)
        self.nops += 1
        return tok

    def dma(self, qeng, fn, reads=(), writes=()):
        k = self.dma_rr
        self.dma_rr = (self.dma_rr + 1) % self.ndma
        key = "dma%d" % k
        waits = self._deps(qeng, reads, writes, True)
        prev = self.cnt[key]
        if prev > 0 and self.known[qeng].get(key, 0) < prev:
            waits[key] = max(waits.get(key, 0), prev)
            self.known[qeng][key] = prev
        self.cnt[key] += 16
        tok = (key, self.cnt[key])
        self._commit(tok, reads, writes)
        self.q[qeng].append((waits, fn, (key, 16)))
        self.nwaits += len(waits)
        self.nops += 1
        return tok

    def wait_tokens(self, eng, toks):
        waits = {}
        for t in toks:
            self._need(eng, t, waits)
        for k, v in waits.items():
            self.known[eng][k] = v
        self.q[eng].append((waits, None, None))

    def emit(self):
        nc = self.nc
        with nc.Block() as block:
            def mk(ename):
                def body(e):
                    for waits, fn, inc in self.q[ename]:
                        for k, v in waits.items():
                            e.wait_ge(self.sem[k], v)
                        if fn is not None:
                            ins = fn(e)
                            ins.then_inc(self.sem[inc[0]], inc[1])
                return body
            block.tensor(mk("pe"))
            block.scalar(mk("act"))
            block.vector(mk("dve"))
            block.gpsimd(mk("pool"))
            block.sync(mk("sp"))


WK_COLS = 3392
WQ_COLS = 2820
K_SLABS = [("a", 0, 320)] + [("bk%d" % g, 320 + 512 * g, 512) for g in range(3)] + \
          [("bv%d" % g, 1856 + 512 * g, 512) for g in range(3)]
Q_SLABS = [("aq", 0, 512), ("iq", 512, 260)] + [("bq%d" % g, 772 + 512 * g, 512) for g in range(3)] + \
          [("mq", 2308, 512)]


def build(S, QB, dbg=False, stop=None):
    NBLK = S // 128
    NQ = NBLK // 2
    NIT = NQ // QB
    N = QB * 128
    assert NBLK % 4 == 0 and NQ % QB == 0

    nc = bass.Bass("TRN2", target_bir_lowering=False)

    def din(name, shape, dt=F32):
        return nc.dram_tensor(name, shape, dt, kind="ExternalInput").ap()

    def dscr(name, shape, dt=BF16):
        return nc.dram_tensor(name, shape, dt).ap()

    xs = din("xs", [S, D])
    cs = din("cs", [S, 64])
    memx = din("memx", [256, D])
    wk = din("wk", [D, WK_COLS])
    wq = din("wq", [D, WQ_COLS])
    wg = din("wg", [D, 3072])
    wbr = din("wbr", [1536, D])
    wo = din("wo", [D, D])
    wmem = din("wmem", [D, 1024])
    w1 = din("w1", [16 * D, 512])
    w3 = din("w3", [16 * D, 512])
    w2 = din("w2", [16 * 512, D])
    wr = din("wr", [D, 20])
    gcols_d = din("gcols", [128, 24])
    bgate_d = din("bgate", [128, 24])
    gfin_d = din("gfin", [128, D])
    rbias_d = din("rbias", [128, 20])
    ident_d = din("ident", [128, 128])
    cm_d = din("dsa_cm", [128, 256])
    m0_d = din("dsa_m0", [128, 128])
    mb_d = din("mbias", [128, 24 * 128])
    mb0_d = din("mb0", [128, 24 * 128])
    out_d = nc.dram_tensor("out", [NQ * 128, D], F32, kind="ExternalOutput").ap()
    dbg_d = {}
    if dbg:
        for nm, shp in (("d_ya", [64, 8 * N]), ("d_yb", [64, 8 * N]), ("d_ym", [128, 4 * N]), ("d_h", [128, QB * D])):
            dbg_d[nm] = nc.dram_tensor(nm, shp, F32, kind="ExternalOutput").ap()

    wk_b = dscr("wk_b", [D, WK_COLS])
    wq_b = dscr("wq_b", [D, WQ_COLS])
    wg_b = dscr("wg_b", [D, 3072])
    wbr_b = dscr("wbr_b", [1536, D])
    wo_b = dscr("wo_b", [D, D])
    wmem_b = dscr("wmem_b", [D, 1024])
    w1_b = dscr("w1_b", [16 * D, 512])
    w3_b = dscr("w3_b", [16 * D, 512])
    w2_b = dscr("w2_b", [16 * 512, D])
    KaT_d = dscr("KaT_d", [128, S])
    kiT_d = dscr("kiT_d", [64, S])
    Va_d = dscr("Va_d", [S, 256])
    bkT_d = dscr("bkT_d", [128, 3, 4, S])
    Vb_d = dscr("Vb_d", [S, 3, 1024])

    P = Prog(nc)
    es = contextlib.ExitStack()
    with es:
        def sb(name, shape, dt=F32):
            return es.enter_context(nc.sbuf_tensor("s_" + name, shape, dt))

        pbank = [es.enter_context(nc.psum_tensor("pb%d" % i, [128, 512], F32)) for i in range(8)]
        Rb = [Res(excl=True) for _ in range(8)]
        rot_state = [0]

        def rot():
            k = 4 + rot_state[0]
            rot_state[0] = (rot_state[0] + 1) % 4
            return pbank[k], Rb[k]

        identf = sb("identf", [128, 128]); identb = sb("identb", [128, 128], BF16)
        i4b = sb("i4b", [128, 512], BF16)
        ones_b = sb("ones_b", [128, 128], BF16)
        gcols = sb("gcols", [128, 24]); bgate = sb("bgate", [128, 24])
        gfin = sb("gfin", [128, D]); rbias = sb("rbias", [128, 20])
        cm = sb("cm", [128, 256]); m0 = sb("m0", [128, 128])
        mbt = sb("mbt", [128, 24 * 128], BF16); mb0t = sb("mb0t", [128, 24 * 128], BF16)
        pow2 = sb("pow2", [128, NBIS + 1])
        wr32 = sb("wr32", [128, 8, 20])
        kmT = sb("kmT", [128, 4, 256], BF16); vmb = sb("vmb", [128, 2, 512], BF16)
        Rc = Res()

        def ld_const(dst, src):
            P.dma("sp", lambda e: e.dma_start(out=dst, in_=src), writes=[Rc])

        ld_const(identf[:], ident_d)
        ld_const(gcols[:], gcols_d); ld_const(bgate[:], bgate_d); ld_const(gfin[:], gfin_d)
        ld_const(rbias[:], rbias_d); ld_const(cm[:], cm_d); ld_const(m0[:], m0_d)
        ld_const(wr32[:], wr.rearrange("(c p) n -> p c n", p=128))
        P.op("dve", lambda e: e.tensor_copy(out=identb[:], in_=identf[:]), reads=[Rc], writes=[Rc])
        for j in range(4):
            P.op("dve", lambda e, j=j: e.tensor_copy(out=i4b[:, j * 128:(j + 1) * 128], in_=identf[:]), reads=[Rc], writes=[Rc])
        P.op("dve", lambda e: e.memset(ones_b[:], 1.0), writes=[Rc])
        for i in range(NBIS + 1):
            P.op("dve", lambda e, i=i: e.memset(pow2[:, i:i + 1], 2.0 ** (-i)), writes=[Rc])

        def finish0():
            toks = [("dma%d" % k, P.cnt["dma%d" % k]) for k in range(P.ndma) if P.cnt["dma%d" % k] > 0]
            toks += [(P.cur[e_][0], P.cnt[P.cur[e_][0]]) for e_ in ("pe", "act", "dve", "pool") if P.cnt[P.cur[e_][0]] > 0]
            P.wait_tokens("sp", toks)
            P.emit()
        if stop == "c":
            finish0(); return nc, P
        Rw_b = Res()
        conv_hist = []

        def convert(src, dst):
            fs = src.rearrange("r c -> (r c)").rearrange("(n k) -> n k", k=1024)
            fd = dst.rearrange("r c -> (r c)").rearrange("(n k) -> n k", k=1024)
            n = fs.shape[0]
            for r0 in range(0, n, 1024):
                r1_ = min(n, r0 + 1024)
                if len(conv_hist) >= 4:
                    P.wait_tokens("pool", [conv_hist[-4]])
                conv_hist.append(P.dma("pool", lambda e, r0=r0, r1_=r1_: e.dma_start(out=fd[r0:r1_, :], in_=fs[r0:r1_, :])))

        for s_, d_ in ((wmem, wmem_b), (wk, wk_b), (wq, wq_b), (wg, wg_b), (wbr, wbr_b), (wo, wo_b),
                       (w1, w1_b), (w3, w3_b), (w2, w2_b)):
            convert(s_, d_)
        conv_tokens = []
        for k in range(P.ndma):
            key = "dma%d" % k
            if P.cnt[key] > 0:
                conv_tokens.append((key, P.cnt[key]))

        if stop == "conv":
            finish0(); return nc, P
        ss_t = sb("ss_t", [128, 4]); rs_t = sb("rs_t", [128, 4])
        sqj = sb("sqj", [128, D], BF16)
        xb_t = [sb("xb0", [128, D], BF16)] * 2
        Rstat = [Res() for _ in range(4)]
        Rsqj = Res()
        Rxb = [Res()] * 2
        rms_ctr = [0]

        def rms_T(x_ap, Rx, gofs, dst_fn, Rdst):
            i = rms_ctr[0] % 4
            j = rms_ctr[0] % 2
            rms_ctr[0] += 1
            ss = ss_t[:, i:i + 1]; rs = rs_t[:, i:i + 1]
            P.op("act", lambda e: e.activation(out=sqj[:], in_=x_ap, func=AF.Square, accum_out=ss),
                 reads=[Rx], writes=[Rsqj, Rstat[i]])
            if RMS_CUT <= 1:
                return i
            P.op("dve", lambda e: e.tensor_scalar(out=rs, in0=ss, scalar1=1.0 / D, scalar2=1e-6, op0=ALU.mult, op1=ALU.add),
                 reads=[Rstat[i]], writes=[Rstat[i]])
            P.op("act", lambda e: e.activation(out=rs, in_=rs, func=AF.Sqrt),
                 reads=[Rstat[i]], writes=[Rstat[i]])
            P.op("dve", lambda e: e.reciprocal(out=rs, in_=rs), reads=[Rstat[i]], writes=[Rstat[i]])
            if RMS_CUT <= 2:
                return i
            xb = xb_t[j]
            P.op("dve", lambda e: e.tensor_scalar(out=xb[:], in0=x_ap, scalar1=rs, scalar2=None, op0=ALU.mult),
                 reads=[Rx, Rstat[i]], writes=[Rxb[j]])
            if RMS_CUT <= 3:
                return i
            for half in range(2):
                pb, Rp = rot()
                pbb = pb[:].bitcast(BF16)
                for c4 in range(4):
                    c = half * 4 + c4
                    P.op("pe", lambda e, c=c, c4=c4, pbb=pbb: e.transpose(out=pbb[:, c4 * 128:(c4 + 1) * 128], in_=xb[:, c * 128:(c + 1) * 128], identity=identb[:]),
                         reads=[Rxb[j], Rc], writes=[Rp])
                if RMS_CUT <= 4:
                    continue
                for c4 in range(4):
                    c = half * 4 + c4
                    eng = "dve"
                    if eng == "dve":
                        P.op("dve", lambda e, c=c, c4=c4, pbb=pbb: e.tensor_scalar(out=dst_fn(c), in0=pbb[:, c4 * 128:(c4 + 1) * 128], scalar1=gcols[:, gofs + c:gofs + c + 1], scalar2=None, op0=ALU.mult),
                             reads=[Rp, Rc], writes=[Rdst])
                    else:
                        P.op("act", lambda e, c=c, c4=c4, pbb=pbb: e.activation(out=dst_fn(c), in_=pbb[:, c4 * 128:(c4 + 1) * 128], func=AF.Copy, scale=gcols[:, gofs + c:gofs + c + 1]),
                             reads=[Rp, Rc], writes=[Rdst])
            return i

        rt = [sb("rt%d" % i, [128, 256]) for i in range(4)]
        Rrt = [Res() for _ in range(4)]

        def rope(ps_ap, Rps, H, cs_ap, Rcs, out_ap, Rout, perm=False):
            n = H * 32
            if os.environ.get("NOROPE"):
                P.op("act", lambda e: e.copy(out=out_ap, in_=ps_ap), reads=[Rps], writes=[Rout])
                return
            if perm:
                x = ps_ap.rearrange("p (g j t d) -> p g j t d", g=2, t=2, d=32)
                o = out_ap.rearrange("p (j g t d) -> p g j t d", g=2, t=2, d=32)
                x1, x2 = x[:, :, :, 0, :], x[:, :, :, 1, :]
                o1, o2 = o[:, :, :, 0, :], o[:, :, :, 1, :]
                cos = cs_ap[:, 0:32].unsqueeze(1).unsqueeze(1).to_broadcast([128, 2, 4, 32])
                sin = cs_ap[:, 32:64].unsqueeze(1).unsqueeze(1).to_broadcast([128, 2, 4, 32])
                tv = [rt[i][:, 0:n].rearrange("p (g j d) -> p g j d", g=2, d=32) for i in range(4)]
            else:
                x = ps_ap.rearrange("p (h t d) -> p h t d", t=2, d=32)
                o = out_ap.rearrange("p (h t d) -> p h t d", t=2, d=32)
                x1, x2 = x[:, :, 0, :], x[:, :, 1, :]
                o1, o2 = o[:, :, 0, :], o[:, :, 1, :]
                cos = cs_ap[:, 0:32].unsqueeze(1).to_broadcast([128, H, 32])
                sin = cs_ap[:, 32:64].unsqueeze(1).to_broadcast([128, H, 32])
                tv = [rt[i][:, 0:n].rearrange("p (h d) -> p h d", d=32) for i in range(4)]
            P.op("dve", lambda e: e.tensor_tensor(out=tv[0], in0=x1, in1=cos, op=ALU.mult), reads=[Rps, Rcs], writes=[Rrt[0]])
            P.op("dve", lambda e: e.tensor_tensor(out=tv[1], in0=x2, in1=sin, op=ALU.mult), reads=[Rps, Rcs], writes=[Rrt[1]])
            P.op("dve", lambda e: e.tensor_tensor(out=tv[2], in0=x2, in1=cos, op=ALU.mult), reads=[Rps, Rcs], writes=[Rrt[2]])
            P.op("dve", lambda e: e.tensor_tensor(out=tv[3], in0=x1, in1=sin, op=ALU.mult), reads=[Rps, Rcs], writes=[Rrt[3]])
            P.op("dve", lambda e: e.tensor_tensor(out=o1, in0=tv[0], in1=tv[1], op=ALU.subtract), reads=[Rrt[0], Rrt[1]], writes=[Rout])
            P.op("dve", lambda e: e.tensor_tensor(out=o2, in0=tv[2], in1=tv[3], op=ALU.add), reads=[Rrt[2], Rrt[3]], writes=[Rout])

        wslab = [sb("wslab%d" % i, [128, 8, 512], BF16) for i in range(2)]
        Rwslab = [Res(), Res()]
        slab_ctr = [0]

        def load_slab(wsrc_b, c0, ncol):
            i = slab_ctr[0] % 2
            slab_ctr[0] += 1
            src = wsrc_b.rearrange("(c p) n -> p c n", p=128)[:, :, c0:c0 + ncol]
            P.dma("sp", lambda e: e.dma_start(out=wslab[i][:, :, 0:ncol], in_=src), reads=[Rw_b], writes=[Rwslab[i]])
            return wslab[i], Rwslab[i]

        def proj(nT, RnT, tok0, ws, Rws, ncol):
            pb, Rp = rot()
            for c in range(8):
                P.op("pe", lambda e, c=c: e.matmul(pb[:, 0:ncol], lhsT=nT[:, c, tok0:tok0 + 128], rhs=ws[:, c, 0:ncol], start=(c == 0), stop=(c == 7)),
                     reads=[RnT, Rws], writes=[Rp])
            flush_deferred()
            return pb, Rp

        tst = [sb("tst%d" % i, [128, 512], BF16) for i in range(2)]
        Rtst = [Res(), Res()]
        tst_ctr = [0]

        def next_tst():
            i = tst_ctr[0] % 2
            tst_ctr[0] += 1
            return tst[i], Rtst[i]

        deferred = []

        def flush_deferred():
            while deferred:
                deferred.pop(0)()

        def transposes(*a_, **k_):
            deferred.append(lambda: _transposes(*a_, **k_))

        def _transposes(src, Rsrc, ncols_each, nchunks, dst_fn, Rdst, eng="act"):
            pb, Rp = rot()
            pbb = pb[:].bitcast(BF16)
            for c in range(nchunks):
                P.op("pe", lambda e, c=c: e.transpose(out=pbb[0:ncols_each, c * 128:(c + 1) * 128], in_=src[:, c * ncols_each:(c + 1) * ncols_each], identity=identb[:]),
                     reads=[Rsrc, Rc], writes=[Rp])
            for c in range(nchunks):
                if eng == "act":
                    P.op("act", lambda e, c=c: e.copy(out=dst_fn(c), in_=pbb[0:ncols_each, c * 128:(c + 1) * 128]), reads=[Rp], writes=[Rdst])
                else:
                    P.op("dve", lambda e, c=c: e.tensor_copy(out=dst_fn(c), in_=pbb[0:ncols_each, c * 128:(c + 1) * 128]), reads=[Rp], writes=[Rdst])

        for e_ in ("sp", "act", "dve", "pe", "pool"):
            P.wait_tokens(e_, conv_tokens)

        xt = [sb("xt%d" % i, [128, D]) for i in range(2)]
        Rxt = [Res(), Res()]
        cst = [sb("cst%d" % i, [128, 64]) for i in range(4)]
        Rcst = [Res() for _ in range(4)]
        nTk = sb("nTk", [128, 8, 512], BF16)
        RnTk = Res()
        pcs = 0
        for (src, dst) in ((mb_d, mbt), (mb0_d, mb0t)):
            for pc in range(3):
                jx = pcs % 2; pcs += 1
                P.dma("sp", lambda e, src=src, pc=pc, jx=jx: e.dma_start(out=xt[jx][:], in_=src[:, pc * 1024:(pc + 1) * 1024]), writes=[Rxt[jx]])
                P.op("dve", lambda e, dst=dst, pc=pc, jx=jx: e.tensor_copy(out=dst[:, pc * 1024:(pc + 1) * 1024], in_=xt[jx][:]), reads=[Rxt[jx]], writes=[Rc])
        if stop == "m1":
            finish0(); return nc, P
        for mbk in range(2):
            P.dma("sp", lambda e, mbk=mbk: e.dma_start(out=xt[mbk][:], in_=memx[mbk * 128:(mbk + 1) * 128, :]), writes=[Rxt[mbk]])
            rms_T(xt[mbk][:], Rxt[mbk], 8, lambda c, mbk=mbk: nTk[:, c, mbk * 128:(mbk + 1) * 128], RnTk)
        if stop == "m2":
            finish0(); return nc, P
        Rkm = Res()
        for half in range(2):
            ws, Rws = load_slab(wmem_b, half * 512, 512)
            for mbk in range(2):
                pb, Rp = proj(nTk, RnTk, mbk * 128, ws, Rws, 512)
                if half == 0:
                    t_, Rt_ = next_tst()
                    P.op("act", lambda e, t_=t_, pb=pb: e.copy(out=t_[:], in_=pb[:]), reads=[Rp], writes=[Rt_])
                    transposes(t_, Rt_, 128, 4, lambda c, mbk=mbk: kmT[:, c, mbk * 128:(mbk + 1) * 128], Rkm)
                else:
                    P.op("act", lambda e, pb=pb, mbk=mbk: e.copy(out=vmb[:, mbk, :], in_=pb[:]), reads=[Rp], writes=[Rkm])

        def finish():
            toks = [("dma%d" % k, P.cnt["dma%d" % k]) for k in range(P.ndma) if P.cnt["dma%d" % k] > 0]
            toks += [(P.cur[e_][0], P.cnt[P.cur[e_][0]]) for e_ in ("pe", "act", "dve", "pool") if P.cnt[P.cur[e_][0]] > 0]
            P.wait_tokens("sp", toks)
            P.emit()

        flush_deferred()
        if stop == "p0":
            finish(); return nc, P
        r1 = sb("r1", [128, 24576], BF16)
        KaTst = r1[:, 0:512]; kiTst = r1[0:64, 512:1024]
        Vast = r1[:, 1024:2048].rearrange("p (b n) -> p b n", b=4)
        bkTst = [r1[:, 2048 + i * 2048:2048 + (i + 1) * 2048].rearrange("p (c n) -> p c n", c=4) for i in range(2)]
        Vbst = [r1[:, 6144 + i * 4096:6144 + (i + 1) * 4096].rearrange("p (b n) -> p b n", b=4) for i in range(2)]
        RKaTst, RkiTst, RVast = Res(), Res(), Res()
        RbkTst = [Res(), Res()]; RVbst = [Res(), Res()]
        RKd = Res()
        P.op("pool", lambda e: e.memset(Vast, 1.0), writes=[RVast])
        for i in range(2):
            P.op("pool", lambda e, i=i: e.memset(Vbst[i], 1.0), writes=[RVbst[i]])

        for T in range(NBLK // 4):
            for bl in range(4):
                sbk = T * 4 + bl
                j = sbk % 2
                P.dma("sp", lambda e, sbk=sbk, j=j: e.dma_start(out=xt[j][:], in_=xs[sbk * 128:(sbk + 1) * 128, :]), writes=[Rxt[j]])
                P.dma("sp", lambda e, sbk=sbk, bl=bl: e.dma_start(out=cst[bl][:], in_=cs[sbk * 128:(sbk + 1) * 128, :]), writes=[Rcst[bl]])
                rms_T(xt[j][:], Rxt[j], 0, lambda c, bl=bl: nTk[:, c, bl * 128:(bl + 1) * 128], RnTk)
            for si, (nm, c0, ncol) in enumerate(K_SLABS):
                ws, Rws = load_slab(wk_b, c0, ncol)
                for bl in range(4):
                    pb, Rp = proj(nTk, RnTk, bl * 128, ws, Rws, ncol)
                    if nm == "a":
                        t_, Rt_ = next_tst()
                        rope(pb[:, 0:192], Rp, 3, cst[bl][:], Rcst[bl], t_[:, 0:192], Rt_)
                        Vv = Vast[:, bl, :].rearrange("p (g t d) -> p g t d", g=2, t=2)[:, :, 0, :]
                        P.op("act", lambda e, pb=pb, Vv=Vv: e.copy(out=Vv, in_=pb[:, 192:320].rearrange("p (g d) -> p g d", g=2)), reads=[Rp], writes=[RVast])
                        transposes(t_, Rt_, 128, 1, lambda c, bl=bl: KaTst[:, bl * 128:(bl + 1) * 128], RKaTst)
                        def ki_tr(t_=t_, Rt_=Rt_, bl=bl):
                            pb2, Rp2 = rot()
                            pbb2 = pb2[:].bitcast(BF16)
                            P.op("pe", lambda e: e.transpose(out=pbb2[0:64, 0:128], in_=t_[:, 128:192], identity=identb[:]), reads=[Rt_, Rc], writes=[Rp2])
                            P.op("act", lambda e: e.copy(out=kiTst[:, bl * 128:(bl + 1) * 128], in_=pbb2[0:64, 0:128]), reads=[Rp2], writes=[RkiTst])
                        deferred.append(ki_tr)
                    elif nm.startswith("bk"):
                        g = int(nm[2]); jb = g % 2
                        t_, Rt_ = next_tst()
                        rope(pb[:, 0:512], Rp, 8, cst[bl][:], Rcst[bl], t_[:, 0:512], Rt_)
                        transposes(t_, Rt_, 128, 4, lambda c, bl=bl, jb=jb: bkTst[jb][:, c, bl * 128:(bl + 1) * 128], RbkTst[jb])
                    else:
                        g = int(nm[2]); jb = g % 2
                        Vv = Vbst[jb][:, bl, :].rearrange("p (h t d) -> p h t d", h=8, t=2)[:, :, 0, :]
                        P.op("act", lambda e, pb=pb, Vv=Vv: e.copy(out=Vv, in_=pb[:, 0:512].rearrange("p (h d) -> p h d", h=8)), reads=[Rp], writes=[RVbst[jb]])
                def stores(nm=nm, T=T):
                    if nm == "a":
                        P.dma("pool", lambda e, T=T: e.dma_start(out=KaT_d[:, T * 512:(T + 1) * 512], in_=KaTst), reads=[RKaTst])
                        P.dma("pool", lambda e, T=T: e.dma_start(out=kiT_d[:, T * 512:(T + 1) * 512], in_=kiTst), reads=[RkiTst])
                        P.dma("pool", lambda e, T=T: e.dma_start(out=Va_d[T * 512:(T + 1) * 512, :].rearrange("(b p) n -> p b n", p=128), in_=Vast), reads=[RVast])
                    elif nm.startswith("bk"):
                        g = int(nm[2]); jb = g % 2
                        P.dma("pool", lambda e, T=T, g=g, jb=jb: e.dma_start(out=bkT_d[:, g, :, T * 512:(T + 1) * 512], in_=bkTst[jb]), reads=[RbkTst[jb]])
                    else:
                        g = int(nm[2]); jb = g % 2
                        P.dma("pool", lambda e, T=T, g=g, jb=jb: e.dma_start(out=Vb_d[T * 512:(T + 1) * 512, g, :].rearrange("(b p) n -> p b n", p=128), in_=Vbst[jb]), reads=[RVbst[jb]])
                deferred.append(stores)
        flush_deferred()
        kd_tokens = [("dma%d" % k, P.cnt["dma%d" % k]) for k in range(P.ndma) if P.cnt["dma%d" % k] > 0]
        P.wait_tokens("sp", kd_tokens)

        if stop == "p1":
            finish(); return nc, P
        xh = sb("xh", [128, QB, D]); Rxh = [Res() for _ in range(QB)]
        nTq = nTk[:, :, 0:N]; RnTq = RnTk
        csq = [sb("csq%d" % i, [128, 64]) for i in range(QB)]; Rcsq = [Res() for _ in range(QB)]
        QaT = [sb("QaT%d" % i, [128, 4, 128], BF16) for i in range(QB)]; RQaT = [Res() for _ in range(QB)]
        iqT = [sb("iqT%d" % i, [64, 4, 128], BF16) for i in range(QB)]; RiqT = [Res() for _ in range(QB)]
        bqT = [sb("bqT%d" % i, [128, 3, 4, 128], BF16) for i in range(QB)]; RbqT = [Res() for _ in range(QB)]
        mqT = [sb("mqT%d" % i, [128, 4, 128], BF16) for i in range(QB)]; RmqT = [Res() for _ in range(QB)]
        wv = [sb("wv%d" % i, [128, 12]) for i in range(QB)]; Rwv = [Res() for _ in range(QB)]
        dg = [sb("dg%d" % i, [128, 4, 128], BF16) for i in range(QB)]; Rdg = [Res() for _ in range(QB)]
        yaT = sb("yaT", [64, 8, N], BF16); ybT = sb("ybT", [64, 8, N], BF16); ymT = sb("ymT", [128, 4, N], BF16)
        RyaT, RybT, RymT = Res(), Res(), Res()
        score = r1[:, 0:16384].bitcast(F32)
        junk = r1[:, 16384:24576]
        Rsc, Rjk = Res(), Res()
        Rmw = [Res(), Res()]
        kib = [sb("kib%d" % i, [64, 512], BF16) for i in range(2)]; Rkib = [Res(), Res()]
        Rh = [sb("Rh%d" % i, [128, 512], BF16) for i in range(4)]; RRh = [Res() for _ in range(4)]
        bis = sb("bis", [128, 8 + NBIS + 1]); Rbis = Res()
        nmb = [sb("nmb%d" % i, [128, 512], BF16) for i in range(2)]; Rnmb = [Res(), Res()]
        kab = [sb("kab%d" % i, [128, 512], BF16) for i in range(2)]; Rkab = [Res(), Res()]
        vab = [sb("vab%d" % i, [128, 4, 256], BF16) for i in range(2)]; Rvab = [Res(), Res()]
        pt = [sb("pt%d" % i, [128, 512], BF16) for i in range(4)]; Rpt = [Res() for _ in range(4)]
        pt_ctr = [0]
        rden = sb("rden", [128, 512]); Rrden = Res()
        NBB = 3
        bkb = [sb("bkb%d" % i, [128, 4, 128], BF16) for i in range(NBB)]; Rbkb = [Res() for _ in range(NBB)]
        vbb = [sb("vbb%d" % i, [128, 1024], BF16) for i in range(NBB)]; Rvbb = [Res() for _ in range(NBB)]
        bb_ctr = [0]
        gt = [sb("gt%d" % i, [128, N]) for i in range(3)]; Rgt = [Res() for _ in range(3)]
        wgs = [sb("wgs0", [128, 8, 384], BF16)] * 2; Rwgs = [Res()] * 2
        wba = [sb("wba0", [64, 16, 128], BF16)] * 2; Rwba = [Res()] * 2
        wbm = [sb("wbm0", [128, 4, 128], BF16)] * 2; Rwbm = [Res()] * 2
        mtmp = [sb("mtmp%d" % i, [128, N]) for i in range(3)]; Rmtmp = [Res() for _ in range(3)]
        mT = sb("mT", [128, 8, N], BF16); RmT = Res()
        xnT = mT; RxnT = RmT
        xf32 = xt[0]; Rxf32 = Rxt[0]
        xnT32 = xt[1][:].rearrange("p (c n) -> p c n", c=8); RxnT32 = Rxt[1]
        rl = sb("rl", [128, 64]); Rrl = Res()
        comb = [sb("comb%d" % i, [128, 16]) for i in range(QB)]; Rcomb = [Res() for _ in range(QB)]
        sA = [sb("sA%d" % i, [128, N], BF16) for i in range(2)]; RsA = [Res(), Res()]
        hT = [sb("hT%d" % i, [128, 4, N], BF16) for i in range(2)]; RhT = [Res(), Res()]
        ot = xf32; Rot = Rxf32
        out_tokens = []

        def next_pt():
            i = pt_ctr[0] % 4
            pt_ctr[0] += 1
            return pt[i], Rpt[i]

        def dsa(t, sq, part):
            nkb = sq + 1
            nk = nkb * 128
            nch = (nk + 511) // 512
            aw = wv[t][:, 4:8]
            am = bis[:, 0:1]; w0 = bis[:, 1:2]; mid = bis[:, 2:3]; cnt = bis[:, 3:4]; sg = bis[:, 4:5]; thr = bis[:, 5:6]
            wt2 = bis[:, 8:8 + NBIS + 1]
            if part == 0:
                for ch in range(nch):
                    ncol = min(512, nk - ch * 512)
                    kb_ = kib[ch % 2]
                    P.dma("sp", lambda e, ch=ch, ncol=ncol, kb_=kb_: e.dma_start(out=kb_[:, 0:ncol], in_=kiT_d[:, ch * 512:ch * 512 + ncol]), reads=[RKd], writes=[Rkib[ch % 2]])
                    for h in range(4):
                        pb, Rp = rot()
                        P.op("pe", lambda e, pb=pb, h=h, ncol=ncol, kb_=kb_: e.matmul(pb[:, 0:ncol], lhsT=iqT[t][:, h, :], rhs=kb_[:, 0:ncol], start=True, stop=True),
                             reads=[RiqT[t], Rkib[ch % 2]], writes=[Rp])
                        P.op("act", lambda e, pb=pb, h=h, ncol=ncol: e.activation(out=Rh[h][:, 0:ncol], in_=pb[:, 0:ncol], func=AF.Relu, scale=aw[:, h:h + 1]),
                             reads=[Rp, Rwv[t]], writes=[RRh[h]])
                    pb, Rp = rot()
                    for h in range(4):
                        P.op("pe", lambda e, pb=pb, h=h, ncol=ncol: e.matmul(pb[:, 0:ncol], lhsT=dg[t][:, h, :], rhs=Rh[h][:, 0:ncol], start=(h == 0), stop=(h == 3)),
                             reads=[Rdg[t], RRh[h]], writes=[Rp])
                    P.op("dve", lambda e, pb=pb, ch=ch, ncol=ncol: e.tensor_copy(out=score[:, ch * 512:ch * 512 + ncol], in_=pb[:, 0:ncol]),
                         reads=[Rp], writes=[Rsc, Rmw[0], Rmw[1]])
                P.op("dve", lambda e: e.tensor_reduce(out=am, in_=score[:, 0:nk], axis=AX, op=ALU.max, apply_absolute_value=True), reads=[Rsc], writes=[Rbis])
                P.op("dve", lambda e: e.tensor_tensor(out=score[:, nk - 256:nk], in0=score[:, nk - 256:nk], in1=cm[:], op=ALU.add), reads=[Rsc, Rc, Rbis], writes=[Rsc])
                P.op("dve", lambda e: e.tensor_tensor(out=score[:, 0:128], in0=score[:, 0:128], in1=m0[:], op=ALU.add), reads=[Rsc, Rc], writes=[Rsc])
                P.op("dve", lambda e: e.tensor_scalar(out=w0, in0=am, scalar1=1.001, scalar2=1e-6, op0=ALU.mult, op1=ALU.add), reads=[Rbis], writes=[Rbis])
                P.op("dve", lambda e: e.tensor_scalar(out=wt2, in0=pow2[:], scalar1=w0, scalar2=None, op0=ALU.mult), reads=[Rbis, Rc], writes=[Rbis])
                P.op("dve", lambda e: e.memset(mid, 0.0), writes=[Rbis])
                for it in range(NBIS):
                    P.op("dve", lambda e: e.tensor_scalar(out=junk[:, 0:nk], in0=score[:, 0:nk], scalar1=mid, scalar2=None, op0=ALU.is_ge, op1=ALU.add, accum_out=cnt),
                         reads=[Rsc, Rbis], writes=[Rjk, Rbis, Rmw[1]])
                    P.op("dve", lambda e: e.tensor_scalar(out=sg, in0=cnt, scalar1=255.5, scalar2=0.5, op0=ALU.is_ge, op1=ALU.subtract), reads=[Rbis], writes=[Rbis])
                    P.op("dve", lambda e, it=it: e.scalar_tensor_tensor(out=mid, in0=sg, scalar=wt2[:, it:it + 1], in1=mid, op0=ALU.mult, op1=ALU.add), reads=[Rbis], writes=[Rbis])
                P.op("dve", lambda e: e.tensor_tensor(out=thr, in0=mid, in1=wt2[:, NBIS:NBIS + 1], op=ALU.subtract), reads=[Rbis], writes=[Rbis])
                return
            def chunk_loads(k4):
                ncol = min(512, nk - k4 * 512)
                nb_here = ncol // 128
                j = k4 % 2
                P.op("dve", lambda e: e.tensor_scalar(out=nmb[j][:, 0:ncol], in0=score[:, k4 * 512:k4 * 512 + ncol], scalar1=thr, scalar2=NEG, op0=ALU.is_lt, op1=ALU.mult),
                     reads=[Rsc, Rbis], writes=[Rnmb[j]])
                P.dma("sp", lambda e: e.dma_start(out=kab[j][:, 0:ncol], in_=KaT_d[:, k4 * 512:k4 * 512 + ncol]), reads=[RKd], writes=[Rkab[j]])
                P.dma("sp", lambda e: e.dma_start(out=vab[j][:, 0:nb_here, :], in_=Va_d[k4 * 512:k4 * 512 + nb_here * 128, :].rearrange("(b p) n -> p b n", p=128)), reads=[RKd], writes=[Rvab[j]])

            items = []
            for k4 in range(nch):
                ncol = min(512, nk - k4 * 512)
                for b in range(ncol // 128):
                    for g in range(2):
                        items.append((k4, b, g))
            last_of_chunk = {}
            for idx, (k4, b, g) in enumerate(items):
                last_of_chunk[k4] = idx
            stage = {}

            def s_stage(idx):
                k4, b, g = items[idx]
                j = k4 % 2
                pb, Rp = rot()
                P.op("pe", lambda e: e.matmul(pb[:], lhsT=kab[j][g * 64:(g + 1) * 64, b * 128:(b + 1) * 128], rhs=QaT[t][g * 64:(g + 1) * 64, :, :].rearrange("p j q -> p (j q)"), start=True, stop=False),
                     reads=[Rkab[j], RQaT[t]], writes=[Rp])
                P.op("pe", lambda e: e.matmul(pb[:], lhsT=nmb[j][:, b * 128:(b + 1) * 128], rhs=i4b[:], start=False, stop=True),
                     reads=[Rnmb[j], Rc], writes=[Rp])
                stage[idx] = (pb, Rp)

            def e_stage(idx):
                pb, Rp = stage[idx]
                p_, Rp_ = next_pt()
                P.op("act", lambda e: e.activation(out=p_[:], in_=pb[:], func=AF.Exp, scale=0.125), reads=[Rp], writes=[Rp_])
                stage[idx] = (p_, Rp_)

            def v_stage(idx):
                k4, b, g = items[idx]
                j = k4 % 2
                kb = k4 * 4 + b
                p_, Rp_ = stage.pop(idx)
                P.op("pe", lambda e: e.matmul(pbank[g][:], lhsT=vab[j][:, b, g * 128:(g + 1) * 128], rhs=p_[:], start=(kb == 0), stop=(kb == nkb - 1)),
                     reads=[Rvab[j], Rp_], writes=[Rb[g]])

            chunk_loads(0)
            if nch > 1:
                chunk_loads(1)
            nit = len(items)
            s_stage(0)
            if nit > 1:
                s_stage(1)
            for idx in range(nit):
                e_stage(idx)
                if idx + 2 < nit:
                    s_stage(idx + 2)
                v_stage(idx)
                k4 = items[idx][0]
                if last_of_chunk[k4] == idx and k4 + 2 < nch:
                    chunk_loads(k4 + 2)
            for g in range(2):
                P.op("dve", lambda e, g=g: e.reciprocal(out=rden[0:64, :], in_=pbank[g][64:128, :]), reads=[Rb[g]], writes=[Rrden])
                P.op("dve", lambda e, g=g: e.tensor_tensor(out=yaT[:, g * 4:(g + 1) * 4, t * 128:(t + 1) * 128], in0=pbank[g][0:64, :].rearrange("p (j q) -> p j q", j=4), in1=rden[0:64, :].rearrange("p (j q) -> p j q", j=4), op=ALU.mult),
                     reads=[Rb[g], Rrden], writes=[RyaT])

        def bmix(t, sq):
            first = [True, True]
            work = []
            for g in range(3):
                for jj in range(B_NB[g], -1, -1):
                    kb = sq - jj
                    if kb >= 0:
                        work.append((g, jj, kb))
            nwork = len(work)
            bufof = {}

            def b_load(w):
                g, jj, kb = work[w]
                i = bb_ctr[0] % NBB
                bb_ctr[0] += 1
                bufof[w] = i
                P.dma("sp", lambda e: e.dma_start(out=bkb[i][:], in_=bkT_d[:, g, :, kb * 128:(kb + 1) * 128]), reads=[RKd], writes=[Rbkb[i]])
                P.dma("sp", lambda e: e.dma_start(out=vbb[i][:], in_=Vb_d[kb * 128:(kb + 1) * 128, g, :]), reads=[RKd], writes=[Rvbb[i]])

            items = [(w, hh) for w in range(nwork) for hh in range(2)]
            nit = len(items)
            stage = {}
            loaded = [0]

            def ensure_loaded(upto):
                while loaded[0] <= min(upto, nwork - 1):
                    b_load(loaded[0])
                    loaded[0] += 1

            def s_stage(idx):
                w, hh = items[idx]
                g, jj, kb = work[w]
                ensure_loaded(w + 1 if hh == 0 else w)
                i = bufof[w]
                mtab = mb0t if kb == 0 else mbt
                mi = B_MOFF[g] + jj
                pb, Rp = rot()
                P.op("pe", lambda e: e.matmul(pb[:], lhsT=mtab[:, mi * 128:(mi + 1) * 128], rhs=i4b[:], start=True, stop=False),
                     reads=[Rc], writes=[Rp])
                for p2 in range(4):
                    P.op("pe", lambda e, p2=p2: e.matmul(pb[:, p2 * 128:(p2 + 1) * 128], lhsT=bkb[i][hh * 64:(hh + 1) * 64, p2, :], rhs=bqT[t][hh * 64:(hh + 1) * 64, g, p2, :], start=False, stop=(p2 == 3), skip_group_check=True),
                         reads=[Rbkb[i], RbqT[t]], writes=[Rp])
                stage[idx] = (pb, Rp)

            def e_stage(idx):
                pb, Rp = stage[idx]
                p_, Rp_ = next_pt()
                P.op("act", lambda e: e.activation(out=p_[:], in_=pb[:], func=AF.Exp, scale=0.125), reads=[Rp], writes=[Rp_])
                stage[idx] = (p_, Rp_)

            def v_stage(idx):
                w, hh = items[idx]
                i = bufof[w]
                p_, Rp_ = stage.pop(idx)
                for h4 in range(4):
                    h = 2 * h4 + hh
                    st = first[hh]
                    first[hh] = False
                    P.op("pe", lambda e, h4=h4, h=h, st=st: e.matmul(pbank[2 + hh][:, h4 * 128:(h4 + 1) * 128], lhsT=vbb[i][:, h * 128:(h + 1) * 128], rhs=p_[:, h4 * 128:(h4 + 1) * 128], start=st, stop=(w == nwork - 1 and h4 == 3), skip_group_check=True),
                         reads=[Rvbb[i], Rp_], writes=[Rb[2 + hh]])

            s_stage(0)
            if nit > 1:
                s_stage(1)
            for idx in range(nit):
                e_stage(idx)
                if idx + 2 < nit:
                    s_stage(idx + 2)
                v_stage(idx)
            if BM_CUT <= 3:
                return
            for hh in range(2):
                P.op("dve", lambda e, hh=hh: e.reciprocal(out=rden[0:64, :], in_=pbank[2 + hh][64:128, :]), reads=[Rb[2 + hh]], writes=[Rrden])
                P.op("dve", lambda e, hh=hh: e.tensor_tensor(out=ybT[:, :, t * 128:(t + 1) * 128].rearrange("p (p2 hf) q -> p hf p2 q", hf=2)[:, hh], in0=pbank[2 + hh][0:64, :].rearrange("p (j q) -> p j q", j=4), in1=rden[0:64, :].rearrange("p (j q) -> p j q", j=4), op=ALU.mult),
                     reads=[Rb[2 + hh], Rrden], writes=[RybT])

        def memattn(t):
            sc = 128.0 ** -0.5
            for mbk in range(2):
                pb, Rp = rot()
                for h in range(4):
                    P.op("pe", lambda e, pb=pb, h=h, mbk=mbk: e.matmul(pb[:, h * 128:(h + 1) * 128], lhsT=kmT[:, h, mbk * 128:(mbk + 1) * 128], rhs=mqT[t][:, h, :], start=(h == 0), stop=(h == 3), skip_group_check=True),
                         reads=[Rkm, RmqT[t]], writes=[Rp])
                p_, Rp_ = next_pt()
                P.op("act", lambda e, pb=pb, p_=p_: e.activation(out=p_[:], in_=pb[:], func=AF.Exp, scale=sc), reads=[Rp], writes=[Rp_])
                for h in range(4):
                    P.op("pe", lambda e, h=h, mbk=mbk, p_=p_: e.matmul(pbank[0][:, h * 128:(h + 1) * 128], lhsT=vmb[:, mbk, h * 128:(h + 1) * 128], rhs=p_[:, h * 128:(h + 1) * 128], start=(mbk == 0 and h == 0), stop=(mbk == 1 and h == 3), skip_group_check=True),
                         reads=[Rkm, Rp_], writes=[Rb[0]])
                P.op("pe", lambda e, mbk=mbk, p_=p_: e.matmul(pbank[1][:], lhsT=ones_b[:], rhs=p_[:], start=(mbk == 0), stop=(mbk == 1)),
                     reads=[Rc, Rp_], writes=[Rb[1]])
            P.op("dve", lambda e: e.reciprocal(out=rden[:], in_=pbank[1][:]), reads=[Rb[1]], writes=[Rrden])
            P.op("dve", lambda e: e.tensor_tensor(out=ymT[:, :, t * 128:(t + 1) * 128], in0=pbank[0][:].rearrange("p (j q) -> p j q", j=4), in1=rden[:].rearrange("p (j q) -> p j q", j=4), op=ALU.mult),
                 reads=[Rb[0], Rrden], writes=[RymT])

        for I in range(NIT):
            for t in range(QB):
                sq = 2 * (I * QB + t) + 1
                P.dma("sp", lambda e, t=t, sq=sq: e.dma_start(out=xh[:, t, :], in_=xs[sq * 128:(sq + 1) * 128, :]), writes=[Rxh[t]])
                P.dma("sp", lambda e, t=t, sq=sq: e.dma_start(out=csq[t][:], in_=cs[sq * 128:(sq + 1) * 128, :]), writes=[Rcsq[t]])
                rms_T(xh[:, t, :], Rxh[t], 0, lambda c, t=t: nTq[:, c, t * 128:(t + 1) * 128], RnTq)
            for (nm, c0, ncol) in Q_SLABS:
                ws, Rws = load_slab(wq_b, c0, ncol)
                for t in range(QB):
                    pb, Rp = proj(nTq, RnTq, t * 128, ws, Rws, ncol)
                    t_, Rt_ = next_tst()
                    if nm == "aq":
                        rope(pb[:, 0:512], Rp, 8, csq[t][:], Rcsq[t], t_[:, 0:512], Rt_, perm=True)
                        transposes(t_, Rt_, 128, 4, lambda c, t=t: QaT[t][:, c, :], RQaT[t])
                    elif nm == "iq":
                        rope(pb[:, 0:256], Rp, 4, csq[t][:], Rcsq[t], t_[:, 0:256], Rt_)
                        transposes(t_, Rt_, 64, 4, lambda c, t=t: iqT[t][:, c, :], RiqT[t])
                        w_ = wv[t]
                        P.op("dve", lambda e, pb=pb, w_=w_: e.tensor_copy(out=w_[:, 0:4], in_=pb[:, 256:260]), reads=[Rp], writes=[Rwv[t]])
                        P.op("dve", lambda e, w_=w_: e.tensor_scalar(out=w_[:, 8:12], in0=w_[:, 0:4], scalar1=0.0, scalar2=2.0, op0=ALU.is_ge, op1=ALU.mult), reads=[Rwv[t]], writes=[Rwv[t]])
                        P.op("dve", lambda e, w_=w_: e.tensor_scalar(out=w_[:, 8:12], in0=w_[:, 8:12], scalar1=-1.0, scalar2=None, op0=ALU.add), reads=[Rwv[t]], writes=[Rwv[t]])
                        P.op("dve", lambda e, w_=w_: e.scalar_tensor_tensor(out=w_[:, 4:8], in0=w_[:, 0:4], scalar=0.0625, in1=w_[:, 8:12], op0=ALU.mult, op1=ALU.mult), reads=[Rwv[t]], writes=[Rwv[t]])
                        for h in range(4):
                            P.op("dve", lambda e, w_=w_, h=h, t=t: e.tensor_scalar(out=dg[t][:, h, :], in0=identf[:], scalar1=w_[:, 8 + h:9 + h], scalar2=None, op0=ALU.mult), reads=[Rwv[t], Rc], writes=[Rdg[t]])
                    elif nm.startswith("bq"):
                        g = int(nm[2])
                        rope(pb[:, 0:512], Rp, 8, csq[t][:], Rcsq[t], t_[:, 0:512], Rt_)
                        transposes(t_, Rt_, 128, 4, lambda c, t=t, g=g: bqT[t][:, g, c, :], RbqT[t])
                    else:
                        P.op("act", lambda e, pb=pb, t_=t_: e.copy(out=t_[:], in_=pb[:]), reads=[Rp], writes=[Rt_])
                        transposes(t_, Rt_, 128, 4, lambda c, t=t: mqT[t][:, c, :], RmqT[t])
            flush_deferred()
            if stop == "q":
                finish(); return nc, P
            for t in range(QB):
                sq = 2 * (I * QB + t) + 1
                dsa(t, sq, 0)
                bmix(t, sq)
                memattn(t)
                dsa(t, sq, 1)
            if dbg and I == NIT - 1:
                for (nm, src, R_) in (("d_ya", yaT, RyaT), ("d_yb", ybT, RybT), ("d_ym", ymT, RymT)):
                    np_ = src.shape[0]
                    P.op("dve", lambda e, src=src, np_=np_: e.tensor_copy(out=score[0:np_, 0:src.shape[1] * N], in_=src[:].rearrange("p a n -> p (a n)")), reads=[R_], writes=[Rsc, Rmw[0], Rmw[1]])
                    out_tokens.append(P.dma("sp", lambda e, nm=nm, np_=np_, src=src: e.dma_start(out=dbg_d[nm], in_=score[0:np_, 0:src.shape[1] * N]), reads=[Rsc]))
            for f in range(8):
                j = f % 2
                P.dma("sp", lambda e, f=f, j=j: e.dma_start(out=wgs[j][:], in_=wg_b.rearrange("(c p) n -> p c n", p=128)[:, :, f * 384:(f + 1) * 384]), reads=[Rw_b], writes=[Rwgs[j]])
                P.dma("sp", lambda e, f=f, j=j: e.dma_start(out=wba[j][:], in_=wbr_b[0:1024, f * 128:(f + 1) * 128].rearrange("(h p) n -> p h n", p=64)), reads=[Rw_b], writes=[Rwba[j]])
                P.dma("sp", lambda e, f=f, j=j: e.dma_start(out=wbm[j][:], in_=wbr_b[1024:1536, f * 128:(f + 1) * 128].rearrange("(h p) n -> p h n", p=128)), reads=[Rw_b], writes=[Rwbm[j]])
                for r in range(3):
                    pb, Rp = rot()
                    for c in range(8):
                        P.op("pe", lambda e, pb=pb, c=c, r=r, j=j: e.matmul(pb[:, 0:N], lhsT=wgs[j][:, c, r * 128:(r + 1) * 128], rhs=nTq[:, c, :], start=(c == 0), stop=(c == 7)),
                             reads=[Rwgs[j], RnTq], writes=[Rp])
                    P.op("act", lambda e, pb=pb, r=r, f=f: e.activation(out=gt[r][:], in_=pb[:, 0:N], func=AF.Sigmoid, bias=bgate[:, f * 3 + r:f * 3 + r + 1], scale=1.0),
                         reads=[Rp, Rc], writes=[Rgt[r]])
                for r in range(3):
                    pb, Rp = rot()
                    if r < 2:
                        ysrc, Ry = (yaT, RyaT) if r == 0 else (ybT, RybT)
                        for h in range(8):
                            P.op("pe", lambda e, pb=pb, h=h, r=r, j=j, ysrc=ysrc: e.matmul(pb[:, 0:N], lhsT=wba[j][:, r * 8 + h, :], rhs=ysrc[:, h, :], start=(h == 0), stop=(h == 7)),
                                 reads=[Rwba[j], Ry], writes=[Rp])
                    else:
                        for h in range(4):
                            P.op("pe", lambda e, pb=pb, h=h, j=j: e.matmul(pb[:, 0:N], lhsT=wbm[j][:, h, :], rhs=ymT[:, h, :], start=(h == 0), stop=(h == 3)),
                                 reads=[Rwbm[j], RymT], writes=[Rp])
                    P.op("dve", lambda e, pb=pb, r=r: e.tensor_tensor(out=mtmp[r][:], in0=pb[:, 0:N], in1=gt[r][:], op=ALU.mult), reads=[Rp, Rgt[r]], writes=[Rmtmp[r]])
                P.op("pool", lambda e: e.tensor_tensor(out=mtmp[0][:], in0=mtmp[0][:], in1=mtmp[1][:], op=ALU.add), reads=[Rmtmp[0], Rmtmp[1]], writes=[Rmtmp[0]])
                P.op("pool", lambda e, f=f: e.tensor_tensor(out=mT[:, f, :], in0=mtmp[0][:], in1=mtmp[2][:], op=ALU.add), reads=[Rmtmp[0], Rmtmp[2]], writes=[RmT])
            for n2 in range(2):
                ws, Rws = load_slab(wo_b, n2 * 512, 512)
                for t in range(QB):
                    pb, Rp = proj(mT, RmT, t * 128, ws, Rws, 512)
                    P.op("dve", lambda e, pb=pb, t=t, n2=n2: e.tensor_tensor(out=xh[:, t, n2 * 512:(n2 + 1) * 512], in0=pb[:], in1=xh[:, t, n2 * 512:(n2 + 1) * 512], op=ALU.add),
                         reads=[Rp, Rxh[t]], writes=[Rxh[t]])
            if dbg and I == NIT - 1:
                out_tokens.append(P.dma("sp", lambda e: e.dma_start(out=dbg_d["d_h"], in_=xh[:].rearrange("p t n -> p (t n)")), reads=Rxh))
            if stop == "d":
                finish(); return nc, P
            for t in range(QB):
                si = rms_T(xh[:, t, :], Rxh[t], 16, lambda c, t=t: xnT[:, c, t * 128:(t + 1) * 128], RxnT)
                rs = rs_t[:, si:si + 1]
                P.op("dve", lambda e, t=t, rs=rs: e.tensor_scalar(out=xf32[:], in0=xh[:, t, :], scalar1=rs, scalar2=None, op0=ALU.mult), reads=[Rxh[t], Rstat[si]], writes=[Rxf32])
                for half in range(2):
                    pb, Rp = rot()
                    for c4 in range(4):
                        c = half * 4 + c4
                        P.op("pe", lambda e, pb=pb, c=c, c4=c4: e.transpose(out=pb[:, c4 * 128:(c4 + 1) * 128], in_=xf32[:, c * 128:(c + 1) * 128], identity=identf[:]), reads=[Rxf32, Rc], writes=[Rp])
                    for c4 in range(4):
                        c = half * 4 + c4
                        P.op("dve", lambda e, pb=pb, c=c, c4=c4: e.tensor_scalar(out=xnT32[:, c, :], in0=pb[:, c4 * 128:(c4 + 1) * 128], scalar1=gcols[:, 16 + c:17 + c], scalar2=None, op0=ALU.mult), reads=[Rp, Rc], writes=[RxnT32])
                pb, Rp = rot()
                for c in range(8):
                    P.op("pe", lambda e, pb=pb, c=c: e.matmul(pb[:, 0:20], lhsT=xnT32[:, c, :], rhs=wr32[:, c, :], start=(c == 0), stop=(c == 7)), reads=[RxnT32, Rc], writes=[Rp])
                lg = rl[:, 0:20]; gmx = rl[:, 20:21]; ngmx = rl[:, 21:22]; gex = rl[:, 22:26]; gsum = rl[:, 26:27]; gw = rl[:, 27:28]
                ohg = rl[:, 28:32]; sel = rl[:, 32:36]; m1 = rl[:, 36:37]; oh1 = rl[:, 37:41]; sel2 = rl[:, 41:45]; m2 = rl[:, 45:46]
                oh2 = rl[:, 46:50]; ee = rl[:, 50:51]; p1 = rl[:, 51:52]; p2 = rl[:, 52:53]; cw = rl[:, 53:57]; nm1 = rl[:, 57:58]; cw2 = rl[:, 58:62]
                RW = dict(reads=[Rrl], writes=[Rrl])
                P.op("dve", lambda e, pb=pb: e.tensor_tensor(out=lg, in0=pb[:, 0:20], in1=rbias[:], op=ALU.add), reads=[Rp, Rc], writes=[Rrl])
                P.op("dve", lambda e: e.tensor_reduce(out=gmx, in_=lg[:, 0:4], axis=AX, op=ALU.max), **RW)
                P.op("dve", lambda e: e.tensor_scalar(out=ngmx, in0=gmx, scalar1=-1.0, scalar2=None, op0=ALU.mult), **RW)
                P.op("act", lambda e: e.activation(out=gex, in_=lg[:, 0:4], func=AF.Exp, bias=ngmx, scale=1.0, accum_out=gsum), **RW)
                P.op("dve", lambda e: e.reciprocal(out=gw, in_=gsum), **RW)
                P.op("dve", lambda e: e.tensor_scalar(out=ohg, in0=lg[:, 0:4], scalar1=gmx, scalar2=None, op0=ALU.is_equal), **RW)
                P.op("dve", lambda e: e.tensor_scalar(out=sel, in0=lg[:, 4:8], scalar1=ohg[:, 0:1], scalar2=None, op0=ALU.mult), **RW)
                for g in range(1, 4):
                    P.op("dve", lambda e, g=g: e.scalar_tensor_tensor(out=sel, in0=lg[:, 4 + 4 * g:8 + 4 * g], scalar=ohg[:, g:g + 1], in1=sel, op0=ALU.mult, op1=ALU.add), **RW)
                P.op("dve", lambda e: e.tensor_reduce(out=m1, in_=sel, axis=AX, op=ALU.max), **RW)
                P.op("dve", lambda e: e.tensor_scalar(out=oh1, in0=sel, scalar1=m1, scalar2=None, op0=ALU.is_equal), **RW)
                P.op("dve", lambda e: e.scalar_tensor_tensor(out=sel2, in0=oh1, scalar=NINF, in1=sel, op0=ALU.mult, op1=ALU.add), **RW)
                P.op("dve", lambda e: e.tensor_reduce(out=m2, in_=sel2, axis=AX, op=ALU.max), **RW)
                P.op("dve", lambda e: e.tensor_scalar(out=oh2, in0=sel2, scalar1=m2, scalar2=None, op0=ALU.is_equal), **RW)
                P.op("dve", lambda e: e.tensor_scalar(out=nm1, in0=m1, scalar1=-1.0, scalar2=None, op0=ALU.mult), **RW)
                P.op("act", lambda e: e.activation(out=ee, in_=m2, func=AF.Exp, bias=nm1, scale=1.0), **RW)
                P.op("dve", lambda e: e.tensor_scalar(out=p1, in0=ee, scalar1=1.0, scalar2=None, op0=ALU.add), **RW)
                P.op("dve", lambda e: e.reciprocal(out=p1, in_=p1), **RW)
                P.op("dve", lambda e: e.tensor_tensor(out=p2, in0=ee, in1=p1, op=ALU.mult), **RW)
                P.op("dve", lambda e: e.tensor_scalar(out=cw, in0=oh1, scalar1=p1, scalar2=None, op0=ALU.mult), **RW)
                P.op("dve", lambda e: e.scalar_tensor_tensor(out=cw2, in0=oh2, scalar=p2, in1=cw, op0=ALU.mult, op1=ALU.add), **RW)
                P.op("dve", lambda e: e.tensor_scalar(out=cw, in0=cw2, scalar1=gw, scalar2=None, op0=ALU.mult), **RW)
                for g in range(4):
                    P.op("dve", lambda e, g=g, t=t: e.tensor_scalar(out=comb[t][:, g * 4:(g + 1) * 4], in0=cw, scalar1=ohg[:, g:g + 1], scalar2=None, op0=ALU.mult), reads=[Rrl], writes=[Rcomb[t]])
            for ex in range(16):
                j = ex % 2
                base = j * 12288
                w1e = r1[:, base:base + 4096].rearrange("p (c n) -> p c n", c=8)
                w3e = r1[:, base + 4096:base + 8192].rearrange("p (c n) -> p c n", c=8)
                w2e = r1[:, base + 8192:base + 12288].rearrange("p (c n) -> p c n", c=4)
                wr_ = [Rmw[j], Rsc, Rjk]
                P.dma("sp", lambda e, ex=ex, w1e=w1e: e.dma_start(out=w1e, in_=w1_b[ex * D:(ex + 1) * D, :].rearrange("(c p) n -> p c n", p=128)), reads=[Rw_b], writes=wr_)
                P.dma("sp", lambda e, ex=ex, w3e=w3e: e.dma_start(out=w3e, in_=w3_b[ex * D:(ex + 1) * D, :].rearrange("(c p) n -> p c n", p=128)), reads=[Rw_b], writes=wr_)
                P.dma("sp", lambda e, ex=ex, w2e=w2e: e.dma_start(out=w2e, in_=w2_b[ex * 512:(ex + 1) * 512, :].rearrange("(c p) n -> p c n", p=128)), reads=[Rw_b], writes=wr_)
                for c in range(4):
                    pa, Rpa = rot()
                    for k in range(8):
                        P.op("pe", lambda e, pa=pa, k=k, c=c, w1e=w1e: e.matmul(pa[:, 0:N], lhsT=w1e[:, k, c * 128:(c + 1) * 128], rhs=xnT[:, k, :], start=(k == 0), stop=(k == 7)), reads=[Rmw[j], RxnT], writes=[Rpa])
                    pb, Rp = rot()
                    for k in range(8):
                        P.op("pe", lambda e, pb=pb, k=k, c=c, w3e=w3e: e.matmul(pb[:, 0:N], lhsT=w3e[:, k, c * 128:(c + 1) * 128], rhs=xnT[:, k, :], start=(k == 0), stop=(k == 7)), reads=[Rmw[j], RxnT], writes=[Rp])
                    sj = c % 2
                    P.op("act", lambda e, pa=pa, sj=sj: e.activation(out=sA[sj][:], in_=pa[:, 0:N], func=AF.Silu), reads=[Rpa], writes=[RsA[sj]])
                    P.op("dve", lambda e, pb=pb, sj=sj, c=c, j=j: e.tensor_tensor(out=hT[j][:, c, :], in0=pb[:, 0:N], in1=sA[sj][:], op=ALU.mult), reads=[Rp, RsA[sj]], writes=[RhT[j]])
                for t in range(QB):
                    for n2 in range(2):
                        pb, Rp = rot()
                        for c in range(4):
                            P.op("pe", lambda e, pb=pb, c=c, t=t, n2=n2, j=j, w2e=w2e: e.matmul(pb[:], lhsT=hT[j][:, c, t * 128:(t + 1) * 128], rhs=w2e[:, c, n2 * 512:(n2 + 1) * 512], start=(c == 0), stop=(c == 3)), reads=[RhT[j], Rmw[j]], writes=[Rp])
                        P.op("dve", lambda e, pb=pb, t=t, n2=n2, ex=ex: e.scalar_tensor_tensor(out=xh[:, t, n2 * 512:(n2 + 1) * 512], in0=pb[:], scalar=comb[t][:, ex:ex + 1], in1=xh[:, t, n2 * 512:(n2 + 1) * 512], op0=ALU.mult, op1=ALU.add),
                             reads=[Rp, Rcomb[t], Rxh[t]], writes=[Rxh[t]])
            for t in range(QB):
                qi = I * QB + t
                ss = ss_t[:, 0:1]; rs = rs_t[:, 0:1]
                P.op("act", lambda e, t=t, ss=ss: e.activation(out=sqj[:], in_=xh[:, t, :], func=AF.Square, accum_out=ss), reads=[Rxh[t]], writes=[Rsqj, Rstat[0]])
                P.op("dve", lambda e, ss=ss, rs=rs: e.tensor_scalar(out=rs, in0=ss, scalar1=1.0 / D, scalar2=1e-6, op0=ALU.mult, op1=ALU.add), reads=[Rstat[0]], writes=[Rstat[0]])
                P.op("act", lambda e, rs=rs: e.activation(out=rs, in_=rs, func=AF.Sqrt), reads=[Rstat[0]], writes=[Rstat[0]])
                P.op("dve", lambda e, rs=rs: e.reciprocal(out=rs, in_=rs), reads=[Rstat[0]], writes=[Rstat[0]])
                P.op("dve", lambda e, t=t, rs=rs: e.scalar_tensor_tensor(out=ot[:], in0=xh[:, t, :], scalar=rs, in1=gfin[:], op0=ALU.mult, op1=ALU.mult), reads=[Rxh[t], Rstat[0], Rc], writes=[Rot])
                out_tokens.append(P.dma("sp", lambda e, qi=qi: e.dma_start(out=out_d[qi * 128:(qi + 1) * 128, :], in_=ot[:]), reads=[Rot]))
        P.wait_tokens("sp", out_tokens)
        P.emit()
    return nc, P


def _host_consts(S, parity):
    pos = np.arange(S, dtype=np.float32) - (0 if parity else 128)
    half = 32
    inv = (10000.0 ** (-np.arange(half, dtype=np.float32) / half)).astype(np.float32)
    ang = pos[:, None] * inv[None, :]
    cs = np.concatenate([np.cos(ang), np.sin(ang)], axis=1).astype(np.float32)
    q = np.arange(128)[:, None]; k = np.arange(128)[None, :]
    cm = np.zeros((128, 256), np.float32)
    cm[:, 128:] = np.where(k <= q, 0.0, NINF)
    m0 = np.full((128, 128), 0.0 if parity else NINF, np.float32)
    mb = np.zeros((128, 24, 128), np.float32)
    for g, (W, d) in enumerate(B_PAT):
        for jj in range(B_NB[g] + 1):
            diff = 128 * jj + q - k
            ok = (diff >= 0) & (diff <= W) & (diff % d == 0)
            mb[:, B_MOFF[g] + jj, :] = np.where(ok, 0.0, NEG)
    mb0 = mb.copy() if parity else np.full_like(mb, NEG)
    return cs, cm, m0, mb.reshape(128, -1), mb0.reshape(128, -1)


def _prep_inputs(inputs, S, n_cores=8):
    f = lambda a: np.ascontiguousarray(np.asarray(a, dtype=np.float32))
    x = f(inputs["x"]); mem = f(inputs["mem"])
    w_in = f(inputs["w_in"])[0]
    o = np.cumsum([0, 512, 128, 128, 256, 64, 4, 1536, 1536, 1536, 512])
    aq, ak, av, iq, ik, iw, bq, bk, bv, mq = [w_in[:, o[i]:o[i + 1]] for i in range(10)]
    wk = np.ascontiguousarray(np.concatenate([ak, ik, av, bk, bv], axis=1))
    wq = np.ascontiguousarray(np.concatenate([aq, iq, iw, bq, mq], axis=1))
    wg0 = f(inputs["w_gate"])[0]
    wg = np.ascontiguousarray(wg0.reshape(D, 3, 8, 128).transpose(0, 2, 1, 3).reshape(D, 3072))
    bg0 = f(inputs["b_gate"])[0].reshape(3, 8, 128)
    bgate = np.ascontiguousarray(bg0.transpose(2, 1, 0).reshape(128, 24))
    wbr = np.ascontiguousarray(f(inputs["w_branch"])[0].reshape(1536, D))
    wo = f(inputs["w_out"])[0]
    wmem = f(inputs["w_mem_kv"])[0]
    w1 = np.ascontiguousarray(f(inputs["w1"])[0].reshape(16 * D, 512))
    w3 = np.ascontiguousarray(f(inputs["w3"])[0].reshape(16 * D, 512))
    w2 = np.ascontiguousarray(f(inputs["w2"])[0].reshape(16 * 512, D))
    wsub = f(inputs["w_sub"])[0]
    wr = np.ascontiguousarray(np.concatenate([f(inputs["w_group"])[0], wsub.transpose(1, 0, 2).reshape(D, 16)], axis=1))
    gc = lambda g: np.asarray(g, np.float32).reshape(8, 128).T
    gcols = np.ascontiguousarray(np.concatenate([gc(f(inputs["g_mix"])[0]), gc(f(inputs["g_mem"])[0]), gc(f(inputs["g_ffn"])[0])], axis=1))
    gfin = np.ascontiguousarray(np.broadcast_to(f(inputs["g_final"])[None, :], (128, D)))
    rb = np.concatenate([f(inputs["b_group"])[0], f(inputs["b_sub"])[0].reshape(16)])
    rbias = np.ascontiguousarray(np.broadcast_to(rb[None, :], (128, 20)))
    ident = np.eye(128, dtype=np.float32)
    shared = dict(wk=wk, wq=wq, wg=wg, wbr=wbr, wo=wo, wmem=wmem, w1=w1, w3=w3, w2=w2, wr=wr, gcols=gcols,
                  bgate=bgate, gfin=gfin, rbias=rbias, ident=ident)
    in_maps = []
    for c in range(n_cores):
        b, p = c // 2, c % 2
        if p == 0:
            xs = np.concatenate([np.zeros((128, D), np.float32), x[b, :S - 128]], axis=0)
        else:
            xs = x[b, :S]
        cs, cm, m0, mb, mb0 = _host_consts(S, p)
        m = dict(shared)
        m.update(xs=np.ascontiguousarray(xs), cs=cs, memx=np.ascontiguousarray(mem[b]), dsa_cm=cm, dsa_m0=m0, mbias=mb, mb0=mb0)
        in_maps.append(m)
    return in_maps


_CACHE = {}


def run(inputs, S, QB=2, dbg=False):
    key = (S, QB, dbg)
    if key not in _CACHE:
        _CACHE[key] = build(S, QB, dbg)
    nc, P = _CACHE[key]
    in_maps = _prep_inputs(inputs, S)
    res = run_bass_kernel_spmd(nc, in_maps, core_ids=list(range(8)))
    B = 4
    out = np.zeros((B, S, D), np.float32)
    for c in range(8):
        b, p = c // 2, c % 2
        o = np.asarray(res.results[c]["out"]).reshape(S // 256, 128, D)
        out[b].reshape(S // 256, 2, 128, D)[:, p] = o
    return out, res


def kernel(**inputs):
    out, _ = run(inputs, 8192, QB=2)
    return out
```

```python
import contextlib
import os
RMS_CUT = int(os.environ.get('RMS_CUT', '9'))
BM_CUT = int(os.environ.get('BM_CUT', '9'))
import numpy as np
import concourse.bass as bass
import concourse.mybir as mybir
from concourse.bass_utils import run_bass_kernel_spmd

F32 = mybir.dt.float32
BF16 = mybir.dt.bfloat16
AF = mybir.ActivationFunctionType
ALU = mybir.AluOpType
AX = mybir.AxisListType.X

D = 1024
NEG = -30000.0
NINF = -1.0e30
NBIS = 17
B_PAT = ((128, 1), (512, 4), (2048, 16))
B_NB = (1, 4, 16)
B_MOFF = (0, 2, 7)
ENGS = ("pe", "act", "dve", "pool", "sp")
EPOCH = 12000


class Res:
    __slots__ = ("w", "r", "excl")

    def __init__(self, excl=False):
        self.w = None
        self.r = []
        self.excl = excl


class Prog:
    def __init__(self, nc, n_dma_sems=24):
        self.nc = nc
        self.q = {e: [] for e in ENGS}
        self.sem = {}
        self.cnt = {}
        self.key_eng = {}
        self.cur = {}
        for e in ENGS:
            self._new_epoch(e, 0)
        self.ndma = n_dma_sems
        for k in range(n_dma_sems):
            key = "dma%d" % k
            self.sem[key] = nc.alloc_semaphore(name="d%d" % k)
            self.cnt[key] = 0
            self.key_eng[key] = "dma"
        self.dma_rr = 0
        self.known = {e: {} for e in ENGS}
        self.nwaits = 0
        self.nops = 0

    def _new_epoch(self, e, k):
        key = "%s#%d" % (e, k)
        self.sem[key] = self.nc.alloc_semaphore(name="s_%s_%d" % (e, k))
        self.cnt[key] = 0
        self.key_eng[key] = e
        self.cur[e] = (key, k)

    def _need(self, eng, tok, waits):
        if tok is None:
            return
        key, val = tok
        if self.known[eng].get(key, 0) >= val:
            return
        if waits.get(key, 0) < val:
            waits[key] = val

    def _deps(self, eng, reads, writes, same_sync):
        waits = {}
        for r in reads:
            if r.excl:
                for t in r.r:
                    if self.key_eng[t[0]] != eng:
                        self._need(eng, t, waits)
            if r.w is not None:
                if self.key_eng[r.w[0]] == eng and not same_sync:
                    continue
                self._need(eng, r.w, waits)
        for w in writes:
            if w.w is not None:
                if not (self.key_eng[w.w[0]] == eng and not same_sync):
                    self._need(eng, w.w, waits)
            for t in w.r:
                if self.key_eng[t[0]] == eng:
                    continue
                self._need(eng, t, waits)
        for k, v in waits.items():
            self.known[eng][k] = v
        return waits

    def _commit(self, tok, reads, writes):
        for r in reads:
            r.r.append(tok)
            if len(r.r) > 48:
                best = {}
                for k, v in r.r:
                    if best.get(k, 0) < v:
                        best[k] = v
                r.r = list(best.items())
        for w in writes:
            w.w = tok
            w.r = []

    def op(self, eng, fn, reads=(), writes=(), same_sync=True):
        if eng == "pe":
            same_sync = False
        waits = self._deps(eng, reads, writes, same_sync)
        key, k = self.cur[eng]
        if self.cnt[key] >= EPOCH:
            self._new_epoch(eng, k + 1)
            key, k = self.cur[eng]
        self.cnt[key] += 1
        tok = (key, self.cnt[key])
        self._commit(tok, reads, writes)
        self.q[eng].append((waits, fn, (key, 1)))
        self.nwaits += len(waits)
        self.nops += 1
        return tok

    def dma(self, qeng, fn, reads=(), writes=()):
        k = self.dma_rr
        self.dma_rr = (self.dma_rr + 1) % self.ndma
        key = "dma%d" % k
        waits = self._deps(qeng, reads, writes, True)
        prev = self.cnt[key]
        if prev > 0 and self.known[qeng].get(key, 0) < prev:
            waits[key] = max(waits.get(key, 0), prev)
            self.known[qeng][key] = prev
        self.cnt[key] += 16
        tok = (key, self.cnt[key])
        self._commit(tok, reads, writes)
        self.q[qeng].append((waits, fn, (key, 16)))
        self.nwaits += len(waits)
        self.nops += 1
        return tok

    def wait_tokens(self, eng, toks):
        waits = {}
        for t in toks:
            self._need(eng, t, waits)
        for k, v in waits.items():
            self.known[eng][k] = v
        self.q[eng].append((waits, None, None))

    def emit(self):
        nc = self.nc
        with nc.Block() as block:
            def mk(ename):
                def body(e):
                    for waits, fn, inc in self.q[ename]:
                        for k, v in waits.items():
                            e.wait_ge(self.sem[k], v)
                        if fn is not None:
                            ins = fn(e)
                            ins.then_inc(self.sem[inc[0]], inc[1])
                return body
            block.tensor(mk("pe"))
            block.scalar(mk("act"))
            block.vector(mk("dve"))
            block.gpsimd(mk("pool"))
            block.sync(mk("sp"))


WK_COLS = 3392
WQ_COLS = 2820
K_SLABS = [("a", 0, 320)] + [("bk%d" % g, 320 + 512 * g, 512) for g in range(3)] + \
          [("bv%d" % g, 1856 + 512 * g, 512) for g in range(3)]
Q_SLABS = [("aq", 0, 512), ("iq", 512, 260)] + [("bq%d" % g, 772 + 512 * g, 512) for g in range(3)] + \
          [("mq", 2308, 512)]


def build(S, QB, dbg=False, stop=None):
    NBLK = S // 128
    NQ = NBLK // 2
    NIT = NQ // QB
    N = QB * 128
    assert NBLK % 4 == 0 and NQ % QB == 0

    nc = bass.Bass("TRN2", target_bir_lowering=False)

    def din(name, shape, dt=F32):
        return nc.dram_tensor(name, shape, dt, kind="ExternalInput").ap()

    def dscr(name, shape, dt=BF16):
        return nc.dram_tensor(name, shape, dt).ap()

    xs = din("xs", [S, D])
    cs = din("cs", [S, 64])
    memx = din("memx", [256, D])
    wk = din("wk", [D, WK_COLS])
    wq = din("wq", [D, WQ_COLS])
    wg = din("wg", [D, 3072])
    wbr = din("wbr", [1536, D])
    wo = din("wo", [D, D])
    wmem = din("wmem", [D, 1024])
    w1 = din("w1", [16 * D, 512])
    w3 = din("w3", [16 * D, 512])
    w2 = din("w2", [16 * 512, D])
    wr = din("wr", [D, 20])
    gcols_d = din("gcols", [128, 24])
    bgate_d = din("bgate", [128, 24])
    gfin_d = din("gfin", [128, D])
    rbias_d = din("rbias", [128, 20])
    ident_d = din("ident", [128, 128])
    cm_d = din("dsa_cm", [128, 256])
    m0_d = din("dsa_m0", [128, 128])
    mb_d = din("mbias", [128, 24 * 128])
    mb0_d = din("mb0", [128, 24 * 128])
    out_d = nc.dram_tensor("out", [NQ * 128, D], F32, kind="ExternalOutput").ap()
    dbg_d = {}
    if dbg:
        for nm, shp in (("d_ya", [64, 8 * N]), ("d_yb", [64, 8 * N]), ("d_ym", [128, 4 * N]), ("d_h", [128, QB * D])):
            dbg_d[nm] = nc.dram_tensor(nm, shp, F32, kind="ExternalOutput").ap()

    wk_b = dscr("wk_b", [D, WK_COLS])
    wq_b = dscr("wq_b", [D, WQ_COLS])
    wg_b = dscr("wg_b", [D, 3072])
    wbr_b = dscr("wbr_b", [1536, D])
    wo_b = dscr("wo_b", [D, D])
    wmem_b = dscr("wmem_b", [D, 1024])
    w1_b = dscr("w1_b", [16 * D, 512])
    w3_b = dscr("w3_b", [16 * D, 512])
    w2_b = dscr("w2_b", [16 * 512, D])
    KaT_d = dscr("KaT_d", [128, S])
    kiT_d = dscr("kiT_d", [64, S])
    Va_d = dscr("Va_d", [S, 256])
    bkT_d = dscr("bkT_d", [128, 3, 4, S])
    Vb_d = dscr("Vb_d", [S, 3, 1024])

    P = Prog(nc)
    es = contextlib.ExitStack()
    with es:
        def sb(name, shape, dt=F32):
            return es.enter_context(nc.sbuf_tensor("s_" + name, shape, dt))

        pbank = [es.enter_context(nc.psum_tensor("pb%d" % i, [128, 512], F32)) for i in range(8)]
        Rb = [Res(excl=True) for _ in range(8)]
        rot_state = [0]

        def rot():
            k = 4 + rot_state[0]
            rot_state[0] = (rot_state[0] + 1) % 4
            return pbank[k], Rb[k]

        identf = sb("identf", [128, 128]); identb = sb("identb", [128, 128], BF16)
        i4b = sb("i4b", [128, 512], BF16)
        ones_b = sb("ones_b", [128, 128], BF16)
        gcols = sb("gcols", [128, 24]); bgate = sb("bgate", [128, 24])
        gfin = sb("gfin", [128, D]); rbias = sb("rbias", [128, 20])
        cm = sb("cm", [128, 256]); m0 = sb("m0", [128, 128])
        mbt = sb("mbt", [128, 24 * 128], BF16); mb0t = sb("mb0t", [128, 24 * 128], BF16)
        pow2 = sb("pow2", [128, NBIS + 1])
        wr32 = sb("wr32", [128, 8, 20])
        kmT = sb("kmT", [128, 4, 256], BF16); vmb = sb("vmb", [128, 2, 512], BF16)
        Rc = Res()

        def ld_const(dst, src):
            P.dma("sp", lambda e: e.dma_start(out=dst, in_=src), writes=[Rc])

        ld_const(identf[:], ident_d)
        ld_const(gcols[:], gcols_d); ld_const(bgate[:], bgate_d); ld_const(gfin[:], gfin_d)
        ld_const(rbias[:], rbias_d); ld_const(cm[:], cm_d); ld_const(m0[:], m0_d)
        ld_const(wr32[:], wr.rearrange("(c p) n -> p c n", p=128))
        P.op("dve", lambda e: e.tensor_copy(out=identb[:], in_=identf[:]), reads=[Rc], writes=[Rc])
        for j in range(4):
            P.op("dve", lambda e, j=j: e.tensor_copy(out=i4b[:, j * 128:(j + 1) * 128], in_=identf[:]), reads=[Rc], writes=[Rc])
        P.op("dve", lambda e: e.memset(ones_b[:], 1.0), writes=[Rc])
        for i in range(NBIS + 1):
            P.op("dve", lambda e, i=i: e.memset(pow2[:, i:i + 1], 2.0 ** (-i)), writes=[Rc])

        def finish0():
            toks = [("dma%d" % k, P.cnt["dma%d" % k]) for k in range(P.ndma) if P.cnt["dma%d" % k] > 0]
            toks += [(P.cur[e_][0], P.cnt[P.cur[e_][0]]) for e_ in ("pe", "act", "dve", "pool") if P.cnt[P.cur[e_][0]] > 0]
            P.wait_tokens("sp", toks)
            P.emit()
        if stop == "c":
            finish0(); return nc, P
        Rw_b = Res()
        conv_hist = []

        def convert(src, dst):
            fs = src.rearrange("r c -> (r c)").rearrange("(n k) -> n k", k=1024)
            fd = dst.rearrange("r c -> (r c)").rearrange("(n k) -> n k", k=1024)
            n = fs.shape[0]
            for r0 in range(0, n, 1024):
                r1_ = min(n, r0 + 1024)
                if len(conv_hist) >= 4:
                    P.wait_tokens("pool", [conv_hist[-4]])
                conv_hist.append(P.dma("pool", lambda e, r0=r0, r1_=r1_: e.dma_start(out=fd[r0:r1_, :], in_=fs[r0:r1_, :])))

        for s_, d_ in ((wmem, wmem_b), (wk, wk_b), (wq, wq_b), (wg, wg_b), (wbr, wbr_b), (wo, wo_b),
                       (w1, w1_b), (w3, w3_b), (w2, w2_b)):
            convert(s_, d_)
        conv_tokens = []
        for k in range(P.ndma):
            key = "dma%d" % k
            if P.cnt[key] > 0:
                conv_tokens.append((key, P.cnt[key]))

        if stop == "conv":
            finish0(); return nc, P
        ss_t = sb("ss_t", [128, 4]); rs_t = sb("rs_t", [128, 4])
        sqj = sb("sqj", [128, D], BF16)
        xb_t = [sb("xb0", [128, D], BF16)] * 2
        Rstat = [Res() for _ in range(4)]
        Rsqj = Res()
        Rxb = [Res()] * 2
        rms_ctr = [0]

        def rms_T(x_ap, Rx, gofs, dst_fn, Rdst):
            i = rms_ctr[0] % 4
            j = rms_ctr[0] % 2
            rms_ctr[0] += 1
            ss = ss_t[:, i:i + 1]; rs = rs_t[:, i:i + 1]
            P.op("act", lambda e: e.activation(out=sqj[:], in_=x_ap, func=AF.Square, accum_out=ss),
                 reads=[Rx], writes=[Rsqj, Rstat[i]])
            if RMS_CUT <= 1:
                return i
            P.op("dve", lambda e: e.tensor_scalar(out=rs, in0=ss, scalar1=1.0 / D, scalar2=1e-6, op0=ALU.mult, op1=ALU.add),
                 reads=[Rstat[i]], writes=[Rstat[i]])
            P.op("act", lambda e: e.activation(out=rs, in_=rs, func=AF.Sqrt),
                 reads=[Rstat[i]], writes=[Rstat[i]])
            P.op("dve", lambda e: e.reciprocal(out=rs, in_=rs), reads=[Rstat[i]], writes=[Rstat[i]])
            if RMS_CUT <= 2:
                return i
            xb = xb_t[j]
            P.op("dve", lambda e: e.tensor_scalar(out=xb[:], in0=x_ap, scalar1=rs, scalar2=None, op0=ALU.mult),
                 reads=[Rx, Rstat[i]], writes=[Rxb[j]])
            if RMS_CUT <= 3:
                return i
            for half in range(2):
                pb, Rp = rot()
                pbb = pb[:].bitcast(BF16)
                for c4 in range(4):
                    c = half * 4 + c4
                    P.op("pe", lambda e, c=c, c4=c4, pbb=pbb: e.transpose(out=pbb[:, c4 * 128:(c4 + 1) * 128], in_=xb[:, c * 128:(c + 1) * 128], identity=identb[:]),
                         reads=[Rxb[j], Rc], writes=[Rp])
                if RMS_CUT <= 4:
                    continue
                for c4 in range(4):
                    c = half * 4 + c4
                    eng = "dve"
                    if eng == "dve":
                        P.op("dve", lambda e, c=c, c4=c4, pbb=pbb: e.tensor_scalar(out=dst_fn(c), in0=pbb[:, c4 * 128:(c4 + 1) * 128], scalar1=gcols[:, gofs + c:gofs + c + 1], scalar2=None, op0=ALU.mult),
                             reads=[Rp, Rc], writes=[Rdst])
                    else:
                        P.op("act", lambda e, c=c, c4=c4, pbb=pbb: e.activation(out=dst_fn(c), in_=pbb[:, c4 * 128:(c4 + 1) * 128], func=AF.Copy, scale=gcols[:, gofs + c:gofs + c + 1]),
                             reads=[Rp, Rc], writes=[Rdst])
            return i

        rt = [sb("rt%d" % i, [128, 256]) for i in range(4)]
        Rrt = [Res() for _ in range(4)]

        def rope(ps_ap, Rps, H, cs_ap, Rcs, out_ap, Rout, perm=False):
            n = H * 32
            if os.environ.get("NOROPE"):
                P.op("act", lambda e: e.copy(out=out_ap, in_=ps_ap), reads=[Rps], writes=[Rout])
                return
            if perm:
                x = ps_ap.rearrange("p (g j t d) -> p g j t d", g=2, t=2, d=32)
                o = out_ap.rearrange("p (j g t d) -> p g j t d", g=2, t=2, d=32)
                x1, x2 = x[:, :, :, 0, :], x[:, :, :, 1, :]
                o1, o2 = o[:, :, :, 0, :], o[:, :, :, 1, :]
                cos = cs_ap[:, 0:32].unsqueeze(1).unsqueeze(1).to_broadcast([128, 2, 4, 32])
                sin = cs_ap[:, 32:64].unsqueeze(1).unsqueeze(1).to_broadcast([128, 2, 4, 32])
                tv = [rt[i][:, 0:n].rearrange("p (g j d) -> p g j d", g=2, d=32) for i in range(4)]
            else:
                x = ps_ap.rearrange("p (h t d) -> p h t d", t=2, d=32)
                o = out_ap.rearrange("p (h t d) -> p h t d", t=2, d=32)
                x1, x2 = x[:, :, 0, :], x[:, :, 1, :]
                o1, o2 = o[:, :, 0, :], o[:, :, 1, :]
                cos = cs_ap[:, 0:32].unsqueeze(1).to_broadcast([128, H, 32])
                sin = cs_ap[:, 32:64].unsqueeze(1).to_broadcast([128, H, 32])
                tv = [rt[i][:, 0:n].rearrange("p (h d) -> p h d", d=32) for i in range(4)]
            P.op("dve", lambda e: e.tensor_tensor(out=tv[0], in0=x1, in1=cos, op=ALU.mult), reads=[Rps, Rcs], writes=[Rrt[0]])
            P.op("dve", lambda e: e.tensor_tensor(out=tv[1], in0=x2, in1=sin, op=ALU.mult), reads=[Rps, Rcs], writes=[Rrt[1]])
            P.op("dve", lambda e: e.tensor_tensor(out=tv[2], in0=x2, in1=cos, op=ALU.mult), reads=[Rps, Rcs], writes=[Rrt[2]])
            P.op("dve", lambda e: e.tensor_tensor(out=tv[3], in0=x1, in1=sin, op=ALU.mult), reads=[Rps, Rcs], writes=[Rrt[3]])
            P.op("dve", lambda e: e.tensor_tensor(out=o1, in0=tv[0], in1=tv[1], op=ALU.subtract), reads=[Rrt[0], Rrt[1]], writes=[Rout])
            P.op("dve", lambda e: e.tensor_tensor(out=o2, in0=tv[2], in1=tv[3], op=ALU.add), reads=[Rrt[2], Rrt[3]], writes=[Rout])

        wslab = [sb("wslab%d" % i, [128, 8, 512], BF16) for i in range(2)]
        Rwslab = [Res(), Res()]
        slab_ctr = [0]

        def load_slab(wsrc_b, c0, ncol):
            i = slab_ctr[0] % 2
            slab_ctr[0] += 1
            src = wsrc_b.rearrange("(c p) n -> p c n", p=128)[:, :, c0:c0 + ncol]
            P.dma("sp", lambda e: e.dma_start(out=wslab[i][:, :, 0:ncol], in_=src), reads=[Rw_b], writes=[Rwslab[i]])
            return wslab[i], Rwslab[i]

        def proj(nT, RnT, tok0, ws, Rws, ncol):
            pb, Rp = rot()
            for c in range(8):
                P.op("pe", lambda e, c=c: e.matmul(pb[:, 0:ncol], lhsT=nT[:, c, tok0:tok0 + 128], rhs=ws[:, c, 0:ncol], start=(c == 0), stop=(c == 7)),
                     reads=[RnT, Rws], writes=[Rp])
            flush_deferred()
            return pb, Rp

        tst = [sb("tst%d" % i, [128, 512], BF16) for i in range(2)]
        Rtst = [Res(), Res()]
        tst_ctr = [0]

        def next_tst():
            i = tst_ctr[0] % 2
            tst_ctr[0] += 1
            return tst[i], Rtst[i]

        deferred = []

        def flush_deferred():
            while deferred:
                deferred.pop(0)()

        def transposes(*a_, **k_):
            deferred.append(lambda: _transposes(*a_, **k_))

        def _transposes(src, Rsrc, ncols_each, nchunks, dst_fn, Rdst, eng="act"):
            pb, Rp = rot()
            pbb = pb[:].bitcast(BF16)
            for c in range(nchunks):
                P.op("pe", lambda e, c=c: e.transpose(out=pbb[0:ncols_each, c * 128:(c + 1) * 128], in_=src[:, c * ncols_each:(c + 1) * ncols_each], identity=identb[:]),
                     reads=[Rsrc, Rc], writes=[Rp])
            for c in range(nchunks):
                if eng == "act":
                    P.op("act", lambda e, c=c: e.copy(out=dst_fn(c), in_=pbb[0:ncols_each, c * 128:(c + 1) * 128]), reads=[Rp], writes=[Rdst])
                else:
                    P.op("dve", lambda e, c=c: e.tensor_copy(out=dst_fn(c), in_=pbb[0:ncols_each, c * 128:(c + 1) * 128]), reads=[Rp], writes=[Rdst])

        for e_ in ("sp", "act", "dve", "pe", "pool"):
            P.wait_tokens(e_, conv_tokens)

        xt = [sb("xt%d" % i, [128, D]) for i in range(2)]
        Rxt = [Res(), Res()]
        cst = [sb("cst%d" % i, [128, 64]) for i in range(4)]
        Rcst = [Res() for _ in range(4)]
        nTk = sb("nTk", [128, 8, 512], BF16)
        RnTk = Res()
        pcs = 0
        for (src, dst) in ((mb_d, mbt), (mb0_d, mb0t)):
            for pc in range(3):
                jx = pcs % 2; pcs += 1
                P.dma("sp", lambda e, src=src, pc=pc, jx=jx: e.dma_start(out=xt[jx][:], in_=src[:, pc * 1024:(pc + 1) * 1024]), writes=[Rxt[jx]])
                P.op("dve", lambda e, dst=dst, pc=pc, jx=jx: e.tensor_copy(out=dst[:, pc * 1024:(pc + 1) * 1024], in_=xt[jx][:]), reads=[Rxt[jx]], writes=[Rc])
        if stop == "m1":
            finish0(); return nc, P
        for mbk in range(2):
            P.dma("sp", lambda e, mbk=mbk: e.dma_start(out=xt[mbk][:], in_=memx[mbk * 128:(mbk + 1) * 128, :]), writes=[Rxt[mbk]])
            rms_T(xt[mbk][:], Rxt[mbk], 8, lambda c, mbk=mbk: nTk[:, c, mbk * 128:(mbk + 1) * 128], RnTk)
        if stop == "m2":
            finish0(); return nc, P
        Rkm = Res()
        for half in range(2):
            ws, Rws = load_slab(wmem_b, half * 512, 512)
            for mbk in range(2):
                pb, Rp = proj(nTk, RnTk, mbk * 128, ws, Rws, 512)
                if half == 0:
                    t_, Rt_ = next_tst()
                    P.op("act", lambda e, t_=t_, pb=pb: e.copy(out=t_[:], in_=pb[:]), reads=[Rp], writes=[Rt_])
                    transposes(t_, Rt_, 128, 4, lambda c, mbk=mbk: kmT[:, c, mbk * 128:(mbk + 1) * 128], Rkm)
                else:
                    P.op("act", lambda e, pb=pb, mbk=mbk: e.copy(out=vmb[:, mbk, :], in_=pb[:]), reads=[Rp], writes=[Rkm])

        def finish():
            toks = [("dma%d" % k, P.cnt["dma%d" % k]) for k in range(P.ndma) if P.cnt["dma%d" % k] > 0]
            toks += [(P.cur[e_][0], P.cnt[P.cur[e_][0]]) for e_ in ("pe", "act", "dve", "pool") if P.cnt[P.cur[e_][0]] > 0]
            P.wait_tokens("sp", toks)
            P.emit()

        flush_deferred()
        if stop == "p0":
            finish(); return nc, P
        r1 = sb("r1", [128, 24576], BF16)
        KaTst = r1[:, 0:512]; kiTst = r1[0:64, 512:1024]
        Vast = r1[:, 1024:2048].rearrange("p (b n) -> p b n", b=4)
        bkTst = [r1[:, 2048 + i * 2048:2048 + (i + 1) * 2048].rearrange("p (c n) -> p c n", c=4) for i in range(2)]
        Vbst = [r1[:, 6144 + i * 4096:6144 + (i + 1) * 4096].rearrange("p (b n) -> p b n", b=4) for i in range(2)]
        RKaTst, RkiTst, RVast = Res(), Res(), Res()
        RbkTst = [Res(), Res()]; RVbst = [Res(), Res()]
        RKd = Res()
        P.op("pool", lambda e: e.memset(Vast, 1.0), writes=[RVast])
        for i in range(2):
            P.op("pool", lambda e, i=i: e.memset(Vbst[i], 1.0), writes=[RVbst[i]])

        for T in range(NBLK // 4):
            for bl in range(4):
                sbk = T * 4 + bl
                j = sbk % 2
                P.dma("sp", lambda e, sbk=sbk, j=j: e.dma_start(out=xt[j][:], in_=xs[sbk * 128:(sbk + 1) * 128, :]), writes=[Rxt[j]])
                P.dma("sp", lambda e, sbk=sbk, bl=bl: e.dma_start(out=cst[bl][:], in_=cs[sbk * 128:(sbk + 1) * 128, :]), writes=[Rcst[bl]])
                rms_T(xt[j][:], Rxt[j], 0, lambda c, bl=bl: nTk[:, c, bl * 128:(bl + 1) * 128], RnTk)
            for si, (nm, c0, ncol) in enumerate(K_SLABS):
                ws, Rws = load_slab(wk_b, c0, ncol)
                for bl in range(4):
                    pb, Rp = proj(nTk, RnTk, bl * 128, ws, Rws, ncol)
                    if nm == "a":
                        t_, Rt_ = next_tst()
                        rope(pb[:, 0:192], Rp, 3, cst[bl][:], Rcst[bl], t_[:, 0:192], Rt_)
                        Vv = Vast[:, bl, :].rearrange("p (g t d) -> p g t d", g=2, t=2)[:, :, 0, :]
                        P.op("act", lambda e, pb=pb, Vv=Vv: e.copy(out=Vv, in_=pb[:, 192:320].rearrange("p (g d) -> p g d", g=2)), reads=[Rp], writes=[RVast])
                        transposes(t_, Rt_, 128, 1, lambda c, bl=bl: KaTst[:, bl * 128:(bl + 1) * 128], RKaTst)
                        def ki_tr(t_=t_, Rt_=Rt_, bl=bl):
                            pb2, Rp2 = rot()
                            pbb2 = pb2[:].bitcast(BF16)
                            P.op("pe", lambda e: e.transpose(out=pbb2[0:64, 0:128], in_=t_[:, 128:192], identity=identb[:]), reads=[Rt_, Rc], writes=[Rp2])
                            P.op("act", lambda e: e.copy(out=kiTst[:, bl * 128:(bl + 1) * 128], in_=pbb2[0:64, 0:128]), reads=[Rp2], writes=[RkiTst])
                        deferred.append(ki_tr)
                    elif nm.startswith("bk"):
                        g = int(nm[2]); jb = g % 2
                        t_, Rt_ = next_tst()
                        rope(pb[:, 0:512], Rp, 8, cst[bl][:], Rcst[bl], t_[:, 0:512], Rt_)
                        transposes(t_, Rt_, 128, 4, lambda c, bl=bl, jb=jb: bkTst[jb][:, c, bl * 128:(bl + 1) * 128], RbkTst[jb])
                    else:
                        g = int(nm[2]); jb = g % 2
                        Vv = Vbst[jb][:, bl, :].rearrange("p (h t d) -> p h t d", h=8, t=2)[:, :, 0, :]
                        P.op("act", lambda e, pb=pb, Vv=Vv: e.copy(out=Vv, in_=pb[:, 0:512].rearrange("p (h d) -> p h d", h=8)), reads=[Rp], writes=[RVbst[jb]])
                def stores(nm=nm, T=T):
                    if nm == "a":
                        P.dma("pool", lambda e, T=T: e.dma_start(out=KaT_d[:, T * 512:(T + 1) * 512], in_=KaTst), reads=[RKaTst])
                        P.dma("pool", lambda e, T=T: e.dma_start(out=kiT_d[:, T * 512:(T + 1) * 512], in_=kiTst), reads=[RkiTst])
                        P.dma("pool", lambda e, T=T: e.dma_start(out=Va_d[T * 512:(T + 1) * 512, :].rearrange("(b p) n -> p b n", p=128), in_=Vast), reads=[RVast])
                    elif nm.startswith("bk"):
                        g = int(nm[2]); jb = g % 2
                        P.dma("pool", lambda e, T=T, g=g, jb=jb: e.dma_start(out=bkT_d[:, g, :, T * 512:(T + 1) * 512], in_=bkTst[jb]), reads=[RbkTst[jb]])
                    else:
                        g = int(nm[2]); jb = g % 2
                        P.dma("pool", lambda e, T=T, g=g, jb=jb: e.dma_start(out=Vb_d[T * 512:(T + 1) * 512, g, :].rearrange("(b p) n -> p b n", p=128), in_=Vbst[jb]), reads=[RVbst[jb]])
                deferred.append(stores)
        flush_deferred()
        kd_tokens = [("dma%d" % k, P.cnt["dma%d" % k]) for k in range(P.ndma) if P.cnt["dma%d" % k] > 0]
        P.wait_tokens("sp", kd_tokens)

        if stop == "p1":
            finish(); return nc, P
        xh = sb("xh", [128, QB, D]); Rxh = [Res() for _ in range(QB)]
        nTq = nTk[:, :, 0:N]; RnTq = RnTk
        csq = [sb("csq%d" % i, [128, 64]) for i in range(QB)]; Rcsq = [Res() for _ in range(QB)]
        QaT = [sb("QaT%d" % i, [128, 4, 128], BF16) for i in range(QB)]; RQaT = [Res() for _ in range(QB)]
        iqT = [sb("iqT%d" % i, [64, 4, 128], BF16) for i in range(QB)]; RiqT = [Res() for _ in range(QB)]
        bqT = [sb("bqT%d" % i, [128, 3, 4, 128], BF16) for i in range(QB)]; RbqT = [Res() for _ in range(QB)]
        mqT = [sb("mqT%d" % i, [128, 4, 128], BF16) for i in range(QB)]; RmqT = [Res() for _ in range(QB)]
        wv = [sb("wv%d" % i, [128, 12]) for i in range(QB)]; Rwv = [Res() for _ in range(QB)]
        dg = [sb("dg%d" % i, [128, 4, 128], BF16) for i in range(QB)]; Rdg = [Res() for _ in range(QB)]
        yaT = sb("yaT", [64, 8, N], BF16); ybT = sb("ybT", [64, 8, N], BF16); ymT = sb("ymT", [128, 4, N], BF16)
        RyaT, RybT, RymT = Res(), Res(), Res()
        score = r1[:, 0:16384].bitcast(F32)
        junk = r1[:, 16384:24576]
        Rsc, Rjk = Res(), Res()
        Rmw = [Res(), Res()]
        Rm1 = [Res(), Res()]; Rm3 = [Res(), Res()]; Rm2 = [Res(), Res()]
        RMOE = [Rmw[0], Rmw[1], Rm1[0], Rm1[1], Rm3[0], Rm3[1], Rm2[0], Rm2[1]]
        RjkA = Res(); Rmid = Res(); RcntD = Res(); RcntA = Res(); Rsg = Res()
        kib = [sb("kib%d" % i, [64, 512], BF16) for i in range(2)]; Rkib = [Res(), Res()]
        Rh = [sb("Rh%d" % i, [128, 512], BF16) for i in range(4)]; RRh = [Res() for _ in range(4)]
        bis = sb("bis", [128, 8 + NBIS + 1]); Rbis = Res()
        nmb = [sb("nmb%d" % i, [128, 512], BF16) for i in range(2)]; Rnmb = [Res(), Res()]
        kab = [sb("kab%d" % i, [128, 512], BF16) for i in range(2)]; Rkab = [Res(), Res()]
        vab = [sb("vab%d" % i, [128, 4, 256], BF16) for i in range(2)]; Rvab = [Res(), Res()]
        pt = [sb("pt%d" % i, [128, 512], BF16) for i in range(4)]; Rpt = [Res() for _ in range(4)]
        pt_ctr = [0]
        rden = sb("rden", [128, 512]); Rrden = Res()
        NBB = 3
        bkb = [sb("bkb%d" % i, [128, 4, 128], BF16) for i in range(NBB)]; Rbkb = [Res() for _ in range(NBB)]
        vbb = [sb("vbb%d" % i, [128, 1024], BF16) for i in range(NBB)]; Rvbb = [Res() for _ in range(NBB)]
        bb_ctr = [0]
        gt = [sb("gt%d" % i, [128, N]) for i in range(3)]; Rgt = [Res() for _ in range(3)]
        wgs = [sb("wgs0", [128, 8, 384], BF16)] * 2; Rwgs = [Res()] * 2
        wba = [sb("wba0", [64, 16, 128], BF16)] * 2; Rwba = [Res()] * 2
        wbm = [sb("wbm0", [128, 4, 128], BF16)] * 2; Rwbm = [Res()] * 2
        mtmp = [sb("mtmp%d" % i, [128, N]) for i in range(3)]; Rmtmp = [Res() for _ in range(3)]
        mT = sb("mT", [128, 8, N], BF16); RmT = Res()
        xnT = mT; RxnT = RmT
        xf32 = xt[0]; Rxf32 = Rxt[0]
        xnT32 = xt[1][:].rearrange("p (c n) -> p c n", c=8); RxnT32 = Rxt[1]
        rl = sb("rl", [128, 64]); Rrl = Res()
        comb = [sb("comb%d" % i, [128, 16]) for i in range(QB)]; Rcomb = [Res() for _ in range(QB)]
        sA = [sb("sA%d" % i, [128, N], BF16) for i in range(2)]; RsA = [Res(), Res()]
        hT = [sb("hT%d" % i, [128, 4, N], BF16) for i in range(2)]; RhT = [Res(), Res()]
        ot = xf32; Rot = Rxf32
        out_tokens = []

        def next_pt():
            i = pt_ctr[0] % 4
            pt_ctr[0] += 1
            return pt[i], Rpt[i]

        def dsa(t, sq, part, inter=None):
            nkb = sq + 1
            nk = nkb * 128
            nch = (nk + 511) // 512
            aw = wv[t][:, 4:8]
            am = bis[:, 0:1]; w0 = bis[:, 1:2]; mid = bis[:, 2:3]; cnt = bis[:, 3:4]; sg = bis[:, 4:5]; thr = bis[:, 5:6]
            wt2 = bis[:, 8:8 + NBIS + 1]
            if part == 0:
                for ch in range(nch):
                    ncol = min(512, nk - ch * 512)
                    kb_ = kib[ch % 2]
                    P.dma("sp", lambda e, ch=ch, ncol=ncol, kb_=kb_: e.dma_start(out=kb_[:, 0:ncol], in_=kiT_d[:, ch * 512:ch * 512 + ncol]), reads=[RKd], writes=[Rkib[ch % 2]])
                    for h in range(4):
                        pb, Rp = rot()
                        P.op("pe", lambda e, pb=pb, h=h, ncol=ncol, kb_=kb_: e.matmul(pb[:, 0:ncol], lhsT=iqT[t][:, h, :], rhs=kb_[:, 0:ncol], start=True, stop=True),
                             reads=[RiqT[t], Rkib[ch % 2]], writes=[Rp])
                        P.op("act", lambda e, pb=pb, h=h, ncol=ncol: e.activation(out=Rh[h][:, 0:ncol], in_=pb[:, 0:ncol], func=AF.Relu, scale=aw[:, h:h + 1]),
                             reads=[Rp, Rwv[t]], writes=[RRh[h]])
                    pb, Rp = rot()
                    for h in range(4):
                        P.op("pe", lambda e, pb=pb, h=h, ncol=ncol: e.matmul(pb[:, 0:ncol], lhsT=dg[t][:, h, :], rhs=Rh[h][:, 0:ncol], start=(h == 0), stop=(h == 3)),
                             reads=[Rdg[t], RRh[h]], writes=[Rp])
                    P.op("dve", lambda e, pb=pb, ch=ch, ncol=ncol: e.tensor_copy(out=score[:, ch * 512:ch * 512 + ncol], in_=pb[:, 0:ncol]),
                         reads=[Rp], writes=[Rsc] + RMOE)
                P.op("dve", lambda e: e.tensor_reduce(out=am, in_=score[:, 0:nk], axis=AX, op=ALU.max, apply_absolute_value=True), reads=[Rsc], writes=[Rbis])
                P.op("dve", lambda e: e.tensor_tensor(out=score[:, nk - 256:nk], in0=score[:, nk - 256:nk], in1=cm[:], op=ALU.add), reads=[Rsc, Rc, Rbis], writes=[Rsc])
                P.op("dve", lambda e: e.tensor_tensor(out=score[:, 0:128], in0=score[:, 0:128], in1=m0[:], op=ALU.add), reads=[Rsc, Rc], writes=[Rsc])
                P.op("dve", lambda e: e.tensor_scalar(out=w0, in0=am, scalar1=1.001, scalar2=1e-6, op0=ALU.mult, op1=ALU.add), reads=[Rbis], writes=[Rbis])
                P.op("dve", lambda e: e.tensor_scalar(out=wt2, in0=pow2[:], scalar1=w0, scalar2=None, op0=ALU.mult), reads=[Rbis, Rc], writes=[Rbis])
                P.op("dve", lambda e: e.memset(mid, 0.0), reads=[Rbis], writes=[Rmid])
                hD = max(128, (nk // 2) // 128 * 128)
                n_act = nk - hD
                cntA = bis[:, 6:7]; tmpc = bis[:, 7:8]
                Cthr = 255.5 - 0.5 * n_act
                ksteps = 0
                if inter is not None:
                    ksteps = (inter[1] + NBIS - 1) // NBIS
                for it in range(NBIS):
                    P.op("dve", lambda e: e.tensor_scalar(out=junk[:, 0:hD], in0=score[:, 0:hD], scalar1=mid, scalar2=None, op0=ALU.is_ge, op1=ALU.add, accum_out=cnt),
                         reads=[Rsc, Rmid], writes=[Rjk, RcntD])
                    P.op("act", lambda e: e.activation(out=junk[:, hD:nk], in_=score[:, hD:nk], func=AF.Sign, scale=-1.0, bias=mid, accum_out=cntA),
                         reads=[Rsc, Rmid], writes=[RjkA, RcntA])
                    P.op("dve", lambda e: e.scalar_tensor_tensor(out=tmpc, in0=cntA, scalar=-0.5, in1=cnt, op0=ALU.mult, op1=ALU.add), reads=[RcntD, RcntA], writes=[Rsg])
                    P.op("dve", lambda e: e.tensor_scalar(out=sg, in0=tmpc, scalar1=Cthr, scalar2=0.5, op0=ALU.is_ge, op1=ALU.subtract), reads=[Rsg], writes=[Rsg])
                    P.op("dve", lambda e, it=it: e.scalar_tensor_tensor(out=mid, in0=sg, scalar=wt2[:, it:it + 1], in1=mid, op0=ALU.mult, op1=ALU.add), reads=[Rsg, Rbis], writes=[Rmid])
                    if inter is not None:
                        for _ in range(ksteps):
                            next(inter[0], None)
                P.op("dve", lambda e: e.tensor_tensor(out=thr, in0=mid, in1=wt2[:, NBIS:NBIS + 1], op=ALU.subtract), reads=[Rmid, Rbis], writes=[Rbis])
                return
            def chunk_loads(k4):
                ncol = min(512, nk - k4 * 512)
                nb_here = ncol // 128
                j = k4 % 2
                P.op("dve", lambda e: e.tensor_scalar(out=nmb[j][:, 0:ncol], in0=score[:, k4 * 512:k4 * 512 + ncol], scalar1=thr, scalar2=NEG, op0=ALU.is_lt, op1=ALU.mult),
                     reads=[Rsc, Rbis], writes=[Rnmb[j]])
                P.dma("sp", lambda e: e.dma_start(out=kab[j][:, 0:ncol], in_=KaT_d[:, k4 * 512:k4 * 512 + ncol]), reads=[RKd], writes=[Rkab[j]])
                P.dma("sp", lambda e: e.dma_start(out=vab[j][:, 0:nb_here, :], in_=Va_d[k4 * 512:k4 * 512 + nb_here * 128, :].rearrange("(b p) n -> p b n", p=128)), reads=[RKd], writes=[Rvab[j]])

            items = []
            for k4 in range(nch):
                ncol = min(512, nk - k4 * 512)
                for b in range(ncol // 128):
                    for g in range(2):
                        items.append((k4, b, g))
            last_of_chunk = {}
            for idx, (k4, b, g) in enumerate(items):
                last_of_chunk[k4] = idx
            stage = {}

            def s_stage(idx):
                k4, b, g = items[idx]
                j = k4 % 2
                pb, Rp = rot()
                P.op("pe", lambda e: e.matmul(pb[:], lhsT=kab[j][g * 64:(g + 1) * 64, b * 128:(b + 1) * 128], rhs=QaT[t][g * 64:(g + 1) * 64, :, :].rearrange("p j q -> p (j q)"), start=True, stop=False),
                     reads=[Rkab[j], RQaT[t]], writes=[Rp])
                P.op("pe", lambda e: e.matmul(pb[:], lhsT=nmb[j][:, b * 128:(b + 1) * 128], rhs=i4b[:], start=False, stop=True),
                     reads=[Rnmb[j], Rc], writes=[Rp])
                stage[idx] = (pb, Rp)

            def e_stage(idx):
                pb, Rp = stage[idx]
                p_, Rp_ = next_pt()
                P.op("act", lambda e: e.activation(out=p_[:], in_=pb[:], func=AF.Exp, scale=0.125), reads=[Rp], writes=[Rp_])
                stage[idx] = (p_, Rp_)

            def v_stage(idx):
                k4, b, g = items[idx]
                j = k4 % 2
                kb = k4 * 4 + b
                p_, Rp_ = stage.pop(idx)
                P.op("pe", lambda e: e.matmul(pbank[g][:], lhsT=vab[j][:, b, g * 128:(g + 1) * 128], rhs=p_[:], start=(kb == 0), stop=(kb == nkb - 1)),
                     reads=[Rvab[j], Rp_], writes=[Rb[g]])

            chunk_loads(0)
            if nch > 1:
                chunk_loads(1)
            nit = len(items)
            s_stage(0)
            if nit > 1:
                s_stage(1)
            for idx in range(nit):
                e_stage(idx)
                if idx + 2 < nit:
                    s_stage(idx + 2)
                v_stage(idx)
                k4 = items[idx][0]
                if last_of_chunk[k4] == idx and k4 + 2 < nch:
                    chunk_loads(k4 + 2)
            for g in range(2):
                P.op("dve", lambda e, g=g: e.reciprocal(out=rden[0:64, :], in_=pbank[g][64:128, :]), reads=[Rb[g]], writes=[Rrden])
                P.op("dve", lambda e, g=g: e.tensor_tensor(out=yaT[:, g * 4:(g + 1) * 4, t * 128:(t + 1) * 128], in0=pbank[g][0:64, :].rearrange("p (j q) -> p j q", j=4), in1=rden[0:64, :].rearrange("p (j q) -> p j q", j=4), op=ALU.mult),
                     reads=[Rb[g], Rrden], writes=[RyaT])

        def bmix(t, sq):
            first = [True, True]
            work = []
            for g in range(3):
                for jj in range(B_NB[g], -1, -1):
                    kb = sq - jj
                    if kb >= 0:
                        work.append((g, jj, kb))
            nwork = len(work)
            bufof = {}

            def b_load(w):
                g, jj, kb = work[w]
                i = bb_ctr[0] % NBB
                bb_ctr[0] += 1
                bufof[w] = i
                P.dma("sp", lambda e: e.dma_start(out=bkb[i][:], in_=bkT_d[:, g, :, kb * 128:(kb + 1) * 128]), reads=[RKd], writes=[Rbkb[i]])
                P.dma("sp", lambda e: e.dma_start(out=vbb[i][:], in_=Vb_d[kb * 128:(kb + 1) * 128, g, :]), reads=[RKd], writes=[Rvbb[i]])

            items = [(w, hh) for w in range(nwork) for hh in range(2)]
            nit = len(items)
            stage = {}
            loaded = [0]

            def ensure_loaded(upto):
                while loaded[0] <= min(upto, nwork - 1):
                    b_load(loaded[0])
                    loaded[0] += 1

            def s_stage(idx):
                w, hh = items[idx]
                g, jj, kb = work[w]
                ensure_loaded(w + 1 if hh == 0 else w)
                i = bufof[w]
                mtab = mb0t if kb == 0 else mbt
                mi = B_MOFF[g] + jj
                pb, Rp = rot()
                P.op("pe", lambda e: e.matmul(pb[:], lhsT=mtab[:, mi * 128:(mi + 1) * 128], rhs=i4b[:], start=True, stop=False),
                     reads=[Rc], writes=[Rp])
                for p2 in range(4):
                    P.op("pe", lambda e, p2=p2: e.matmul(pb[:, p2 * 128:(p2 + 1) * 128], lhsT=bkb[i][hh * 64:(hh + 1) * 64, p2, :], rhs=bqT[t][hh * 64:(hh + 1) * 64, g, p2, :], start=False, stop=(p2 == 3), skip_group_check=True),
                         reads=[Rbkb[i], RbqT[t]], writes=[Rp])
                stage[idx] = (pb, Rp)

            def e_stage(idx):
                pb, Rp = stage[idx]
                p_, Rp_ = next_pt()
                P.op("act", lambda e: e.activation(out=p_[:], in_=pb[:], func=AF.Exp, scale=0.125), reads=[Rp], writes=[Rp_])
                stage[idx] = (p_, Rp_)

            def v_stage(idx):
                w, hh = items[idx]
                i = bufof[w]
                p_, Rp_ = stage.pop(idx)
                for h4 in range(4):
                    h = 2 * h4 + hh
                    st = first[hh]
                    first[hh] = False
                    P.op("pe", lambda e, h4=h4, h=h, st=st: e.matmul(pbank[2 + hh][:, h4 * 128:(h4 + 1) * 128], lhsT=vbb[i][:, h * 128:(h + 1) * 128], rhs=p_[:, h4 * 128:(h4 + 1) * 128], start=st, stop=(w == nwork - 1 and h4 == 3), skip_group_check=True),
                         reads=[Rvbb[i], Rp_], writes=[Rb[2 + hh]])

            yield nit
            s_stage(0)
            if nit > 1:
                s_stage(1)
            for idx in range(nit):
                e_stage(idx)
                if idx + 2 < nit:
                    s_stage(idx + 2)
                v_stage(idx)
                yield idx
            for hh in range(2):
                P.op("dve", lambda e, hh=hh: e.reciprocal(out=rden[0:64, :], in_=pbank[2 + hh][64:128, :]), reads=[Rb[2 + hh]], writes=[Rrden])
                P.op("dve", lambda e, hh=hh: e.tensor_tensor(out=ybT[:, :, t * 128:(t + 1) * 128].rearrange("p (p2 hf) q -> p hf p2 q", hf=2)[:, hh], in0=pbank[2 + hh][0:64, :].rearrange("p (j q) -> p j q", j=4), in1=rden[0:64, :].rearrange("p (j q) -> p j q", j=4), op=ALU.mult),
                     reads=[Rb[2 + hh], Rrden], writes=[RybT])

        def memattn(t):
            sc = 128.0 ** -0.5
            for mbk in range(2):
                pb, Rp = rot()
                for h in range(4):
                    P.op("pe", lambda e, pb=pb, h=h, mbk=mbk: e.matmul(pb[:, h * 128:(h + 1) * 128], lhsT=kmT[:, h, mbk * 128:(mbk + 1) * 128], rhs=mqT[t][:, h, :], start=(h == 0), stop=(h == 3), skip_group_check=True),
                         reads=[Rkm, RmqT[t]], writes=[Rp])
                p_, Rp_ = next_pt()
                P.op("act", lambda e, pb=pb, p_=p_: e.activation(out=p_[:], in_=pb[:], func=AF.Exp, scale=sc), reads=[Rp], writes=[Rp_])
                for h in range(4):
                    P.op("pe", lambda e, h=h, mbk=mbk, p_=p_: e.matmul(pbank[0][:, h * 128:(h + 1) * 128], lhsT=vmb[:, mbk, h * 128:(h + 1) * 128], rhs=p_[:, h * 128:(h + 1) * 128], start=(mbk == 0 and h == 0), stop=(mbk == 1 and h == 3), skip_group_check=True),
                         reads=[Rkm, Rp_], writes=[Rb[0]])
                P.op("pe", lambda e, mbk=mbk, p_=p_: e.matmul(pbank[1][:], lhsT=ones_b[:], rhs=p_[:], start=(mbk == 0), stop=(mbk == 1)),
                     reads=[Rc, Rp_], writes=[Rb[1]])
            P.op("dve", lambda e: e.reciprocal(out=rden[:], in_=pbank[1][:]), reads=[Rb[1]], writes=[Rrden])
            P.op("dve", lambda e: e.tensor_tensor(out=ymT[:, :, t * 128:(t + 1) * 128], in0=pbank[0][:].rearrange("p (j q) -> p j q", j=4), in1=rden[:].rearrange("p (j q) -> p j q", j=4), op=ALU.mult),
                 reads=[Rb[0], Rrden], writes=[RymT])

        for I in range(NIT):
            for t in range(QB):
                sq = 2 * (I * QB + t) + 1
                P.dma("sp", lambda e, t=t, sq=sq: e.dma_start(out=xh[:, t, :], in_=xs[sq * 128:(sq + 1) * 128, :]), writes=[Rxh[t]])
                P.dma("sp", lambda e, t=t, sq=sq: e.dma_start(out=csq[t][:], in_=cs[sq * 128:(sq + 1) * 128, :]), writes=[Rcsq[t]])
                rms_T(xh[:, t, :], Rxh[t], 0, lambda c, t=t: nTq[:, c, t * 128:(t + 1) * 128], RnTq)
            for (nm, c0, ncol) in Q_SLABS:
                ws, Rws = load_slab(wq_b, c0, ncol)
                for t in range(QB):
                    pb, Rp = proj(nTq, RnTq, t * 128, ws, Rws, ncol)
                    t_, Rt_ = next_tst()
                    if nm == "aq":
                        rope(pb[:, 0:512], Rp, 8, csq[t][:], Rcsq[t], t_[:, 0:512], Rt_, perm=True)
                        transposes(t_, Rt_, 128, 4, lambda c, t=t: QaT[t][:, c, :], RQaT[t])
                    elif nm == "iq":
                        rope(pb[:, 0:256], Rp, 4, csq[t][:], Rcsq[t], t_[:, 0:256], Rt_)
                        transposes(t_, Rt_, 64, 4, lambda c, t=t: iqT[t][:, c, :], RiqT[t])
                        w_ = wv[t]
                        P.op("dve", lambda e, pb=pb, w_=w_: e.tensor_copy(out=w_[:, 0:4], in_=pb[:, 256:260]), reads=[Rp], writes=[Rwv[t]])
                        P.op("dve", lambda e, w_=w_: e.tensor_scalar(out=w_[:, 8:12], in0=w_[:, 0:4], scalar1=0.0, scalar2=2.0, op0=ALU.is_ge, op1=ALU.mult), reads=[Rwv[t]], writes=[Rwv[t]])
                        P.op("dve", lambda e, w_=w_: e.tensor_scalar(out=w_[:, 8:12], in0=w_[:, 8:12], scalar1=-1.0, scalar2=None, op0=ALU.add), reads=[Rwv[t]], writes=[Rwv[t]])
                        P.op("dve", lambda e, w_=w_: e.scalar_tensor_tensor(out=w_[:, 4:8], in0=w_[:, 0:4], scalar=0.0625, in1=w_[:, 8:12], op0=ALU.mult, op1=ALU.mult), reads=[Rwv[t]], writes=[Rwv[t]])
                        for h in range(4):
                            P.op("dve", lambda e, w_=w_, h=h, t=t: e.tensor_scalar(out=dg[t][:, h, :], in0=identf[:], scalar1=w_[:, 8 + h:9 + h], scalar2=None, op0=ALU.mult), reads=[Rwv[t], Rc], writes=[Rdg[t]])
                    elif nm.startswith("bq"):
                        g = int(nm[2])
                        rope(pb[:, 0:512], Rp, 8, csq[t][:], Rcsq[t], t_[:, 0:512], Rt_)
                        transposes(t_, Rt_, 128, 4, lambda c, t=t, g=g: bqT[t][:, g, c, :], RbqT[t])
                    else:
                        P.op("act", lambda e, pb=pb, t_=t_: e.copy(out=t_[:], in_=pb[:]), reads=[Rp], writes=[Rt_])
                        transposes(t_, Rt_, 128, 4, lambda c, t=t: mqT[t][:, c, :], RmqT[t])
            flush_deferred()
            if stop == "q":
                finish(); return nc, P
            for t in range(QB):
                sq = 2 * (I * QB + t) + 1
                gen = bmix(t, sq)
                nit_b = next(gen)
                dsa(t, sq, 0, inter=(gen, nit_b))
                for _ in gen:
                    pass
                memattn(t)
                dsa(t, sq, 1)
            if dbg and I == NIT - 1:
                for (nm, src, R_) in (("d_ya", yaT, RyaT), ("d_yb", ybT, RybT), ("d_ym", ymT, RymT)):
                    np_ = src.shape[0]
                    P.op("dve", lambda e, src=src, np_=np_: e.tensor_copy(out=score[0:np_, 0:src.shape[1] * N], in_=src[:].rearrange("p a n -> p (a n)")), reads=[R_], writes=[Rsc] + RMOE)
                    out_tokens.append(P.dma("sp", lambda e, nm=nm, np_=np_, src=src: e.dma_start(out=dbg_d[nm], in_=score[0:np_, 0:src.shape[1] * N]), reads=[Rsc]))
            for f in range(8):
                j = f % 2
                P.dma("sp", lambda e, f=f, j=j: e.dma_start(out=wgs[j][:], in_=wg_b.rearrange("(c p) n -> p c n", p=128)[:, :, f * 384:(f + 1) * 384]), reads=[Rw_b], writes=[Rwgs[j]])
                P.dma("sp", lambda e, f=f, j=j: e.dma_start(out=wba[j][:], in_=wbr_b[0:1024, f * 128:(f + 1) * 128].rearrange("(h p) n -> p h n", p=64)), reads=[Rw_b], writes=[Rwba[j]])
                P.dma("sp", lambda e, f=f, j=j: e.dma_start(out=wbm[j][:], in_=wbr_b[1024:1536, f * 128:(f + 1) * 128].rearrange("(h p) n -> p h n", p=128)), reads=[Rw_b], writes=[Rwbm[j]])
                for r in range(3):
                    pb, Rp = rot()
                    for c in range(8):
                        P.op("pe", lambda e, pb=pb, c=c, r=r, j=j: e.matmul(pb[:, 0:N], lhsT=wgs[j][:, c, r * 128:(r + 1) * 128], rhs=nTq[:, c, :], start=(c == 0), stop=(c == 7)),
                             reads=[Rwgs[j], RnTq], writes=[Rp])
                    P.op("act", lambda e, pb=pb, r=r, f=f: e.activation(out=gt[r][:], in_=pb[:, 0:N], func=AF.Sigmoid, bias=bgate[:, f * 3 + r:f * 3 + r + 1], scale=1.0),
                         reads=[Rp, Rc], writes=[Rgt[r]])
                for r in range(3):
                    pb, Rp = rot()
                    if r < 2:
                        ysrc, Ry = (yaT, RyaT) if r == 0 else (ybT, RybT)
                        for h in range(8):
                            P.op("pe", lambda e, pb=pb, h=h, r=r, j=j, ysrc=ysrc: e.matmul(pb[:, 0:N], lhsT=wba[j][:, r * 8 + h, :], rhs=ysrc[:, h, :], start=(h == 0), stop=(h == 7)),
                                 reads=[Rwba[j], Ry], writes=[Rp])
                    else:
                        for h in range(4):
                            P.op("pe", lambda e, pb=pb, h=h, j=j: e.matmul(pb[:, 0:N], lhsT=wbm[j][:, h, :], rhs=ymT[:, h, :], start=(h == 0), stop=(h == 3)),
                                 reads=[Rwbm[j], RymT], writes=[Rp])
                    P.op("dve", lambda e, pb=pb, r=r: e.tensor_tensor(out=mtmp[r][:], in0=pb[:, 0:N], in1=gt[r][:], op=ALU.mult), reads=[Rp, Rgt[r]], writes=[Rmtmp[r]])
                P.op("pool", lambda e: e.tensor_tensor(out=mtmp[0][:], in0=mtmp[0][:], in1=mtmp[1][:], op=ALU.add), reads=[Rmtmp[0], Rmtmp[1]], writes=[Rmtmp[0]])
                P.op("pool", lambda e, f=f: e.tensor_tensor(out=mT[:, f, :], in0=mtmp[0][:], in1=mtmp[2][:], op=ALU.add), reads=[Rmtmp[0], Rmtmp[2]], writes=[RmT])
            for n2 in range(2):
                ws, Rws = load_slab(wo_b, n2 * 512, 512)
                for t in range(QB):
                    pb, Rp = proj(mT, RmT, t * 128, ws, Rws, 512)
                    P.op("dve", lambda e, pb=pb, t=t, n2=n2: e.tensor_tensor(out=xh[:, t, n2 * 512:(n2 + 1) * 512], in0=pb[:], in1=xh[:, t, n2 * 512:(n2 + 1) * 512], op=ALU.add),
                         reads=[Rp, Rxh[t]], writes=[Rxh[t]])
            if dbg and I == NIT - 1:
                out_tokens.append(P.dma("sp", lambda e: e.dma_start(out=dbg_d["d_h"], in_=xh[:].rearrange("p t n -> p (t n)")), reads=Rxh))
            if stop == "d":
                finish(); return nc, P
            for t in range(QB):
                si = rms_T(xh[:, t, :], Rxh[t], 16, lambda c, t=t: xnT[:, c, t * 128:(t + 1) * 128], RxnT)
                rs = rs_t[:, si:si + 1]
                P.op("dve", lambda e, t=t, rs=rs: e.tensor_scalar(out=xf32[:], in0=xh[:, t, :], scalar1=rs, scalar2=None, op0=ALU.mult), reads=[Rxh[t], Rstat[si]], writes=[Rxf32])
                for half in range(2):
                    pb, Rp = rot()
                    for c4 in range(4):
                        c = half * 4 + c4
                        P.op("pe", lambda e, pb=pb, c=c, c4=c4: e.transpose(out=pb[:, c4 * 128:(c4 + 1) * 128], in_=xf32[:, c * 128:(c + 1) * 128], identity=identf[:]), reads=[Rxf32, Rc], writes=[Rp])
                    for c4 in range(4):
                        c = half * 4 + c4
                        P.op("dve", lambda e, pb=pb, c=c, c4=c4: e.tensor_scalar(out=xnT32[:, c, :], in0=pb[:, c4 * 128:(c4 + 1) * 128], scalar1=gcols[:, 16 + c:17 + c], scalar2=None, op0=ALU.mult), reads=[Rp, Rc], writes=[RxnT32])
                pb, Rp = rot()
                for c in range(8):
                    P.op("pe", lambda e, pb=pb, c=c: e.matmul(pb[:, 0:20], lhsT=xnT32[:, c, :], rhs=wr32[:, c, :], start=(c == 0), stop=(c == 7)), reads=[RxnT32, Rc], writes=[Rp])
                lg = rl[:, 0:20]; gmx = rl[:, 20:21]; ngmx = rl[:, 21:22]; gex = rl[:, 22:26]; gsum = rl[:, 26:27]; gw = rl[:, 27:28]
                ohg = rl[:, 28:32]; sel = rl[:, 32:36]; m1 = rl[:, 36:37]; oh1 = rl[:, 37:41]; sel2 = rl[:, 41:45]; m2 = rl[:, 45:46]
                oh2 = rl[:, 46:50]; ee = rl[:, 50:51]; p1 = rl[:, 51:52]; p2 = rl[:, 52:53]; cw = rl[:, 53:57]; nm1 = rl[:, 57:58]; cw2 = rl[:, 58:62]
                RW = dict(reads=[Rrl], writes=[Rrl])
                P.op("dve", lambda e, pb=pb: e.tensor_tensor(out=lg, in0=pb[:, 0:20], in1=rbias[:], op=ALU.add), reads=[Rp, Rc], writes=[Rrl])
                P.op("dve", lambda e: e.tensor_reduce(out=gmx, in_=lg[:, 0:4], axis=AX, op=ALU.max), **RW)
                P.op("dve", lambda e: e.tensor_scalar(out=ngmx, in0=gmx, scalar1=-1.0, scalar2=None, op0=ALU.mult), **RW)
                P.op("act", lambda e: e.activation(out=gex, in_=lg[:, 0:4], func=AF.Exp, bias=ngmx, scale=1.0, accum_out=gsum), **RW)
                P.op("dve", lambda e: e.reciprocal(out=gw, in_=gsum), **RW)
                P.op("dve", lambda e: e.tensor_scalar(out=ohg, in0=lg[:, 0:4], scalar1=gmx, scalar2=None, op0=ALU.is_equal), **RW)
                P.op("dve", lambda e: e.tensor_scalar(out=sel, in0=lg[:, 4:8], scalar1=ohg[:, 0:1], scalar2=None, op0=ALU.mult), **RW)
                for g in range(1, 4):
                    P.op("dve", lambda e, g=g: e.scalar_tensor_tensor(out=sel, in0=lg[:, 4 + 4 * g:8 + 4 * g], scalar=ohg[:, g:g + 1], in1=sel, op0=ALU.mult, op1=ALU.add), **RW)
                P.op("dve", lambda e: e.tensor_reduce(out=m1, in_=sel, axis=AX, op=ALU.max), **RW)
                P.op("dve", lambda e: e.tensor_scalar(out=oh1, in0=sel, scalar1=m1, scalar2=None, op0=ALU.is_equal), **RW)
                P.op("dve", lambda e: e.scalar_tensor_tensor(out=sel2, in0=oh1, scalar=NINF, in1=sel, op0=ALU.mult, op1=ALU.add), **RW)
                P.op("dve", lambda e: e.tensor_reduce(out=m2, in_=sel2, axis=AX, op=ALU.max), **RW)
                P.op("dve", lambda e: e.tensor_scalar(out=oh2, in0=sel2, scalar1=m2, scalar2=None, op0=ALU.is_equal), **RW)
                P.op("dve", lambda e: e.tensor_scalar(out=nm1, in0=m1, scalar1=-1.0, scalar2=None, op0=ALU.mult), **RW)
                P.op("act", lambda e: e.activation(out=ee, in_=m2, func=AF.Exp, bias=nm1, scale=1.0), **RW)
                P.op("dve", lambda e: e.tensor_scalar(out=p1, in0=ee, scalar1=1.0, scalar2=None, op0=ALU.add), **RW)
                P.op("dve", lambda e: e.reciprocal(out=p1, in_=p1), **RW)
                P.op("dve", lambda e: e.tensor_tensor(out=p2, in0=ee, in1=p1, op=ALU.mult), **RW)
                P.op("dve", lambda e: e.tensor_scalar(out=cw, in0=oh1, scalar1=p1, scalar2=None, op0=ALU.mult), **RW)
                P.op("dve", lambda e: e.scalar_tensor_tensor(out=cw2, in0=oh2, scalar=p2, in1=cw, op0=ALU.mult, op1=ALU.add), **RW)
                P.op("dve", lambda e: e.tensor_scalar(out=cw, in0=cw2, scalar1=gw, scalar2=None, op0=ALU.mult), **RW)
                for g in range(4):
                    P.op("dve", lambda e, g=g, t=t: e.tensor_scalar(out=comb[t][:, g * 4:(g + 1) * 4], in0=cw, scalar1=ohg[:, g:g + 1], scalar2=None, op0=ALU.mult), reads=[Rrl], writes=[Rcomb[t]])
            for ex in range(16):
                j = ex % 2
                base = j * 12288
                w1e = r1[:, base:base + 4096].rearrange("p (c n) -> p c n", c=8)
                w3e = r1[:, base + 4096:base + 8192].rearrange("p (c n) -> p c n", c=8)
                w2e = r1[:, base + 8192:base + 12288].rearrange("p (c n) -> p c n", c=4)
                if ex == 0:
                    gtok = []
                    for R_ in (Rsc, Rjk, RjkA):
                        if R_.w is not None:
                            gtok.append(R_.w)
                        gtok += list(R_.r)
                    P.wait_tokens("sp", gtok)
                P.dma("sp", lambda e, ex=ex, w1e=w1e: e.dma_start(out=w1e, in_=w1_b[ex * D:(ex + 1) * D, :].rearrange("(c p) n -> p c n", p=128)), reads=[Rw_b], writes=[Rm1[j]])
                P.dma("sp", lambda e, ex=ex, w3e=w3e: e.dma_start(out=w3e, in_=w3_b[ex * D:(ex + 1) * D, :].rearrange("(c p) n -> p c n", p=128)), reads=[Rw_b], writes=[Rm3[j]])
                P.dma("sp", lambda e, ex=ex, w2e=w2e: e.dma_start(out=w2e, in_=w2_b[ex * 512:(ex + 1) * 512, :].rearrange("(c p) n -> p c n", p=128)), reads=[Rw_b], writes=[Rm2[j]])
                for c in range(4):
                    pa, Rpa = rot()
                    for k in range(8):
                        P.op("pe", lambda e, pa=pa, k=k, c=c, w1e=w1e: e.matmul(pa[:, 0:N], lhsT=w1e[:, k, c * 128:(c + 1) * 128], rhs=xnT[:, k, :], start=(k == 0), stop=(k == 7)), reads=[Rm1[j], RxnT], writes=[Rpa])
                    pb, Rp = rot()
                    for k in range(8):
                        P.op("pe", lambda e, pb=pb, k=k, c=c, w3e=w3e: e.matmul(pb[:, 0:N], lhsT=w3e[:, k, c * 128:(c + 1) * 128], rhs=xnT[:, k, :], start=(k == 0), stop=(k == 7)), reads=[Rm3[j], RxnT], writes=[Rp])
                    sj = c % 2
                    P.op("act", lambda e, pa=pa, sj=sj: e.activation(out=sA[sj][:], in_=pa[:, 0:N], func=AF.Silu), reads=[Rpa], writes=[RsA[sj]])
                    P.op("dve", lambda e, pb=pb, sj=sj, c=c, j=j: e.tensor_tensor(out=hT[j][:, c, :], in0=pb[:, 0:N], in1=sA[sj][:], op=ALU.mult), reads=[Rp, RsA[sj]], writes=[RhT[j]])
                for t in range(QB):
                    for n2 in range(2):
                        pb, Rp = rot()
                        for c in range(4):
                            P.op("pe", lambda e, pb=pb, c=c, t=t, n2=n2, j=j, w2e=w2e: e.matmul(pb[:], lhsT=hT[j][:, c, t * 128:(t + 1) * 128], rhs=w2e[:, c, n2 * 512:(n2 + 1) * 512], start=(c == 0), stop=(c == 3)), reads=[RhT[j], Rm2[j]], writes=[Rp])
                        P.op("dve", lambda e, pb=pb, t=t, n2=n2, ex=ex: e.scalar_tensor_tensor(out=xh[:, t, n2 * 512:(n2 + 1) * 512], in0=pb[:], scalar=comb[t][:, ex:ex + 1], in1=xh[:, t, n2 * 512:(n2 + 1) * 512], op0=ALU.mult, op1=ALU.add),
                             reads=[Rp, Rcomb[t], Rxh[t]], writes=[Rxh[t]])
            for t in range(QB):
                qi = I * QB + t
                ss = ss_t[:, 0:1]; rs = rs_t[:, 0:1]
                P.op("act", lambda e, t=t, ss=ss: e.activation(out=sqj[:], in_=xh[:, t, :], func=AF.Square, accum_out=ss), reads=[Rxh[t]], writes=[Rsqj, Rstat[0]])
                P.op("dve", lambda e, ss=ss, rs=rs: e.tensor_scalar(out=rs, in0=ss, scalar1=1.0 / D, scalar2=1e-6, op0=ALU.mult, op1=ALU.add), reads=[Rstat[0]], writes=[Rstat[0]])
                P.op("act", lambda e, rs=rs: e.activation(out=rs, in_=rs, func=AF.Sqrt), reads=[Rstat[0]], writes=[Rstat[0]])
                P.op("dve", lambda e, rs=rs: e.reciprocal(out=rs, in_=rs), reads=[Rstat[0]], writes=[Rstat[0]])
                P.op("dve", lambda e, t=t, rs=rs: e.scalar_tensor_tensor(out=ot[:], in0=xh[:, t, :], scalar=rs, in1=gfin[:], op0=ALU.mult, op1=ALU.mult), reads=[Rxh[t], Rstat[0], Rc], writes=[Rot])
                out_tokens.append(P.dma("sp", lambda e, qi=qi: e.dma_start(out=out_d[qi * 128:(qi + 1) * 128, :], in_=ot[:]), reads=[Rot]))
        P.wait_tokens("sp", out_tokens)
        P.emit()
    return nc, P


def _host_consts(S, parity):
    pos = np.arange(S, dtype=np.float32) - (0 if parity else 128)
    half = 32
    inv = (10000.0 ** (-np.arange(half, dtype=np.float32) / half)).astype(np.float32)
    ang = pos[:, None] * inv[None, :]
    cs = np.concatenate([np.cos(ang), np.sin(ang)], axis=1).astype(np.float32)
    q = np.arange(128)[:, None]; k = np.arange(128)[None, :]
    cm = np.zeros((128, 256), np.float32)
    cm[:, 128:] = np.where(k <= q, 0.0, NINF)
    m0 = np.full((128, 128), 0.0 if parity else NINF, np.float32)
    mb = np.zeros((128, 24, 128), np.float32)
    for g, (W, d) in enumerate(B_PAT):
        for jj in range(B_NB[g] + 1):
            diff = 128 * jj + q - k
            ok = (diff >= 0) & (diff <= W) & (diff % d == 0)
            mb[:, B_MOFF[g] + jj, :] = np.where(ok, 0.0, NEG)
    mb0 = mb.copy() if parity else np.full_like(mb, NEG)
    return cs, cm, m0, mb.reshape(128, -1), mb0.reshape(128, -1)


def _prep_inputs(inputs, S, n_cores=8):
    f = lambda a: np.ascontiguousarray(np.asarray(a, dtype=np.float32))
    x = f(inputs["x"]); mem = f(inputs["mem"])
    w_in = f(inputs["w_in"])[0]
    o = np.cumsum([0, 512, 128, 128, 256, 64, 4, 1536, 1536, 1536, 512])
    aq, ak, av, iq, ik, iw, bq, bk, bv, mq = [w_in[:, o[i]:o[i + 1]] for i in range(10)]
    wk = np.ascontiguousarray(np.concatenate([ak, ik, av, bk, bv], axis=1))
    wq = np.ascontiguousarray(np.concatenate([aq, iq, iw, bq, mq], axis=1))
    wg0 = f(inputs["w_gate"])[0]
    wg = np.ascontiguousarray(wg0.reshape(D, 3, 8, 128).transpose(0, 2, 1, 3).reshape(D, 3072))
    bg0 = f(inputs["b_gate"])[0].reshape(3, 8, 128)
    bgate = np.ascontiguousarray(bg0.transpose(2, 1, 0).reshape(128, 24))
    wbr = np.ascontiguousarray(f(inputs["w_branch"])[0].reshape(1536, D))
    wo = f(inputs["w_out"])[0]
    wmem = f(inputs["w_mem_kv"])[0]
    w1 = np.ascontiguousarray(f(inputs["w1"])[0].reshape(16 * D, 512))
    w3 = np.ascontiguousarray(f(inputs["w3"])[0].reshape(16 * D, 512))
    w2 = np.ascontiguousarray(f(inputs["w2"])[0].reshape(16 * 512, D))
    wsub = f(inputs["w_sub"])[0]
    wr = np.ascontiguousarray(np.concatenate([f(inputs["w_group"])[0], wsub.transpose(1, 0, 2).reshape(D, 16)], axis=1))
    gc = lambda g: np.asarray(g, np.float32).reshape(8, 128).T
    gcols = np.ascontiguousarray(np.concatenate([gc(f(inputs["g_mix"])[0]), gc(f(inputs["g_mem"])[0]), gc(f(inputs["g_ffn"])[0])], axis=1))
    gfin = np.ascontiguousarray(np.broadcast_to(f(inputs["g_final"])[None, :], (128, D)))
    rb = np.concatenate([f(inputs["b_group"])[0], f(inputs["b_sub"])[0].reshape(16)])
    rbias = np.ascontiguousarray(np.broadcast_to(rb[None, :], (128, 20)))
    ident = np.eye(128, dtype=np.float32)
    shared = dict(wk=wk, wq=wq, wg=wg, wbr=wbr, wo=wo, wmem=wmem, w1=w1, w3=w3, w2=w2, wr=wr, gcols=gcols,
                  bgate=bgate, gfin=gfin, rbias=rbias, ident=ident)
    in_maps = []
    for c in range(n_cores):
        b, p = c // 2, c % 2
        if p == 0:
            xs = np.concatenate([np.zeros((128, D), np.float32), x[b, :S - 128]], axis=0)
        else:
            xs = x[b, :S]
        cs, cm, m0, mb, mb0 = _host_consts(S, p)
        m = dict(shared)
        m.update(xs=np.ascontiguousarray(xs), cs=cs, memx=np.ascontiguousarray(mem[b]), dsa_cm=cm, dsa_m0=m0, mbias=mb, mb0=mb0)
        in_maps.append(m)
    return in_maps


_CACHE = {}


def run(inputs, S, QB=2, dbg=False):
    key = (S, QB, dbg)
    if key not in _CACHE:
        _CACHE[key] = build(S, QB, dbg)
    nc, P = _CACHE[key]
    in_maps = _prep_inputs(inputs, S)
    res = run_bass_kernel_spmd(nc, in_maps, core_ids=list(range(8)))
    B = 4
    out = np.zeros((B, S, D), np.float32)
    for c in range(8):
        b, p = c // 2, c % 2
        o = np.asarray(res.results[c]["out"]).reshape(S // 256, 128, D)
        out[b].reshape(S // 256, 2, 128, D)[:, p] = o
    return out, res


def kernel(**inputs):
    out, _ = run(inputs, 8192, QB=2)
    return out
```

```python
import contextlib
import os
RMS_CUT = int(os.environ.get('RMS_CUT', '9'))
BM_CUT = int(os.environ.get('BM_CUT', '9'))
import numpy as np
import concourse.bass as bass
import concourse.mybir as mybir
from concourse.bass_utils import run_bass_kernel_spmd

F32 = mybir.dt.float32
BF16 = mybir.dt.bfloat16
AF = mybir.ActivationFunctionType
ALU = mybir.AluOpType
AX = mybir.AxisListType.X

D = 1024
NEG = -30000.0
NINF = -1.0e30
NBIS = 17
B_PAT = ((128, 1), (512, 4), (2048, 16))
B_NB = (1, 4, 16)
B_MOFF = (0, 2, 7)
ENGS = ("pe", "act", "dve", "pool", "sp")
EPOCH = 12000


class Res:
    __slots__ = ("w", "r", "excl")

    def __init__(self, excl=False):
        self.w = None
        self.r = []
        self.excl = excl


class Prog:
    def __init__(self, nc, n_dma_sems=24):
        self.nc = nc
        self.q = {e: [] for e in ENGS}
        self.sem = {}
        self.cnt = {}
        self.key_eng = {}
        self.cur = {}
        for e in ENGS:
            self._new_epoch(e, 0)
        self.ndma = n_dma_sems
        for k in range(n_dma_sems):
            key = "dma%d" % k
            self.sem[key] = nc.alloc_semaphore(name="d%d" % k)
            self.cnt[key] = 0
            self.key_eng[key] = "dma"
        self.dma_rr = 0
        self.known = {e: {} for e in ENGS}
        self.nwaits = 0
        self.nops = 0

    def _new_epoch(self, e, k):
        key = "%s#%d" % (e, k)
        self.sem[key] = self.nc.alloc_semaphore(name="s_%s_%d" % (e, k))
        self.cnt[key] = 0
        self.key_eng[key] = e
        self.cur[e] = (key, k)

    def _need(self, eng, tok, waits):
        if tok is None:
            return
        key, val = tok
        if self.known[eng].get(key, 0) >= val:
            return
        if waits.get(key, 0) < val:
            waits[key] = val

    def _deps(self, eng, reads, writes, same_sync):
        waits = {}
        for r in reads:
            if r.excl:
                for t in r.r:
                    if self.key_eng[t[0]] != eng:
                        self._need(eng, t, waits)
            if r.w is not None:
                if self.key_eng[r.w[0]] == eng and not same_sync:
                    continue
                self._need(eng, r.w, waits)
        for w in writes:
            if w.w is not None:
                if not (self.key_eng[w.w[0]] == eng and not same_sync):
                    self._need(eng, w.w, waits)
            for t in w.r:
                if self.key_eng[t[0]] == eng:
                    continue
                self._need(eng, t, waits)
        for k, v in waits.items():
            self.known[eng][k] = v
        return waits

    def _commit(self, tok, reads, writes):
        for r in reads:
            r.r.append(tok)
            if len(r.r) > 48:
                best = {}
                for k, v in r.r:
                    if best.get(k, 0) < v:
                        best[k] = v
                r.r = list(best.items())
        for w in writes:
            w.w = tok
            w.r = []

    def op(self, eng, fn, reads=(), writes=(), same_sync=True):
        if eng == "pe":
            same_sync = False
        waits = self._deps(eng, reads, writes, same_sync)
        key, k = self.cur[eng]
        if self.cnt[key] >= EPOCH:
            self._new_epoch(eng, k + 1)
            key, k = self.cur[eng]
        self.cnt[key] += 1
        tok = (key, self.cnt[key])
        self._commit(tok, reads, writes)
        self.q[eng].append((waits, fn, (key, 1)))
        self.nwaits += len(waits)
        self.nops += 1
        return tok

    def dma(self, qeng, fn, reads=(), writes=()):
        k = self.dma_rr
        self.dma_rr = (self.dma_rr + 1) % self.ndma
        key = "dma%d" % k
        waits = self._deps(qeng, reads, writes, True)
        prev = self.cnt[key]
        if prev > 0 and self.known[qeng].get(key, 0) < prev:
            waits[key] = max(waits.get(key, 0), prev)
            self.known[qeng][key] = prev
        self.cnt[key] += 16
        tok = (key, self.cnt[key])
        self._commit(tok, reads, writes)
        self.q[qeng].append((waits, fn, (key, 16)))
        self.nwaits += len(waits)
        self.nops += 1
        return tok

    def wait_tokens(self, eng, toks):
        waits = {}
        for t in toks:
            self._need(eng, t, waits)
        for k, v in waits.items():
            self.known[eng][k] = v
        self.q[eng].append((waits, None, None))

    def emit(self):
        nc = self.nc
        with nc.Block() as block:
            def mk(ename):
                def body(e):
                    for waits, fn, inc in self.q[ename]:
                        for k, v in waits.items():
                            e.wait_ge(self.sem[k], v)
                        if fn is not None:
                            ins = fn(e)
                            ins.then_inc(self.sem[inc[0]], inc[1])
                return body
            block.tensor(mk("pe"))
            block.scalar(mk("act"))
            block.vector(mk("dve"))
            block.gpsimd(mk("pool"))
            block.sync(mk("sp"))


WK_COLS = 3392
WQ_COLS = 2820
K_SLABS = [("a", 0, 320)] + [("bk%d" % g, 320 + 512 * g, 512) for g in range(3)] + \
          [("bv%d" % g, 1856 + 512 * g, 512) for g in range(3)]
Q_SLABS = [("aq", 0, 512), ("iq", 512, 260)] + [("bq%d" % g, 772 + 512 * g, 512) for g in range(3)] + \
          [("mq", 2308, 512)]


def build(S, QB, dbg=False, stop=None):
    NBLK = S // 128
    NQ = NBLK // 2
    NIT = NQ // QB
    N = QB * 128
    assert NBLK % 4 == 0 and NQ % QB == 0

    nc = bass.Bass("TRN2", target_bir_lowering=False)

    def din(name, shape, dt=F32):
        return nc.dram_tensor(name, shape, dt, kind="ExternalInput").ap()

    def dscr(name, shape, dt=BF16):
        return nc.dram_tensor(name, shape, dt).ap()

    xs = din("xs", [S, D])
    cs = din("cs", [S, 64])
    memx = din("memx", [256, D])
    wk = din("wk", [D, WK_COLS])
    wq = din("wq", [D, WQ_COLS])
    wg = din("wg", [D, 3072])
    wbr = din("wbr", [1536, D])
    wo = din("wo", [D, D])
    wmem = din("wmem", [D, 1024])
    w1 = din("w1", [16 * D, 512])
    w3 = din("w3", [16 * D, 512])
    w2 = din("w2", [16 * 512, D])
    wr = din("wr", [D, 20])
    gcols_d = din("gcols", [128, 24])
    bgate_d = din("bgate", [128, 24])
    gfin_d = din("gfin", [128, D])
    rbias_d = din("rbias", [128, 20])
    ident_d = din("ident", [128, 128])
    cm_d = din("dsa_cm", [128, 256])
    m0_d = din("dsa_m0", [128, 128])
    mb_d = din("mbias", [128, 24 * 128])
    mb0_d = din("mb0", [128, 24 * 128])
    out_d = nc.dram_tensor("out", [NQ * 128, D], F32, kind="ExternalOutput").ap()
    dbg_d = {}
    if dbg:
        for nm, shp in (("d_ya", [64, 8 * N]), ("d_yb", [64, 8 * N]), ("d_ym", [128, 4 * N]), ("d_h", [128, QB * D])):
            dbg_d[nm] = nc.dram_tensor(nm, shp, F32, kind="ExternalOutput").ap()

    wk_b = dscr("wk_b", [D, WK_COLS])
    wq_b = dscr("wq_b", [D, WQ_COLS])
    wg_b = dscr("wg_b", [D, 3072])
    wbr_b = dscr("wbr_b", [1536, D])
    wo_b = dscr("wo_b", [D, D])
    wmem_b = dscr("wmem_b", [D, 1024])
    w1_b = dscr("w1_b", [16 * D, 512])
    w3_b = dscr("w3_b", [16 * D, 512])
    w2_b = dscr("w2_b", [16 * 512, D])
    KaT_d = dscr("KaT_d", [128, S])
    kiT_d = dscr("kiT_d", [64, S])
    Va_d = dscr("Va_d", [S, 256])
    bkT_d = dscr("bkT_d", [128, 3, 4, S])
    Vb_d = dscr("Vb_d", [S, 3, 1024])

    P = Prog(nc)
    es = contextlib.ExitStack()
    with es:
        def sb(name, shape, dt=F32):
            return es.enter_context(nc.sbuf_tensor("s_" + name, shape, dt))

        pbank = [es.enter_context(nc.psum_tensor("pb%d" % i, [128, 512], F32)) for i in range(8)]
        Rb = [Res(excl=True) for _ in range(8)]
        rot_state = [0]

        def rot():
            k = 4 + rot_state[0]
            rot_state[0] = (rot_state[0] + 1) % 4
            return pbank[k], Rb[k]

        identf = sb("identf", [128, 128]); identb = sb("identb", [128, 128], BF16)
        i4b = sb("i4b", [128, 512], BF16)
        ones_b = sb("ones_b", [128, 128], BF16)
        gcols = sb("gcols", [128, 24]); bgate = sb("bgate", [128, 24])
        gfin = sb("gfin", [128, D]); rbias = sb("rbias", [128, 20])
        cm = sb("cm", [128, 256]); m0 = sb("m0", [128, 128])
        mbt = sb("mbt", [128, 24 * 128], BF16); mb0t = sb("mb0t", [128, 24 * 128], BF16)
        pow2 = sb("pow2", [128, NBIS + 1])
        wr32 = sb("wr32", [128, 8, 20])
        kmT = sb("kmT", [128, 4, 256], BF16); vmb = sb("vmb", [128, 2, 512], BF16)
        Rc = Res()

        def ld_const(dst, src):
            P.dma("sp", lambda e: e.dma_start(out=dst, in_=src), writes=[Rc])

        ld_const(identf[:], ident_d)
        ld_const(gcols[:], gcols_d); ld_const(bgate[:], bgate_d); ld_const(gfin[:], gfin_d)
        ld_const(rbias[:], rbias_d); ld_const(cm[:], cm_d); ld_const(m0[:], m0_d)
        ld_const(wr32[:], wr.rearrange("(c p) n -> p c n", p=128))
        P.op("dve", lambda e: e.tensor_copy(out=identb[:], in_=identf[:]), reads=[Rc], writes=[Rc])
        for j in range(4):
            P.op("dve", lambda e, j=j: e.tensor_copy(out=i4b[:, j * 128:(j + 1) * 128], in_=identf[:]), reads=[Rc], writes=[Rc])
        P.op("dve", lambda e: e.memset(ones_b[:], 1.0), writes=[Rc])
        for i in range(NBIS + 1):
            P.op("dve", lambda e, i=i: e.memset(pow2[:, i:i + 1], 2.0 ** (-i)), writes=[Rc])

        def finish0():
            toks = [("dma%d" % k, P.cnt["dma%d" % k]) for k in range(P.ndma) if P.cnt["dma%d" % k] > 0]
            toks += [(P.cur[e_][0], P.cnt[P.cur[e_][0]]) for e_ in ("pe", "act", "dve", "pool") if P.cnt[P.cur[e_][0]] > 0]
            P.wait_tokens("sp", toks)
            P.emit()
        if stop == "c":
            finish0(); return nc, P
        Rw_b = Res()
        conv_hist = []

        def convert(src, dst):
            fs = src.rearrange("r c -> (r c)").rearrange("(n k) -> n k", k=1024)
            fd = dst.rearrange("r c -> (r c)").rearrange("(n k) -> n k", k=1024)
            n = fs.shape[0]
            for r0 in range(0, n, 1024):
                r1_ = min(n, r0 + 1024)
                if len(conv_hist) >= 4:
                    P.wait_tokens("pool", [conv_hist[-4]])
                conv_hist.append(P.dma("pool", lambda e, r0=r0, r1_=r1_: e.dma_start(out=fd[r0:r1_, :], in_=fs[r0:r1_, :])))

        for s_, d_ in ((wmem, wmem_b), (wk, wk_b), (wq, wq_b), (wg, wg_b), (wbr, wbr_b), (wo, wo_b),
                       (w1, w1_b), (w3, w3_b), (w2, w2_b)):
            convert(s_, d_)
        conv_tokens = []
        for k in range(P.ndma):
            key = "dma%d" % k
            if P.cnt[key] > 0:
                conv_tokens.append((key, P.cnt[key]))

        if stop == "conv":
            finish0(); return nc, P
        ss_t = sb("ss_t", [128, 4]); rs_t = sb("rs_t", [128, 4])
        sqj = sb("sqj", [128, D], BF16)
        xb_t = [sb("xb0", [128, D], BF16)] * 2
        Rstat = [Res() for _ in range(4)]
        Rsqj = Res()
        Rxb = [Res()] * 2
        rms_ctr = [0]

        def rms_T(x_ap, Rx, gofs, dst_fn, Rdst):
            i = rms_ctr[0] % 4
            j = rms_ctr[0] % 2
            rms_ctr[0] += 1
            ss = ss_t[:, i:i + 1]; rs = rs_t[:, i:i + 1]
            P.op("act", lambda e: e.activation(out=sqj[:], in_=x_ap, func=AF.Square, accum_out=ss),
                 reads=[Rx], writes=[Rsqj, Rstat[i]])
            if RMS_CUT <= 1:
                return i
            P.op("dve", lambda e: e.tensor_scalar(out=rs, in0=ss, scalar1=1.0 / D, scalar2=1e-6, op0=ALU.mult, op1=ALU.add),
                 reads=[Rstat[i]], writes=[Rstat[i]])
            P.op("act", lambda e: e.activation(out=rs, in_=rs, func=AF.Sqrt),
                 reads=[Rstat[i]], writes=[Rstat[i]])
            P.op("dve", lambda e: e.reciprocal(out=rs, in_=rs), reads=[Rstat[i]], writes=[Rstat[i]])
            if RMS_CUT <= 2:
                return i
            xb = xb_t[j]
            P.op("dve", lambda e: e.tensor_scalar(out=xb[:], in0=x_ap, scalar1=rs, scalar2=None, op0=ALU.mult),
                 reads=[Rx, Rstat[i]], writes=[Rxb[j]])
            if RMS_CUT <= 3:
                return i
            for half in range(2):
                pb, Rp = rot()
                pbb = pb[:].bitcast(BF16)
                for c4 in range(4):
                    c = half * 4 + c4
                    P.op("pe", lambda e, c=c, c4=c4, pbb=pbb: e.transpose(out=pbb[:, c4 * 128:(c4 + 1) * 128], in_=xb[:, c * 128:(c + 1) * 128], identity=identb[:]),
                         reads=[Rxb[j], Rc], writes=[Rp])
                if RMS_CUT <= 4:
                    continue
                for c4 in range(4):
                    c = half * 4 + c4
                    eng = "dve"
                    if eng == "dve":
                        P.op("dve", lambda e, c=c, c4=c4, pbb=pbb: e.tensor_scalar(out=dst_fn(c), in0=pbb[:, c4 * 128:(c4 + 1) * 128], scalar1=gcols[:, gofs + c:gofs + c + 1], scalar2=None, op0=ALU.mult),
                             reads=[Rp, Rc], writes=[Rdst])
                    else:
                        P.op("act", lambda e, c=c, c4=c4, pbb=pbb: e.activation(out=dst_fn(c), in_=pbb[:, c4 * 128:(c4 + 1) * 128], func=AF.Copy, scale=gcols[:, gofs + c:gofs + c + 1]),
                             reads=[Rp, Rc], writes=[Rdst])
            return i

        rt = [sb("rt%d" % i, [128, 256]) for i in range(4)]
        Rrt = [Res() for _ in range(4)]

        def rope(ps_ap, Rps, H, cs_ap, Rcs, out_ap, Rout, perm=False):
            n = H * 32
            if os.environ.get("NOROPE"):
                P.op("act", lambda e: e.copy(out=out_ap, in_=ps_ap), reads=[Rps], writes=[Rout])
                return
            if perm:
                x = ps_ap.rearrange("p (g j t d) -> p g j t d", g=2, t=2, d=32)
                o = out_ap.rearrange("p (j g t d) -> p g j t d", g=2, t=2, d=32)
                x1, x2 = x[:, :, :, 0, :], x[:, :, :, 1, :]
                o1, o2 = o[:, :, :, 0, :], o[:, :, :, 1, :]
                cos = cs_ap[:, 0:32].unsqueeze(1).unsqueeze(1).to_broadcast([128, 2, 4, 32])
                sin = cs_ap[:, 32:64].unsqueeze(1).unsqueeze(1).to_broadcast([128, 2, 4, 32])
                tv = [rt[i][:, 0:n].rearrange("p (g j d) -> p g j d", g=2, d=32) for i in range(4)]
            else:
                x = ps_ap.rearrange("p (h t d) -> p h t d", t=2, d=32)
                o = out_ap.rearrange("p (h t d) -> p h t d", t=2, d=32)
                x1, x2 = x[:, :, 0, :], x[:, :, 1, :]
                o1, o2 = o[:, :, 0, :], o[:, :, 1, :]
                cos = cs_ap[:, 0:32].unsqueeze(1).to_broadcast([128, H, 32])
                sin = cs_ap[:, 32:64].unsqueeze(1).to_broadcast([128, H, 32])
                tv = [rt[i][:, 0:n].rearrange("p (h d) -> p h d", d=32) for i in range(4)]
            P.op("dve", lambda e: e.tensor_tensor(out=tv[0], in0=x1, in1=cos, op=ALU.mult), reads=[Rps, Rcs], writes=[Rrt[0]])
            P.op("dve", lambda e: e.tensor_tensor(out=tv[1], in0=x2, in1=sin, op=ALU.mult), reads=[Rps, Rcs], writes=[Rrt[1]])
            P.op("dve", lambda e: e.tensor_tensor(out=tv[2], in0=x2, in1=cos, op=ALU.mult), reads=[Rps, Rcs], writes=[Rrt[2]])
            P.op("dve", lambda e: e.tensor_tensor(out=tv[3], in0=x1, in1=sin, op=ALU.mult), reads=[Rps, Rcs], writes=[Rrt[3]])
            P.op("dve", lambda e: e.tensor_tensor(out=o1, in0=tv[0], in1=tv[1], op=ALU.subtract), reads=[Rrt[0], Rrt[1]], writes=[Rout])
            P.op("dve", lambda e: e.tensor_tensor(out=o2, in0=tv[2], in1=tv[3], op=ALU.add), reads=[Rrt[2], Rrt[3]], writes=[Rout])

        wslab = [sb("wslab%d" % i, [128, 8, 512], BF16) for i in range(2)]
        Rwslab = [Res(), Res()]
        slab_ctr = [0]

        def load_slab(wsrc_b, c0, ncol):
            i = slab_ctr[0] % 2
            slab_ctr[0] += 1
            src = wsrc_b.rearrange("(c p) n -> p c n", p=128)[:, :, c0:c0 + ncol]
            P.dma("sp", lambda e: e.dma_start(out=wslab[i][:, :, 0:ncol], in_=src), reads=[Rw_b], writes=[Rwslab[i]])
            return wslab[i], Rwslab[i]

        def proj(nT, RnT, tok0, ws, Rws, ncol):
            pb, Rp = rot()
            for c in range(8):
                P.op("pe", lambda e, c=c: e.matmul(pb[:, 0:ncol], lhsT=nT[:, c, tok0:tok0 + 128], rhs=ws[:, c, 0:ncol], start=(c == 0), stop=(c == 7)),
                     reads=[RnT, Rws], writes=[Rp])
            flush_deferred()
            return pb, Rp

        tst = [sb("tst%d" % i, [128, 512], BF16) for i in range(2)]
        Rtst = [Res(), Res()]
        tst_ctr = [0]

        def next_tst():
            i = tst_ctr[0] % 2
            tst_ctr[0] += 1
            return tst[i], Rtst[i]

        deferred = []

        def flush_deferred():
            while deferred:
                deferred.pop(0)()

        def transposes(*a_, **k_):
            deferred.append(lambda: _transposes(*a_, **k_))

        def _transposes(src, Rsrc, ncols_each, nchunks, dst_fn, Rdst, eng="act"):
            pb, Rp = rot()
            pbb = pb[:].bitcast(BF16)
            for c in range(nchunks):
                P.op("pe", lambda e, c=c: e.transpose(out=pbb[0:ncols_each, c * 128:(c + 1) * 128], in_=src[:, c * ncols_each:(c + 1) * ncols_each], identity=identb[:]),
                     reads=[Rsrc, Rc], writes=[Rp])
            for c in range(nchunks):
                if eng == "act":
                    P.op("act", lambda e, c=c: e.copy(out=dst_fn(c), in_=pbb[0:ncols_each, c * 128:(c + 1) * 128]), reads=[Rp], writes=[Rdst])
                else:
                    P.op("dve", lambda e, c=c: e.tensor_copy(out=dst_fn(c), in_=pbb[0:ncols_each, c * 128:(c + 1) * 128]), reads=[Rp], writes=[Rdst])

        for e_ in ("sp", "act", "dve", "pe", "pool"):
            P.wait_tokens(e_, conv_tokens)

        xt = [sb("xt%d" % i, [128, D]) for i in range(2)]
        Rxt = [Res(), Res()]
        cst = [sb("cst%d" % i, [128, 64]) for i in range(4)]
        Rcst = [Res() for _ in range(4)]
        nTk = sb("nTk", [128, 8, 512], BF16)
        RnTk = Res()
        pcs = 0
        for (src, dst) in ((mb_d, mbt), (mb0_d, mb0t)):
            for pc in range(3):
                jx = pcs % 2; pcs += 1
                P.dma("sp", lambda e, src=src, pc=pc, jx=jx: e.dma_start(out=xt[jx][:], in_=src[:, pc * 1024:(pc + 1) * 1024]), writes=[Rxt[jx]])
                P.op("dve", lambda e, dst=dst, pc=pc, jx=jx: e.tensor_copy(out=dst[:, pc * 1024:(pc + 1) * 1024], in_=xt[jx][:]), reads=[Rxt[jx]], writes=[Rc])
        if stop == "m1":
            finish0(); return nc, P
        for mbk in range(2):
            P.dma("sp", lambda e, mbk=mbk: e.dma_start(out=xt[mbk][:], in_=memx[mbk * 128:(mbk + 1) * 128, :]), writes=[Rxt[mbk]])
            rms_T(xt[mbk][:], Rxt[mbk], 8, lambda c, mbk=mbk: nTk[:, c, mbk * 128:(mbk + 1) * 128], RnTk)
        if stop == "m2":
            finish0(); return nc, P
        Rkm = Res()
        for half in range(2):
            ws, Rws = load_slab(wmem_b, half * 512, 512)
            for mbk in range(2):
                pb, Rp = proj(nTk, RnTk, mbk * 128, ws, Rws, 512)
                if half == 0:
                    t_, Rt_ = next_tst()
                    P.op("act", lambda e, t_=t_, pb=pb: e.copy(out=t_[:], in_=pb[:]), reads=[Rp], writes=[Rt_])
                    transposes(t_, Rt_, 128, 4, lambda c, mbk=mbk: kmT[:, c, mbk * 128:(mbk + 1) * 128], Rkm)
                else:
                    P.op("act", lambda e, pb=pb, mbk=mbk: e.copy(out=vmb[:, mbk, :], in_=pb[:]), reads=[Rp], writes=[Rkm])

        def finish():
            toks = [("dma%d" % k, P.cnt["dma%d" % k]) for k in range(P.ndma) if P.cnt["dma%d" % k] > 0]
            toks += [(P.cur[e_][0], P.cnt[P.cur[e_][0]]) for e_ in ("pe", "act", "dve", "pool") if P.cnt[P.cur[e_][0]] > 0]
            P.wait_tokens("sp", toks)
            P.emit()

        flush_deferred()
        if stop == "p0":
            finish(); return nc, P
        r1 = sb("r1", [128, 24576], BF16)
        KaTst = r1[:, 0:512]; kiTst = r1[0:64, 512:1024]
        Vast = r1[:, 1024:2048].rearrange("p (b n) -> p b n", b=4)
        bkTst = [r1[:, 2048 + i * 2048:2048 + (i + 1) * 2048].rearrange("p (c n) -> p c n", c=4) for i in range(2)]
        Vbst = [r1[:, 6144 + i * 4096:6144 + (i + 1) * 4096].rearrange("p (b n) -> p b n", b=4) for i in range(2)]
        RKaTst, RkiTst, RVast = Res(), Res(), Res()
        RbkTst = [Res(), Res()]; RVbst = [Res(), Res()]
        RKd = Res()
        P.op("pool", lambda e: e.memset(Vast, 1.0), writes=[RVast])
        for i in range(2):
            P.op("pool", lambda e, i=i: e.memset(Vbst[i], 1.0), writes=[RVbst[i]])

        for T in range(NBLK // 4):
            for bl in range(4):
                sbk = T * 4 + bl
                j = sbk % 2
                P.dma("sp", lambda e, sbk=sbk, j=j: e.dma_start(out=xt[j][:], in_=xs[sbk * 128:(sbk + 1) * 128, :]), writes=[Rxt[j]])
                P.dma("sp", lambda e, sbk=sbk, bl=bl: e.dma_start(out=cst[bl][:], in_=cs[sbk * 128:(sbk + 1) * 128, :]), writes=[Rcst[bl]])
                rms_T(xt[j][:], Rxt[j], 0, lambda c, bl=bl: nTk[:, c, bl * 128:(bl + 1) * 128], RnTk)
            for si, (nm, c0, ncol) in enumerate(K_SLABS):
                ws, Rws = load_slab(wk_b, c0, ncol)
                for bl in range(4):
                    pb, Rp = proj(nTk, RnTk, bl * 128, ws, Rws, ncol)
                    if nm == "a":
                        t_, Rt_ = next_tst()
                        rope(pb[:, 0:192], Rp, 3, cst[bl][:], Rcst[bl], t_[:, 0:192], Rt_)
                        Vv = Vast[:, bl, :].rearrange("p (g t d) -> p g t d", g=2, t=2)[:, :, 0, :]
                        P.op("act", lambda e, pb=pb, Vv=Vv: e.copy(out=Vv, in_=pb[:, 192:320].rearrange("p (g d) -> p g d", g=2)), reads=[Rp], writes=[RVast])
                        transposes(t_, Rt_, 128, 1, lambda c, bl=bl: KaTst[:, bl * 128:(bl + 1) * 128], RKaTst)
                        def ki_tr(t_=t_, Rt_=Rt_, bl=bl):
                            pb2, Rp2 = rot()
                            pbb2 = pb2[:].bitcast(BF16)
                            P.op("pe", lambda e: e.transpose(out=pbb2[0:64, 0:128], in_=t_[:, 128:192], identity=identb[:]), reads=[Rt_, Rc], writes=[Rp2])
                            P.op("act", lambda e: e.copy(out=kiTst[:, bl * 128:(bl + 1) * 128], in_=pbb2[0:64, 0:128]), reads=[Rp2], writes=[RkiTst])
                        deferred.append(ki_tr)
                    elif nm.startswith("bk"):
                        g = int(nm[2]); jb = g % 2
                        t_, Rt_ = next_tst()
                        rope(pb[:, 0:512], Rp, 8, cst[bl][:], Rcst[bl], t_[:, 0:512], Rt_)
                        transposes(t_, Rt_, 128, 4, lambda c, bl=bl, jb=jb: bkTst[jb][:, c, bl * 128:(bl + 1) * 128], RbkTst[jb])
                    else:
                        g = int(nm[2]); jb = g % 2
                        Vv = Vbst[jb][:, bl, :].rearrange("p (h t d) -> p h t d", h=8, t=2)[:, :, 0, :]
                        P.op("act", lambda e, pb=pb, Vv=Vv: e.copy(out=Vv, in_=pb[:, 0:512].rearrange("p (h d) -> p h d", h=8)), reads=[Rp], writes=[RVbst[jb]])
                def stores(nm=nm, T=T):
                    if nm == "a":
                        P.dma("pool", lambda e, T=T: e.dma_start(out=KaT_d[:, T * 512:(T + 1) * 512], in_=KaTst), reads=[RKaTst])
                        P.dma("pool", lambda e, T=T: e.dma_start(out=kiT_d[:, T * 512:(T + 1) * 512], in_=kiTst), reads=[RkiTst])
                        P.dma("pool", lambda e, T=T: e.dma_start(out=Va_d[T * 512:(T + 1) * 512, :].rearrange("(b p) n -> p b n", p=128), in_=Vast), reads=[RVast])
                    elif nm.startswith("bk"):
                        g = int(nm[2]); jb = g % 2
                        P.dma("pool", lambda e, T=T, g=g, jb=jb: e.dma_start(out=bkT_d[:, g, :, T * 512:(T + 1) * 512], in_=bkTst[jb]), reads=[RbkTst[jb]])
                    else:
                        g = int(nm[2]); jb = g % 2
                        P.dma("pool", lambda e, T=T, g=g, jb=jb: e.dma_start(out=Vb_d[T * 512:(T + 1) * 512, g, :].rearrange("(b p) n -> p b n", p=128), in_=Vbst[jb]), reads=[RVbst[jb]])
                deferred.append(stores)
        flush_deferred()
        kd_tokens = [("dma%d" % k, P.cnt["dma%d" % k]) for k in range(P.ndma) if P.cnt["dma%d" % k] > 0]
        P.wait_tokens("sp", kd_tokens)

        if stop == "p1":
            finish(); return nc, P
        xh = sb("xh", [128, QB, D]); Rxh = [Res() for _ in range(QB)]
        nTq = nTk[:, :, 0:N]; RnTq = RnTk
        csq = [sb("csq%d" % i, [128, 64]) for i in range(QB)]; Rcsq = [Res() for _ in range(QB)]
        QaT = [sb("QaT%d" % i, [128, 4, 128], BF16) for i in range(QB)]; RQaT = [Res() for _ in range(QB)]
        iqT = [sb("iqT%d" % i, [64, 4, 128], BF16) for i in range(QB)]; RiqT = [Res() for _ in range(QB)]
        bqT = [sb("bqT%d" % i, [128, 3, 4, 128], BF16) for i in range(QB)]; RbqT = [Res() for _ in range(QB)]
        mqT = [sb("mqT%d" % i, [128, 4, 128], BF16) for i in range(QB)]; RmqT = [Res() for _ in range(QB)]
        wv = [sb("wv%d" % i, [128, 12]) for i in range(QB)]; Rwv = [Res() for _ in range(QB)]
        dg = [sb("dg%d" % i, [128, 4, 128], BF16) for i in range(QB)]; Rdg = [Res() for _ in range(QB)]
        yaT = sb("yaT", [64, 8, N], BF16); ybT = sb("ybT", [64, 8, N], BF16); ymT = sb("ymT", [128, 4, N], BF16)
        RyaT, RybT, RymT = Res(), Res(), Res()
        score = r1[:, 0:16384].bitcast(F32)
        junk = r1[:, 16384:24576]
        Rsc, Rjk = Res(), Res()
        Rmw = [Res(), Res()]
        Rm1 = [Res(), Res()]; Rm3 = [Res(), Res()]; Rm2 = [Res(), Res()]
        RMOE = [Rmw[0], Rmw[1], Rm1[0], Rm1[1], Rm3[0], Rm3[1], Rm2[0], Rm2[1]]
        RjkA = Res(); Rmid = Res(); RcntD = Res(); RcntA = Res(); Rsg = Res()
        kib = [sb("kib%d" % i, [64, 512], BF16) for i in range(2)]; Rkib = [Res(), Res()]
        Rh = [sb("Rh%d" % i, [128, 512], BF16) for i in range(4)]; RRh = [Res() for _ in range(4)]
        bis = sb("bis", [128, 8 + NBIS + 1]); Rbis = Res()
        nmb = [sb("nmb%d" % i, [128, 512], BF16) for i in range(2)]; Rnmb = [Res(), Res()]
        kab = [sb("kab%d" % i, [128, 512], BF16) for i in range(2)]; Rkab = [Res(), Res()]
        vab = [sb("vab%d" % i, [128, 4, 256], BF16) for i in range(2)]; Rvab = [Res(), Res()]
        pt = [sb("pt%d" % i, [128, 512], BF16) for i in range(4)]; Rpt = [Res() for _ in range(4)]
        pt_ctr = [0]
        rden = sb("rden", [128, 512]); Rrden = Res()
        NBB = 3
        bkb = [sb("bkb%d" % i, [128, 4, 128], BF16) for i in range(NBB)]; Rbkb = [Res() for _ in range(NBB)]
        vbb = [sb("vbb%d" % i, [128, 1024], BF16) for i in range(NBB)]; Rvbb = [Res() for _ in range(NBB)]
        bb_ctr = [0]
        gt = [sb("gt%d" % i, [128, N]) for i in range(3)]; Rgt = [Res() for _ in range(3)]
        wgs = [sb("wgs0", [128, 8, 384], BF16)] * 2; Rwgs = [Res()] * 2
        wba = [sb("wba0", [64, 16, 128], BF16)] * 2; Rwba = [Res()] * 2
        wbm = [sb("wbm0", [128, 4, 128], BF16)] * 2; Rwbm = [Res()] * 2
        mtmp = [sb("mtmp%d" % i, [128, N]) for i in range(3)]; Rmtmp = [Res() for _ in range(3)]
        mT = sb("mT", [128, 8, N], BF16); RmT = Res()
        xnT = mT; RxnT = RmT
        xf32 = xt[0]; Rxf32 = Rxt[0]
        xnT32 = xt[1][:].rearrange("p (c n) -> p c n", c=8); RxnT32 = Rxt[1]
        rl = sb("rl", [128, 64]); Rrl = Res()
        comb = [sb("comb%d" % i, [128, 16]) for i in range(QB)]; Rcomb = [Res() for _ in range(QB)]
        sA = [sb("sA%d" % i, [128, N], BF16) for i in range(2)]; RsA = [Res(), Res()]
        hT = [sb("hT%d" % i, [128, 4, N], BF16) for i in range(2)]; RhT = [Res(), Res()]
        ot = xf32; Rot = Rxf32
        out_tokens = []

        def next_pt():
            i = pt_ctr[0] % 4
            pt_ctr[0] += 1
            return pt[i], Rpt[i]

        def dsa(t, sq, part, inter=None):
            nkb = sq + 1
            nk = nkb * 128
            nch = (nk + 511) // 512
            aw = wv[t][:, 4:8]
            am = bis[:, 0:1]; w0 = bis[:, 1:2]; mid = bis[:, 2:3]; cnt = bis[:, 3:4]; sg = bis[:, 4:5]; thr = bis[:, 5:6]
            wt2 = bis[:, 8:8 + NBIS + 1]
            if part == 0:
                for ch in range(nch):
                    ncol = min(512, nk - ch * 512)
                    kb_ = kib[ch % 2]
                    P.dma("sp", lambda e, ch=ch, ncol=ncol, kb_=kb_: e.dma_start(out=kb_[:, 0:ncol], in_=kiT_d[:, ch * 512:ch * 512 + ncol]), reads=[RKd], writes=[Rkib[ch % 2]])
                    for h in range(4):
                        pb, Rp = rot()
                        P.op("pe", lambda e, pb=pb, h=h, ncol=ncol, kb_=kb_: e.matmul(pb[:, 0:ncol], lhsT=iqT[t][:, h, :], rhs=kb_[:, 0:ncol], start=True, stop=True),
                             reads=[RiqT[t], Rkib[ch % 2]], writes=[Rp])
                        P.op("act", lambda e, pb=pb, h=h, ncol=ncol: e.activation(out=Rh[h][:, 0:ncol], in_=pb[:, 0:ncol], func=AF.Relu, scale=aw[:, h:h + 1]),
                             reads=[Rp, Rwv[t]], writes=[RRh[h]])
                    pb, Rp = rot()
                    for h in range(4):
                        P.op("pe", lambda e, pb=pb, h=h, ncol=ncol: e.matmul(pb[:, 0:ncol], lhsT=dg[t][:, h, :], rhs=Rh[h][:, 0:ncol], start=(h == 0), stop=(h == 3)),
                             reads=[Rdg[t], RRh[h]], writes=[Rp])
                    P.op("dve", lambda e, pb=pb, ch=ch, ncol=ncol: e.tensor_copy(out=score[:, ch * 512:ch * 512 + ncol], in_=pb[:, 0:ncol]),
                         reads=[Rp], writes=[Rsc] + RMOE)
                P.op("dve", lambda e: e.tensor_reduce(out=am, in_=score[:, 0:nk], axis=AX, op=ALU.max, apply_absolute_value=True), reads=[Rsc], writes=[Rbis])
                P.op("dve", lambda e: e.tensor_tensor(out=score[:, nk - 256:nk], in0=score[:, nk - 256:nk], in1=cm[:], op=ALU.add), reads=[Rsc, Rc, Rbis], writes=[Rsc])
                P.op("dve", lambda e: e.tensor_tensor(out=score[:, 0:128], in0=score[:, 0:128], in1=m0[:], op=ALU.add), reads=[Rsc, Rc], writes=[Rsc])
                P.op("dve", lambda e: e.tensor_scalar(out=w0, in0=am, scalar1=1.001, scalar2=1e-6, op0=ALU.mult, op1=ALU.add), reads=[Rbis], writes=[Rbis])
                P.op("dve", lambda e: e.tensor_scalar(out=wt2, in0=pow2[:], scalar1=w0, scalar2=None, op0=ALU.mult), reads=[Rbis, Rc], writes=[Rbis])
                P.op("dve", lambda e: e.memset(mid, 0.0), reads=[Rbis], writes=[Rmid])
                hD = max(128, (nk // 2) // 128 * 128)
                n_act = nk - hD
                cntA = bis[:, 6:7]; tmpc = bis[:, 7:8]
                Cthr = 255.5 - 0.5 * n_act
                ksteps = 0
                if inter is not None:
                    ksteps = (inter[1] + NBIS - 1) // NBIS
                for it in range(NBIS):
                    P.op("dve", lambda e: e.tensor_scalar(out=junk[:, 0:hD], in0=score[:, 0:hD], scalar1=mid, scalar2=None, op0=ALU.is_ge, op1=ALU.add, accum_out=cnt),
                         reads=[Rsc, Rmid], writes=[Rjk, RcntD])
                    P.op("act", lambda e: e.activation(out=junk[:, hD:nk], in_=score[:, hD:nk], func=AF.Sign, scale=-1.0, bias=mid, accum_out=cntA),
                         reads=[Rsc, Rmid], writes=[RjkA, RcntA])
                    P.op("dve", lambda e: e.scalar_tensor_tensor(out=tmpc, in0=cntA, scalar=-0.5, in1=cnt, op0=ALU.mult, op1=ALU.add), reads=[RcntD, RcntA], writes=[Rsg])
                    P.op("dve", lambda e: e.tensor_scalar(out=sg, in0=tmpc, scalar1=Cthr, scalar2=0.5, op0=ALU.is_ge, op1=ALU.subtract), reads=[Rsg], writes=[Rsg])
                    P.op("dve", lambda e, it=it: e.scalar_tensor_tensor(out=mid, in0=sg, scalar=wt2[:, it:it + 1], in1=mid, op0=ALU.mult, op1=ALU.add), reads=[Rsg, Rbis], writes=[Rmid])
                    if inter is not None:
                        for _ in range(ksteps):
                            next(inter[0], None)
                P.op("dve", lambda e: e.tensor_tensor(out=thr, in0=mid, in1=wt2[:, NBIS:NBIS + 1], op=ALU.subtract), reads=[Rmid, Rbis], writes=[Rbis])
                return
            def chunk_loads(k4):
                ncol = min(512, nk - k4 * 512)
                nb_here = ncol // 128
                j = k4 % 2
                P.op("dve", lambda e: e.tensor_scalar(out=nmb[j][:, 0:ncol], in0=score[:, k4 * 512:k4 * 512 + ncol], scalar1=thr, scalar2=NEG, op0=ALU.is_lt, op1=ALU.mult),
                     reads=[Rsc, Rbis], writes=[Rnmb[j]])
                P.dma("sp", lambda e: e.dma_start(out=kab[j][:, 0:ncol], in_=KaT_d[:, k4 * 512:k4 * 512 + ncol]), reads=[RKd], writes=[Rkab[j]])
                P.dma("sp", lambda e: e.dma_start(out=vab[j][:, 0:nb_here, :], in_=Va_d[k4 * 512:k4 * 512 + nb_here * 128, :].rearrange("(b p) n -> p b n", p=128)), reads=[RKd], writes=[Rvab[j]])

            items = []
            for k4 in range(nch):
                ncol = min(512, nk - k4 * 512)
                for b in range(ncol // 128):
                    for g in range(2):
                        items.append((k4, b, g))
            last_of_chunk = {}
            for idx, (k4, b, g) in enumerate(items):
                last_of_chunk[k4] = idx
            stage = {}

            def s_pair(m):
                banks = []
                for idx in (2 * m, 2 * m + 1):
                    k4, b, g = items[idx]
                    j = k4 % 2
                    pb, Rp = rot()
                    banks.append((pb, Rp))
                    P.op("pe", lambda e, pb=pb, g=g, b=b, j=j: e.matmul(pb[:], lhsT=kab[j][g * 64:(g + 1) * 64, b * 128:(b + 1) * 128], rhs=QaT[t][g * 64:(g + 1) * 64, :, :].rearrange("p j q -> p (j q)"), start=True, stop=False),
                         reads=[Rkab[j], RQaT[t]], writes=[Rp])
                for (pb, Rp), idx in zip(banks, (2 * m, 2 * m + 1)):
                    k4, b, g = items[idx]
                    j = k4 % 2
                    P.op("pe", lambda e, pb=pb, b=b, j=j: e.matmul(pb[:], lhsT=nmb[j][:, b * 128:(b + 1) * 128], rhs=i4b[:], start=False, stop=True),
                         reads=[Rnmb[j], Rc], writes=[Rp])
                    stage[idx] = (pb, Rp)

            def e_stage(idx):
                pb, Rp = stage[idx]
                p_, Rp_ = next_pt()
                P.op("act", lambda e: e.activation(out=p_[:], in_=pb[:], func=AF.Exp, scale=0.125), reads=[Rp], writes=[Rp_])
                stage[idx] = (p_, Rp_)

            def v_stage(idx):
                k4, b, g = items[idx]
                j = k4 % 2
                kb = k4 * 4 + b
                p_, Rp_ = stage.pop(idx)
                P.op("pe", lambda e: e.matmul(pbank[g][:], lhsT=vab[j][:, b, g * 128:(g + 1) * 128], rhs=p_[:], start=(kb == 0), stop=(kb == nkb - 1)),
                     reads=[Rvab[j], Rp_], writes=[Rb[g]])

            chunk_loads(0)
            if nch > 1:
                chunk_loads(1)
            nit = len(items)
            npair = nit // 2
            s_pair(0)
            for m in range(npair):
                e_stage(2 * m)
                e_stage(2 * m + 1)
                if m + 1 < npair:
                    s_pair(m + 1)
                v_stage(2 * m)
                v_stage(2 * m + 1)
                k4 = items[2 * m + 1][0]
                if last_of_chunk[k4] == 2 * m + 1 and k4 + 2 < nch:
                    chunk_loads(k4 + 2)
            for g in range(2):
                P.op("dve", lambda e, g=g: e.reciprocal(out=rden[0:64, :], in_=pbank[g][64:128, :]), reads=[Rb[g]], writes=[Rrden])
                P.op("dve", lambda e, g=g: e.tensor_tensor(out=yaT[:, g * 4:(g + 1) * 4, t * 128:(t + 1) * 128], in0=pbank[g][0:64, :].rearrange("p (j q) -> p j q", j=4), in1=rden[0:64, :].rearrange("p (j q) -> p j q", j=4), op=ALU.mult),
                     reads=[Rb[g], Rrden], writes=[RyaT])

        def bmix(t, sq):
            first = [True, True]
            work = []
            for g in range(3):
                for jj in range(B_NB[g], -1, -1):
                    kb = sq - jj
                    if kb >= 0:
                        work.append((g, jj, kb))
            nwork = len(work)
            bufof = {}

            def b_load(w):
                g, jj, kb = work[w]
                i = bb_ctr[0] % NBB
                bb_ctr[0] += 1
                bufof[w] = i
                P.dma("sp", lambda e: e.dma_start(out=bkb[i][:], in_=bkT_d[:, g, :, kb * 128:(kb + 1) * 128]), reads=[RKd], writes=[Rbkb[i]])
                P.dma("sp", lambda e: e.dma_start(out=vbb[i][:], in_=Vb_d[kb * 128:(kb + 1) * 128, g, :]), reads=[RKd], writes=[Rvbb[i]])

            items = [(w, hh) for w in range(nwork) for hh in range(2)]
            nit = len(items)
            stage = {}
            loaded = [0]

            def ensure_loaded(upto):
                while loaded[0] <= min(upto, nwork - 1):
                    b_load(loaded[0])
                    loaded[0] += 1

            def s_pair(w):
                g, jj, kb = work[w]
                ensure_loaded(w + 1)
                i = bufof[w]
                mtab = mb0t if kb == 0 else mbt
                mi = B_MOFF[g] + jj
                banks = []
                for hh in range(2):
                    pb, Rp = rot()
                    banks.append((pb, Rp))
                    P.op("pe", lambda e, pb=pb: e.matmul(pb[:], lhsT=mtab[:, mi * 128:(mi + 1) * 128], rhs=i4b[:], start=True, stop=False),
                         reads=[Rc], writes=[Rp])
                for p2 in range(4):
                    for hh in range(2):
                        pb, Rp = banks[hh]
                        P.op("pe", lambda e, p2=p2, hh=hh, pb=pb: e.matmul(pb[:, p2 * 128:(p2 + 1) * 128], lhsT=bkb[i][hh * 64:(hh + 1) * 64, p2, :], rhs=bqT[t][hh * 64:(hh + 1) * 64, g, p2, :], start=False, stop=(p2 == 3), skip_group_check=True),
                             reads=[Rbkb[i], RbqT[t]], writes=[Rp])
                for hh in range(2):
                    stage[2 * w + hh] = banks[hh]

            def e_stage(idx):
                pb, Rp = stage[idx]
                p_, Rp_ = next_pt()
                P.op("act", lambda e: e.activation(out=p_[:], in_=pb[:], func=AF.Exp, scale=0.125), reads=[Rp], writes=[Rp_])
                stage[idx] = (p_, Rp_)

            def v_stage(idx):
                w, hh = items[idx]
                i = bufof[w]
                p_, Rp_ = stage.pop(idx)
                for h4 in range(4):
                    h = 2 * h4 + hh
                    st = first[hh]
                    first[hh] = False
                    P.op("pe", lambda e, h4=h4, h=h, st=st: e.matmul(pbank[2 + hh][:, h4 * 128:(h4 + 1) * 128], lhsT=vbb[i][:, h * 128:(h + 1) * 128], rhs=p_[:, h4 * 128:(h4 + 1) * 128], start=st, stop=(w == nwork - 1 and h4 == 3), skip_group_check=True),
                         reads=[Rvbb[i], Rp_], writes=[Rb[2 + hh]])

            yield nwork
            s_pair(0)
            for w in range(nwork):
                e_stage(2 * w)
                e_stage(2 * w + 1)
                if w + 1 < nwork:
                    s_pair(w + 1)
                v_stage(2 * w)
                v_stage(2 * w + 1)
                yield w
            for hh in range(2):
                P.op("dve", lambda e, hh=hh: e.reciprocal(out=rden[0:64, :], in_=pbank[2 + hh][64:128, :]), reads=[Rb[2 + hh]], writes=[Rrden])
                P.op("dve", lambda e, hh=hh: e.tensor_tensor(out=ybT[:, :, t * 128:(t + 1) * 128].rearrange("p (p2 hf) q -> p hf p2 q", hf=2)[:, hh], in0=pbank[2 + hh][0:64, :].rearrange("p (j q) -> p j q", j=4), in1=rden[0:64, :].rearrange("p (j q) -> p j q", j=4), op=ALU.mult),
                     reads=[Rb[2 + hh], Rrden], writes=[RybT])

        ptm = [sb("ptm%d" % i, [128, 512], BF16) for i in range(2)]
        Rptm = [Res(), Res()]

        def memattn_a(t):
            sc = 128.0 ** -0.5
            for mbk in range(2):
                pb, Rp = rot()
                for h in range(4):
                    P.op("pe", lambda e, pb=pb, h=h, mbk=mbk: e.matmul(pb[:, h * 128:(h + 1) * 128], lhsT=kmT[:, h, mbk * 128:(mbk + 1) * 128], rhs=mqT[t][:, h, :], start=(h == 0), stop=(h == 3), skip_group_check=True),
                         reads=[Rkm, RmqT[t]], writes=[Rp])
                P.op("act", lambda e, pb=pb, mbk=mbk: e.activation(out=ptm[mbk][:], in_=pb[:], func=AF.Exp, scale=sc), reads=[Rp], writes=[Rptm[mbk]])

        def memattn_b(t):
            for mbk in range(2):
                for h in range(4):
                    P.op("pe", lambda e, h=h, mbk=mbk: e.matmul(pbank[0][:, h * 128:(h + 1) * 128], lhsT=vmb[:, mbk, h * 128:(h + 1) * 128], rhs=ptm[mbk][:, h * 128:(h + 1) * 128], start=(mbk == 0 and h == 0), stop=(mbk == 1 and h == 3), skip_group_check=True),
                         reads=[Rkm, Rptm[mbk]], writes=[Rb[0]])
                P.op("pe", lambda e, mbk=mbk: e.matmul(pbank[1][:], lhsT=ones_b[:], rhs=ptm[mbk][:], start=(mbk == 0), stop=(mbk == 1)),
                     reads=[Rc, Rptm[mbk]], writes=[Rb[1]])
            P.op("dve", lambda e: e.reciprocal(out=rden[:], in_=pbank[1][:]), reads=[Rb[1]], writes=[Rrden])
            P.op("dve", lambda e: e.tensor_tensor(out=ymT[:, :, t * 128:(t + 1) * 128], in0=pbank[0][:].rearrange("p (j q) -> p j q", j=4), in1=rden[:].rearrange("p (j q) -> p j q", j=4), op=ALU.mult),
                 reads=[Rb[0], Rrden], writes=[RymT])

        for I in range(NIT):
            xpre = wslab[0][:].rearrange("p c n -> p (c n)").bitcast(F32)
            if I == 0 or QB != 2:
                for t in range(QB):
                    sq = 2 * (I * QB + t) + 1
                    P.dma("sp", lambda e, t=t, sq=sq: e.dma_start(out=xh[:, t, :], in_=xs[sq * 128:(sq + 1) * 128, :]), writes=[Rxh[t]])
                    P.dma("sp", lambda e, t=t, sq=sq: e.dma_start(out=csq[t][:], in_=cs[sq * 128:(sq + 1) * 128, :]), writes=[Rcsq[t]])
                    rms_T(xh[:, t, :], Rxh[t], 0, lambda c, t=t: nTq[:, c, t * 128:(t + 1) * 128], RnTq)
            else:
                for t in range(QB):
                    P.op("pool", lambda e, t=t: e.tensor_copy(out=xh[:, t, :], in_=xpre[:, t * 1024:(t + 1) * 1024]), reads=[Rwslab[0]], writes=[Rxh[t]])
                slab_ctr[0] = 1

            def prefetch_next(I1):
                for t in range(QB):
                    sq = 2 * (I1 * QB + t) + 1
                    P.dma("sp", lambda e, t=t, sq=sq: e.dma_start(out=xpre[:, t * 1024:(t + 1) * 1024], in_=xs[sq * 128:(sq + 1) * 128, :]), writes=[Rwslab[0]])
                    P.dma("sp", lambda e, t=t, sq=sq: e.dma_start(out=csq[t][:], in_=cs[sq * 128:(sq + 1) * 128, :]), writes=[Rcsq[t]])
                for t in range(QB):
                    rms_T(xpre[:, t * 1024:(t + 1) * 1024], Rwslab[0], 0, lambda c, t=t: nTq[:, c, t * 128:(t + 1) * 128], RnTq)
            for (nm, c0, ncol) in Q_SLABS:
                ws, Rws = load_slab(wq_b, c0, ncol)
                for t in range(QB):
                    pb, Rp = proj(nTq, RnTq, t * 128, ws, Rws, ncol)
                    t_, Rt_ = next_tst()
                    if nm == "aq":
                        rope(pb[:, 0:512], Rp, 8, csq[t][:], Rcsq[t], t_[:, 0:512], Rt_, perm=True)
                        transposes(t_, Rt_, 128, 4, lambda c, t=t: QaT[t][:, c, :], RQaT[t])
                    elif nm == "iq":
                        rope(pb[:, 0:256], Rp, 4, csq[t][:], Rcsq[t], t_[:, 0:256], Rt_)
                        transposes(t_, Rt_, 64, 4, lambda c, t=t: iqT[t][:, c, :], RiqT[t])
                        w_ = wv[t]
                        P.op("dve", lambda e, pb=pb, w_=w_: e.tensor_copy(out=w_[:, 0:4], in_=pb[:, 256:260]), reads=[Rp], writes=[Rwv[t]])
                        P.op("dve", lambda e, w_=w_: e.tensor_scalar(out=w_[:, 8:12], in0=w_[:, 0:4], scalar1=0.0, scalar2=2.0, op0=ALU.is_ge, op1=ALU.mult), reads=[Rwv[t]], writes=[Rwv[t]])
                        P.op("dve", lambda e, w_=w_: e.tensor_scalar(out=w_[:, 8:12], in0=w_[:, 8:12], scalar1=-1.0, scalar2=None, op0=ALU.add), reads=[Rwv[t]], writes=[Rwv[t]])
                        P.op("dve", lambda e, w_=w_: e.scalar_tensor_tensor(out=w_[:, 4:8], in0=w_[:, 0:4], scalar=0.0625, in1=w_[:, 8:12], op0=ALU.mult, op1=ALU.mult), reads=[Rwv[t]], writes=[Rwv[t]])
                        for h in range(4):
                            P.op("dve", lambda e, w_=w_, h=h, t=t: e.tensor_scalar(out=dg[t][:, h, :], in0=identf[:], scalar1=w_[:, 8 + h:9 + h], scalar2=None, op0=ALU.mult), reads=[Rwv[t], Rc], writes=[Rdg[t]])
                    elif nm.startswith("bq"):
                        g = int(nm[2])
                        rope(pb[:, 0:512], Rp, 8, csq[t][:], Rcsq[t], t_[:, 0:512], Rt_)
                        transposes(t_, Rt_, 128, 4, lambda c, t=t, g=g: bqT[t][:, g, c, :], RbqT[t])
                    else:
                        P.op("act", lambda e, pb=pb, t_=t_: e.copy(out=t_[:], in_=pb[:]), reads=[Rp], writes=[Rt_])
                        transposes(t_, Rt_, 128, 4, lambda c, t=t: mqT[t][:, c, :], RmqT[t])
            flush_deferred()
            if stop == "q":
                finish(); return nc, P
            for t in range(QB):
                sq = 2 * (I * QB + t) + 1
                memattn_a(t)
                gen = bmix(t, sq)
                nit_b = next(gen)
                dsa(t, sq, 0, inter=(gen, nit_b))
                for _ in gen:
                    pass
                memattn_b(t)
                dsa(t, sq, 1)
            if dbg and I == NIT - 1:
                for (nm, src, R_) in (("d_ya", yaT, RyaT), ("d_yb", ybT, RybT), ("d_ym", ymT, RymT)):
                    np_ = src.shape[0]
                    P.op("dve", lambda e, src=src, np_=np_: e.tensor_copy(out=score[0:np_, 0:src.shape[1] * N], in_=src[:].rearrange("p a n -> p (a n)")), reads=[R_], writes=[Rsc] + RMOE)
                    out_tokens.append(P.dma("sp", lambda e, nm=nm, np_=np_, src=src: e.dma_start(out=dbg_d[nm], in_=score[0:np_, 0:src.shape[1] * N]), reads=[Rsc]))
            for f in range(8):
                j = f % 2
                P.dma("sp", lambda e, f=f, j=j: e.dma_start(out=wgs[j][:], in_=wg_b.rearrange("(c p) n -> p c n", p=128)[:, :, f * 384:(f + 1) * 384]), reads=[Rw_b], writes=[Rwgs[j]])
                P.dma("sp", lambda e, f=f, j=j: e.dma_start(out=wba[j][:], in_=wbr_b[0:1024, f * 128:(f + 1) * 128].rearrange("(h p) n -> p h n", p=64)), reads=[Rw_b], writes=[Rwba[j]])
                P.dma("sp", lambda e, f=f, j=j: e.dma_start(out=wbm[j][:], in_=wbr_b[1024:1536, f * 128:(f + 1) * 128].rearrange("(h p) n -> p h n", p=128)), reads=[Rw_b], writes=[Rwbm[j]])
                for r in range(3):
                    pb, Rp = rot()
                    for c in range(8):
                        P.op("pe", lambda e, pb=pb, c=c, r=r, j=j: e.matmul(pb[:, 0:N], lhsT=wgs[j][:, c, r * 128:(r + 1) * 128], rhs=nTq[:, c, :], start=(c == 0), stop=(c == 7)),
                             reads=[Rwgs[j], RnTq], writes=[Rp])
                    P.op("act", lambda e, pb=pb, r=r, f=f: e.activation(out=gt[r][:], in_=pb[:, 0:N], func=AF.Sigmoid, bias=bgate[:, f * 3 + r:f * 3 + r + 1], scale=1.0),
                         reads=[Rp, Rc], writes=[Rgt[r]])
                for r in range(3):
                    pb, Rp = rot()
                    if r < 2:
                        ysrc, Ry = (yaT, RyaT) if r == 0 else (ybT, RybT)
                        for h in range(8):
                            P.op("pe", lambda e, pb=pb, h=h, r=r, j=j, ysrc=ysrc: e.matmul(pb[:, 0:N], lhsT=wba[j][:, r * 8 + h, :], rhs=ysrc[:, h, :], start=(h == 0), stop=(h == 7)),
                                 reads=[Rwba[j], Ry], writes=[Rp])
                    else:
                        for h in range(4):
                            P.op("pe", lambda e, pb=pb, h=h, j=j: e.matmul(pb[:, 0:N], lhsT=wbm[j][:, h, :], rhs=ymT[:, h, :], start=(h == 0), stop=(h == 3)),
                                 reads=[Rwbm[j], RymT], writes=[Rp])
                    P.op("dve", lambda e, pb=pb, r=r: e.tensor_tensor(out=mtmp[r][:], in0=pb[:, 0:N], in1=gt[r][:], op=ALU.mult), reads=[Rp, Rgt[r]], writes=[Rmtmp[r]])
                P.op("pool", lambda e: e.tensor_tensor(out=mtmp[0][:], in0=mtmp[0][:], in1=mtmp[1][:], op=ALU.add), reads=[Rmtmp[0], Rmtmp[1]], writes=[Rmtmp[0]])
                P.op("pool", lambda e, f=f: e.tensor_tensor(out=mT[:, f, :], in0=mtmp[0][:], in1=mtmp[2][:], op=ALU.add), reads=[Rmtmp[0], Rmtmp[2]], writes=[RmT])
            for n2 in range(2):
                ws, Rws = load_slab(wo_b, n2 * 512, 512)
                for t in range(QB):
                    pb, Rp = proj(mT, RmT, t * 128, ws, Rws, 512)
                    P.op("dve", lambda e, pb=pb, t=t, n2=n2: e.tensor_tensor(out=xh[:, t, n2 * 512:(n2 + 1) * 512], in0=pb[:], in1=xh[:, t, n2 * 512:(n2 + 1) * 512], op=ALU.add),
                         reads=[Rp, Rxh[t]], writes=[Rxh[t]])
            if dbg and I == NIT - 1:
                out_tokens.append(P.dma("sp", lambda e: e.dma_start(out=dbg_d["d_h"], in_=xh[:].rearrange("p t n -> p (t n)")), reads=Rxh))
            if stop == "d":
                finish(); return nc, P
            for t in range(QB):
                si = rms_T(xh[:, t, :], Rxh[t], 16, lambda c, t=t: xnT[:, c, t * 128:(t + 1) * 128], RxnT)
                rs = rs_t[:, si:si + 1]
                P.op("dve", lambda e, t=t, rs=rs: e.tensor_scalar(out=xf32[:], in0=xh[:, t, :], scalar1=rs, scalar2=None, op0=ALU.mult), reads=[Rxh[t], Rstat[si]], writes=[Rxf32])
                for half in range(2):
                    pb, Rp = rot()
                    for c4 in range(4):
                        c = half * 4 + c4
                        P.op("pe", lambda e, pb=pb, c=c, c4=c4: e.transpose(out=pb[:, c4 * 128:(c4 + 1) * 128], in_=xf32[:, c * 128:(c + 1) * 128], identity=identf[:]), reads=[Rxf32, Rc], writes=[Rp])
                    for c4 in range(4):
                        c = half * 4 + c4
                        P.op("dve", lambda e, pb=pb, c=c, c4=c4: e.tensor_scalar(out=xnT32[:, c, :], in0=pb[:, c4 * 128:(c4 + 1) * 128], scalar1=gcols[:, 16 + c:17 + c], scalar2=None, op0=ALU.mult), reads=[Rp, Rc], writes=[RxnT32])
                pb, Rp = rot()
                for c in range(8):
                    P.op("pe", lambda e, pb=pb, c=c: e.matmul(pb[:, 0:20], lhsT=xnT32[:, c, :], rhs=wr32[:, c, :], start=(c == 0), stop=(c == 7)), reads=[RxnT32, Rc], writes=[Rp])
                lg = rl[:, 0:20]; gmx = rl[:, 20:21]; ngmx = rl[:, 21:22]; gex = rl[:, 22:26]; gsum = rl[:, 26:27]; gw = rl[:, 27:28]
                ohg = rl[:, 28:32]; sel = rl[:, 32:36]; m1 = rl[:, 36:37]; oh1 = rl[:, 37:41]; sel2 = rl[:, 41:45]; m2 = rl[:, 45:46]
                oh2 = rl[:, 46:50]; ee = rl[:, 50:51]; p1 = rl[:, 51:52]; p2 = rl[:, 52:53]; cw = rl[:, 53:57]; nm1 = rl[:, 57:58]; cw2 = rl[:, 58:62]
                RW = dict(reads=[Rrl], writes=[Rrl])
                P.op("dve", lambda e, pb=pb: e.tensor_tensor(out=lg, in0=pb[:, 0:20], in1=rbias[:], op=ALU.add), reads=[Rp, Rc], writes=[Rrl])
                P.op("dve", lambda e: e.tensor_reduce(out=gmx, in_=lg[:, 0:4], axis=AX, op=ALU.max), **RW)
                P.op("dve", lambda e: e.tensor_scalar(out=ngmx, in0=gmx, scalar1=-1.0, scalar2=None, op0=ALU.mult), **RW)
                P.op("act", lambda e: e.activation(out=gex, in_=lg[:, 0:4], func=AF.Exp, bias=ngmx, scale=1.0, accum_out=gsum), **RW)
                P.op("dve", lambda e: e.reciprocal(out=gw, in_=gsum), **RW)
                P.op("dve", lambda e: e.tensor_scalar(out=ohg, in0=lg[:, 0:4], scalar1=gmx, scalar2=None, op0=ALU.is_equal), **RW)
                P.op("dve", lambda e: e.tensor_scalar(out=sel, in0=lg[:, 4:8], scalar1=ohg[:, 0:1], scalar2=None, op0=ALU.mult), **RW)
                for g in range(1, 4):
                    P.op("dve", lambda e, g=g: e.scalar_tensor_tensor(out=sel, in0=lg[:, 4 + 4 * g:8 + 4 * g], scalar=ohg[:, g:g + 1], in1=sel, op0=ALU.mult, op1=ALU.add), **RW)
                P.op("dve", lambda e: e.tensor_reduce(out=m1, in_=sel, axis=AX, op=ALU.max), **RW)
                P.op("dve", lambda e: e.tensor_scalar(out=oh1, in0=sel, scalar1=m1, scalar2=None, op0=ALU.is_equal), **RW)
                P.op("dve", lambda e: e.scalar_tensor_tensor(out=sel2, in0=oh1, scalar=NINF, in1=sel, op0=ALU.mult, op1=ALU.add), **RW)
                P.op("dve", lambda e: e.tensor_reduce(out=m2, in_=sel2, axis=AX, op=ALU.max), **RW)
                P.op("dve", lambda e: e.tensor_scalar(out=oh2, in0=sel2, scalar1=m2, scalar2=None, op0=ALU.is_equal), **RW)
                P.op("dve", lambda e: e.tensor_scalar(out=nm1, in0=m1, scalar1=-1.0, scalar2=None, op0=ALU.mult), **RW)
                P.op("act", lambda e: e.activation(out=ee, in_=m2, func=AF.Exp, bias=nm1, scale=1.0), **RW)
                P.op("dve", lambda e: e.tensor_scalar(out=p1, in0=ee, scalar1=1.0, scalar2=None, op0=ALU.add), **RW)
                P.op("dve", lambda e: e.reciprocal(out=p1, in_=p1), **RW)
                P.op("dve", lambda e: e.tensor_tensor(out=p2, in0=ee, in1=p1, op=ALU.mult), **RW)
                P.op("dve", lambda e: e.tensor_scalar(out=cw, in0=oh1, scalar1=p1, scalar2=None, op0=ALU.mult), **RW)
                P.op("dve", lambda e: e.scalar_tensor_tensor(out=cw2, in0=oh2, scalar=p2, in1=cw, op0=ALU.mult, op1=ALU.add), **RW)
                P.op("dve", lambda e: e.tensor_scalar(out=cw, in0=cw2, scalar1=gw, scalar2=None, op0=ALU.mult), **RW)
                for g in range(4):
                    P.op("dve", lambda e, g=g, t=t: e.tensor_scalar(out=comb[t][:, g * 4:(g + 1) * 4], in0=cw, scalar1=ohg[:, g:g + 1], scalar2=None, op0=ALU.mult), reads=[Rrl], writes=[Rcomb[t]])
            for ex in range(16):
                if ex == 2 and I + 1 < NIT and QB == 2:
                    prefetch_next(I + 1)
                j = ex % 2
                base = j * 12288
                w1e = r1[:, base:base + 4096].rearrange("p (c n) -> p c n", c=8)
                w3e = r1[:, base + 4096:base + 8192].rearrange("p (c n) -> p c n", c=8)
                w2e = r1[:, base + 8192:base + 12288].rearrange("p (c n) -> p c n", c=4)
                if ex == 0:
                    gtok = []
                    for R_ in (Rsc, Rjk, RjkA):
                        if R_.w is not None:
                            gtok.append(R_.w)
                        gtok += list(R_.r)
                    P.wait_tokens("sp", gtok)
                P.dma("sp", lambda e, ex=ex, w1e=w1e: e.dma_start(out=w1e, in_=w1_b[ex * D:(ex + 1) * D, :].rearrange("(c p) n -> p c n", p=128)), reads=[Rw_b], writes=[Rm1[j]])
                P.dma("sp", lambda e, ex=ex, w3e=w3e: e.dma_start(out=w3e, in_=w3_b[ex * D:(ex + 1) * D, :].rearrange("(c p) n -> p c n", p=128)), reads=[Rw_b], writes=[Rm3[j]])
                P.dma("sp", lambda e, ex=ex, w2e=w2e: e.dma_start(out=w2e, in_=w2_b[ex * 512:(ex + 1) * 512, :].rearrange("(c p) n -> p c n", p=128)), reads=[Rw_b], writes=[Rm2[j]])
                for c in range(4):
                    pa, Rpa = rot()
                    for k in range(8):
                        P.op("pe", lambda e, pa=pa, k=k, c=c, w1e=w1e: e.matmul(pa[:, 0:N], lhsT=w1e[:, k, c * 128:(c + 1) * 128], rhs=xnT[:, k, :], start=(k == 0), stop=(k == 7)), reads=[Rm1[j], RxnT], writes=[Rpa])
                    pb, Rp = rot()
                    for k in range(8):
                        P.op("pe", lambda e, pb=pb, k=k, c=c, w3e=w3e: e.matmul(pb[:, 0:N], lhsT=w3e[:, k, c * 128:(c + 1) * 128], rhs=xnT[:, k, :], start=(k == 0), stop=(k == 7)), reads=[Rm3[j], RxnT], writes=[Rp])
                    sj = c % 2
                    P.op("act", lambda e, pa=pa, sj=sj: e.activation(out=sA[sj][:], in_=pa[:, 0:N], func=AF.Silu), reads=[Rpa], writes=[RsA[sj]])
                    P.op("dve", lambda e, pb=pb, sj=sj, c=c, j=j: e.tensor_tensor(out=hT[j][:, c, :], in0=pb[:, 0:N], in1=sA[sj][:], op=ALU.mult), reads=[Rp, RsA[sj]], writes=[RhT[j]])
                for t in range(QB):
                    for n2 in range(2):
                        pb, Rp = rot()
                        for c in range(4):
                            P.op("pe", lambda e, pb=pb, c=c, t=t, n2=n2, j=j, w2e=w2e: e.matmul(pb[:], lhsT=hT[j][:, c, t * 128:(t + 1) * 128], rhs=w2e[:, c, n2 * 512:(n2 + 1) * 512], start=(c == 0), stop=(c == 3)), reads=[RhT[j], Rm2[j]], writes=[Rp])
                        P.op("dve", lambda e, pb=pb, t=t, n2=n2, ex=ex: e.scalar_tensor_tensor(out=xh[:, t, n2 * 512:(n2 + 1) * 512], in0=pb[:], scalar=comb[t][:, ex:ex + 1], in1=xh[:, t, n2 * 512:(n2 + 1) * 512], op0=ALU.mult, op1=ALU.add),
                             reads=[Rp, Rcomb[t], Rxh[t]], writes=[Rxh[t]])
            for t in range(QB):
                qi = I * QB + t
                ss = ss_t[:, 0:1]; rs = rs_t[:, 0:1]
                P.op("act", lambda e, t=t, ss=ss: e.activation(out=sqj[:], in_=xh[:, t, :], func=AF.Square, accum_out=ss), reads=[Rxh[t]], writes=[Rsqj, Rstat[0]])
                P.op("dve", lambda e, ss=ss, rs=rs: e.tensor_scalar(out=rs, in0=ss, scalar1=1.0 / D, scalar2=1e-6, op0=ALU.mult, op1=ALU.add), reads=[Rstat[0]], writes=[Rstat[0]])
                P.op("act", lambda e, rs=rs: e.activation(out=rs, in_=rs, func=AF.Sqrt), reads=[Rstat[0]], writes=[Rstat[0]])
                P.op("dve", lambda e, rs=rs: e.reciprocal(out=rs, in_=rs), reads=[Rstat[0]], writes=[Rstat[0]])
                P.op("dve", lambda e, t=t, rs=rs: e.scalar_tensor_tensor(out=ot[:], in0=xh[:, t, :], scalar=rs, in1=gfin[:], op0=ALU.mult, op1=ALU.mult), reads=[Rxh[t], Rstat[0], Rc], writes=[Rot])
                out_tokens.append(P.dma("sp", lambda e, qi=qi: e.dma_start(out=out_d[qi * 128:(qi + 1) * 128, :], in_=ot[:]), reads=[Rot]))
        P.wait_tokens("sp", out_tokens)
        P.emit()
    return nc, P


def _host_consts(S, parity):
    pos = np.arange(S, dtype=np.float32) - (0 if parity else 128)
    half = 32
    inv = (10000.0 ** (-np.arange(half, dtype=np.float32) / half)).astype(np.float32)
    ang = pos[:, None] * inv[None, :]
    cs = np.concatenate([np.cos(ang), np.sin(ang)], axis=1).astype(np.float32)
    q = np.arange(128)[:, None]; k = np.arange(128)[None, :]
    cm = np.zeros((128, 256), np.float32)
    cm[:, 128:] = np.where(k <= q, 0.0, NINF)
    m0 = np.full((128, 128), 0.0 if parity else NINF, np.float32)
    mb = np.zeros((128, 24, 128), np.float32)
    for g, (W, d) in enumerate(B_PAT):
        for jj in range(B_NB[g] + 1):
            diff = 128 * jj + q - k
            ok = (diff >= 0) & (diff <= W) & (diff % d == 0)
            mb[:, B_MOFF[g] + jj, :] = np.where(ok, 0.0, NEG)
    mb0 = mb.copy() if parity else np.full_like(mb, NEG)
    return cs, cm, m0, mb.reshape(128, -1), mb0.reshape(128, -1)


def _prep_inputs(inputs, S, n_cores=8):
    f = lambda a: np.ascontiguousarray(np.asarray(a, dtype=np.float32))
    x = f(inputs["x"]); mem = f(inputs["mem"])
    w_in = f(inputs["w_in"])[0]
    o = np.cumsum([0, 512, 128, 128, 256, 64, 4, 1536, 1536, 1536, 512])
    aq, ak, av, iq, ik, iw, bq, bk, bv, mq = [w_in[:, o[i]:o[i + 1]] for i in range(10)]
    wk = np.ascontiguousarray(np.concatenate([ak, ik, av, bk, bv], axis=1))
    wq = np.ascontiguousarray(np.concatenate([aq, iq, iw, bq, mq], axis=1))
    wg0 = f(inputs["w_gate"])[0]
    wg = np.ascontiguousarray(wg0.reshape(D, 3, 8, 128).transpose(0, 2, 1, 3).reshape(D, 3072))
    bg0 = f(inputs["b_gate"])[0].reshape(3, 8, 128)
    bgate = np.ascontiguousarray(bg0.transpose(2, 1, 0).reshape(128, 24))
    wbr = np.ascontiguousarray(f(inputs["w_branch"])[0].reshape(1536, D))
    wo = f(inputs["w_out"])[0]
    wmem = f(inputs["w_mem_kv"])[0]
    w1 = np.ascontiguousarray(f(inputs["w1"])[0].reshape(16 * D, 512))
    w3 = np.ascontiguousarray(f(inputs["w3"])[0].reshape(16 * D, 512))
    w2 = np.ascontiguousarray(f(inputs["w2"])[0].reshape(16 * 512, D))
    wsub = f(inputs["w_sub"])[0]
    wr = np.ascontiguousarray(np.concatenate([f(inputs["w_group"])[0], wsub.transpose(1, 0, 2).reshape(D, 16)], axis=1))
    gc = lambda g: np.asarray(g, np.float32).reshape(8, 128).T
    gcols = np.ascontiguousarray(np.concatenate([gc(f(inputs["g_mix"])[0]), gc(f(inputs["g_mem"])[0]), gc(f(inputs["g_ffn"])[0])], axis=1))
    gfin = np.ascontiguousarray(np.broadcast_to(f(inputs["g_final"])[None, :], (128, D)))
    rb = np.concatenate([f(inputs["b_group"])[0], f(inputs["b_sub"])[0].reshape(16)])
    rbias = np.ascontiguousarray(np.broadcast_to(rb[None, :], (128, 20)))
    ident = np.eye(128, dtype=np.float32)
    shared = dict(wk=wk, wq=wq, wg=wg, wbr=wbr, wo=wo, wmem=wmem, w1=w1, w3=w3, w2=w2, wr=wr, gcols=gcols,
                  bgate=bgate, gfin=gfin, rbias=rbias, ident=ident)
    in_maps = []
    for c in range(n_cores):
        b, p = c // 2, c % 2
        if p == 0:
            xs = np.concatenate([np.zeros((128, D), np.float32), x[b, :S - 128]], axis=0)
        else:
            xs = x[b, :S]
        cs, cm, m0, mb, mb0 = _host_consts(S, p)
        m = dict(shared)
        m.update(xs=np.ascontiguousarray(xs), cs=cs, memx=np.ascontiguousarray(mem[b]), dsa_cm=cm, dsa_m0=m0, mbias=mb, mb0=mb0)
        in_maps.append(m)
    return in_maps


_CACHE = {}


def run(inputs, S, QB=2, dbg=False):
    key = (S, QB, dbg)
    if key not in _CACHE:
        _CACHE[key] = build(S, QB, dbg)
    nc, P = _CACHE[key]
    in_maps = _prep_inputs(inputs, S)
    res = run_bass_kernel_spmd(nc, in_maps, core_ids=list(range(8)))
    B = 4
    out = np.zeros((B, S, D), np.float32)
    for c in range(8):
        b, p = c // 2, c % 2
        o = np.asarray(res.results[c]["out"]).reshape(S // 256, 128, D)
        out[b].reshape(S // 256, 2, 128, D)[:, p] = o
    return out, res


def kernel(**inputs):
    out, _ = run(inputs, 8192, QB=2)
    return out
```
